# Optimizing a Trainium2 kernel written in Bass

```python
import jax, jax.numpy as jnp
from jax import lax
import numpy as np

D_MODEL = 1024
BATCH = 4
SEQ = 8192
DEPTH = 4

HEAD_DIM = 64
N_HEADS_MOBA = 4
N_HEADS_FOX = 4
N_HEADS_MLA = 4
N_HEADS_DIL = 4
W_MOBA = N_HEADS_MOBA * HEAD_DIM
W_FOX = N_HEADS_FOX * HEAD_DIM
W_DIL = N_HEADS_DIL * HEAD_DIM

MOBA_BLOCK = 256
MOBA_TOPK = 3
MOBA_Q_CHUNK = 64
Q_BLOCK = 128

MLA_Q_RANK = 192
MLA_KV_RANK = 128
MLA_NOPE_DIM = 64
MLA_ROPE_DIM = 32
MLA_V_DIM = 64
MLA_Q_UP = N_HEADS_MLA * (MLA_NOPE_DIM + MLA_ROPE_DIM)
MLA_KV_UP = N_HEADS_MLA * (MLA_NOPE_DIM + MLA_V_DIM)
W_MLA = N_HEADS_MLA * MLA_V_DIM

DILATED_CONFIGS = ((128, 1), (512, 4), (2048, 16))
ROPE_THETA = 10000.0
RMS_EPS = 1e-6
D_FF = -(-8 * D_MODEL // (3 * 256)) * 256

IN_SIZES = (3 * W_MOBA, 3 * W_FOX, N_HEADS_FOX, MLA_Q_RANK, MLA_KV_RANK, MLA_ROPE_DIM, 3 * W_DIL)
D_IN = 3 * W_MOBA + 3 * W_FOX + N_HEADS_FOX + MLA_Q_RANK + MLA_KV_RANK + MLA_ROPE_DIM + 3 * W_DIL
SPLIT_POINTS = (
    3 * W_MOBA,
    3 * W_MOBA + 3 * W_FOX,
    3 * W_MOBA + 3 * W_FOX + N_HEADS_FOX,
    3 * W_MOBA + 3 * W_FOX + N_HEADS_FOX + MLA_Q_RANK,
    3 * W_MOBA + 3 * W_FOX + N_HEADS_FOX + MLA_Q_RANK + MLA_KV_RANK,
    3 * W_MOBA + 3 * W_FOX + N_HEADS_FOX + MLA_Q_RANK + MLA_KV_RANK + MLA_ROPE_DIM,
)
MIX_WIDTH = W_MOBA + W_FOX + W_MLA + W_DIL

kernel_name = "hybrid_parallel_heads_decoder"


def rmsnorm(x, g):
    xf = x.astype(jnp.float32)
    y = xf * lax.rsqrt(jnp.mean(xf * xf, axis=-1, keepdims=True) + RMS_EPS)
    return (y * g.astype(jnp.float32)).astype(x.dtype)


def rope_tables(positions, dim):
    inv_freq = ROPE_THETA ** (-jnp.arange(0, dim, 2, dtype=jnp.float32) / dim)
    ang = positions.astype(jnp.float32)[..., None] * inv_freq
    return jnp.cos(ang), jnp.sin(ang)


def apply_rope(x, cos, sin):
    x1, x2 = jnp.split(x.astype(jnp.float32), 2, axis=-1)
    c = cos[:, None]
    s = sin[:, None]
    return jnp.concatenate([x1 * c - x2 * s, x2 * c + x1 * s], axis=-1).astype(x.dtype)


def to_heads(t, n_heads):
    b, s, _ = t.shape
    return t.reshape(b, s, n_heads, -1).transpose(0, 2, 1, 3)


def from_heads(t):
    b, h, s, d = t.shape
    return t.transpose(0, 2, 1, 3).reshape(b, s, h * d)


def pad_seq(t, new_len):
    pad = new_len - t.shape[2]
    return jnp.pad(t, ((0, 0), (0, 0), (0, pad), (0, 0)))


def blocked_causal_attention(q, k, v, scale, cum=None):
    b, h, s, dq = q.shape
    nq = s // Q_BLOCK
    qb = jnp.moveaxis(q.reshape(b, h, nq, Q_BLOCK, dq), 2, 0)
    starts = jnp.arange(nq, dtype=jnp.int32) * Q_BLOCK
    key_pos = jnp.arange(s, dtype=jnp.int32)

    def body(xs):
        if cum is None:
            qi, start = xs
        else:
            qi, start, ci = xs
        sc = jnp.einsum('bhqd,bhkd->bhqk', qi, k, preferred_element_type=jnp.float32) * scale
        if cum is not None:
            sc = sc + (ci[..., None] - cum[:, :, None, :])
        q_pos = start + jnp.arange(Q_BLOCK, dtype=jnp.int32)
        sc = jnp.where(key_pos[None, :] <= q_pos[:, None], sc, -jnp.inf)
        p = jax.nn.softmax(sc, axis=-1)
        return jnp.einsum('bhqk,bhkd->bhqd', p.astype(v.dtype), v)

    if cum is None:
        xs = (qb, starts)
    else:
        xs = (qb, starts, jnp.moveaxis(cum.reshape(b, h, nq, Q_BLOCK), 2, 0))
    out = lax.map(body, xs)
    return jnp.moveaxis(out, 0, 2).reshape(b, h, s, v.shape[-1])


def moba_attention(q, k, v):
    b, h, s, d = q.shape
    sp = -(-s // MOBA_BLOCK) * MOBA_BLOCK
    q, k, v = pad_seq(q, sp), pad_seq(k, sp), pad_seq(v, sp)
    nb = sp // MOBA_BLOCK
    scale = d ** -0.5
    kb = k.reshape(b, h, nb, MOBA_BLOCK, d)
    vb = v.reshape(b, h, nb, MOBA_BLOCK, d)
    k_mean = jnp.mean(kb.astype(jnp.float32), axis=3)
    gate = jnp.einsum('bhsd,bhnd->bhsn', q.astype(jnp.float32), k_mean)
    q_blk = jnp.arange(sp, dtype=jnp.int32) // MOBA_BLOCK
    past = jnp.arange(nb, dtype=jnp.int32)[None, :] < q_blk[:, None]
    gate = jnp.where(past, gate, -jnp.inf)
    k_sel = min(MOBA_TOPK, nb)
    _, sel = lax.top_k(gate, k_sel)
    sel_valid = jnp.arange(k_sel, dtype=jnp.int32)[None, :] < q_blk[:, None]

    nc = sp // MOBA_Q_CHUNK
    qc = jnp.moveaxis(q.reshape(b, h, nc, MOBA_Q_CHUNK, d), 2, 0)
    selc = jnp.moveaxis(sel.reshape(b, h, nc, MOBA_Q_CHUNK, k_sel), 2, 0)
    validc = sel_valid.reshape(nc, MOBA_Q_CHUNK, k_sel)
    starts = jnp.arange(nc, dtype=jnp.int32) * MOBA_Q_CHUNK
    b_idx = jnp.arange(b)[:, None, None, None]
    h_idx = jnp.arange(h)[None, :, None, None]
    n_sel = k_sel * MOBA_BLOCK

    def body(xs):
        qi, si, vi, start = xs
        own = start // MOBA_BLOCK
        k_own = lax.dynamic_index_in_dim(kb, own, axis=2, keepdims=False)
        v_own = lax.dynamic_index_in_dim(vb, own, axis=2, keepdims=False)
        q_pos = start + jnp.arange(MOBA_Q_CHUNK, dtype=jnp.int32)
        k_pos = own * MOBA_BLOCK + jnp.arange(MOBA_BLOCK, dtype=jnp.int32)
        s_own = jnp.einsum('bhqd,bhkd->bhqk', qi, k_own, preferred_element_type=jnp.float32) * scale
        s_own = jnp.where(k_pos[None, :] <= q_pos[:, None], s_own, -jnp.inf)
        k_g = kb[b_idx, h_idx, si]
        v_g = vb[b_idx, h_idx, si]
        s_g = jnp.einsum('bhqd,bhqjkd->bhqjk', qi, k_g, preferred_element_type=jnp.float32) * scale
        s_g = jnp.where(vi[None, None, :, :, None], s_g, -jnp.inf)
        s_all = jnp.concatenate([s_g.reshape(b, h, MOBA_Q_CHUNK, n_sel), s_own], axis=-1)
        p = jax.nn.softmax(s_all, axis=-1).astype(v.dtype)
        p_g = p[..., :n_sel].reshape(b, h, MOBA_Q_CHUNK, k_sel, MOBA_BLOCK)
        p_own = p[..., n_sel:]
        return (jnp.einsum('bhqjk,bhqjkd->bhqd', p_g, v_g)
                + jnp.einsum('bhqk,bhkd->bhqd', p_own, v_own))

    out = lax.map(body, (qc, selc, validc, starts))
    return jnp.moveaxis(out, 0, 2).reshape(b, h, sp, d)[:, :, :s]


def dilated_branch(q, k, v, window, dilation):
    b, h, s, d = q.shape
    L = window // dilation
    seg = dilation * L
    sp = -(-s // seg) * seg
    n = sp // dilation
    nb = n // L
    scale = d ** -0.5

    def to_sub(t):
        t = pad_seq(t, sp).reshape(b, h, n, dilation, d).transpose(0, 1, 3, 2, 4)
        return t.reshape(b, h, dilation, nb, L, d)

    def with_prev(t):
        prev = jnp.concatenate([jnp.zeros_like(t[:, :, :, :1]), t[:, :, :, :-1]], axis=3)
        return jnp.concatenate([prev, t], axis=4)

    qs = to_sub(q)
    k2 = with_prev(to_sub(k))
    v2 = with_prev(to_sub(v))
    sc = jnp.einsum('bhrnqd,bhrnkd->bhrnqk', qs, k2, preferred_element_type=jnp.float32) * scale
    i = jnp.arange(L)[:, None]
    m = jnp.arange(2 * L)[None, :]
    diff = L + i - m
    blk = jnp.arange(nb)[:, None, None]
    mask = (diff >= 0) & (diff <= L) & ((m >= L) | (blk > 0))
    sc = jnp.where(mask, sc, -jnp.inf)
    lse = jax.nn.logsumexp(sc, axis=-1)
    p = jnp.exp(sc - lse[..., None]).astype(v.dtype)
    o = jnp.einsum('bhrnqk,bhrnkd->bhrnqd', p, v2)
    o = o.reshape(b, h, dilation, n, d).transpose(0, 1, 3, 2, 4).reshape(b, h, sp, d)[:, :, :s]
    lse = lse.reshape(b, h, dilation, n).transpose(0, 1, 3, 2).reshape(b, h, sp)[:, :, :s]
    return o, lse


def dilated_mixture(q, k, v):
    outs, lses = [], []
    for window, dilation in DILATED_CONFIGS:
        o, l = dilated_branch(q, k, v, window, dilation)
        outs.append(o.astype(jnp.float32))
        lses.append(l)
    wts = jax.nn.softmax(jnp.stack(lses, axis=0), axis=0)
    out = jnp.sum(wts[..., None] * jnp.stack(outs, axis=0), axis=0)
    return out.astype(q.dtype)


def setup_inputs(seed: int = 0) -> dict:
    key = jax.random.key(seed)
    ks = jax.random.split(key, 16)
    f32 = jnp.float32

    def nrm(k, shape, fan_in):
        return jax.random.normal(k, shape, f32) * (fan_in ** -0.5)

    def gain(k, dim):
        return 1.0 + 0.02 * jax.random.normal(k, (DEPTH, dim), f32)

    x = jax.random.normal(ks[0], (BATCH, SEQ, D_MODEL), f32)
    positions = jnp.broadcast_to(jnp.arange(SEQ, dtype=jnp.int32)[None, :], (BATCH, SEQ))
    return {
        "x": x,
        "positions": positions,
        "w_in": nrm(ks[1], (DEPTH, D_MODEL, D_IN), D_MODEL),
        "b_forget": 0.1 * jax.random.normal(ks[2], (DEPTH, N_HEADS_FOX), f32),
        "g_mla_q": gain(ks[3], MLA_Q_RANK),
        "w_mla_q_up": nrm(ks[4], (DEPTH, MLA_Q_RANK, MLA_Q_UP), MLA_Q_RANK),
        "g_mla_kv": gain(ks[5], MLA_KV_RANK),
        "w_mla_kv_up": nrm(ks[6], (DEPTH, MLA_KV_RANK, MLA_KV_UP), MLA_KV_RANK),
        "w_out": nrm(ks[7], (DEPTH, MIX_WIDTH, D_MODEL), MIX_WIDTH),
        "g_pre_mix": gain(ks[8], D_MODEL),
        "g_post_mix": gain(ks[9], D_MODEL),
        "w_gate": nrm(ks[10], (DEPTH, D_MODEL, D_FF), D_MODEL),
        "w_up": nrm(ks[11], (DEPTH, D_MODEL, D_FF), D_MODEL),
        "w_down": nrm(ks[12], (DEPTH, D_FF, D_MODEL), D_FF),
        "g_pre_ffn": gain(ks[13], D_MODEL),
        "g_post_ffn": gain(ks[14], D_MODEL),
    }


def reference(x, positions, w_in, b_forget, g_mla_q, w_mla_q_up, g_mla_kv, w_mla_kv_up, w_out,
              g_pre_mix, g_post_mix, w_gate, w_up, w_down, g_pre_ffn, g_post_ffn):
    cos_h, sin_h = rope_tables(positions, HEAD_DIM)
    cos_r, sin_r = rope_tables(positions, MLA_ROPE_DIM)
    b, s, _ = x.shape
    for l in range(DEPTH):
        h = rmsnorm(x, g_pre_mix[l])
        z = jnp.einsum('bsd,de->bse', h, w_in[l])
        z_moba, z_fox, z_fg, z_cq, z_ckv, z_kr, z_dil = jnp.split(z, SPLIT_POINTS, axis=-1)

        qa, ka, va = [to_heads(t, N_HEADS_MOBA) for t in jnp.split(z_moba, 3, axis=-1)]
        qa, ka = apply_rope(qa, cos_h, sin_h), apply_rope(ka, cos_h, sin_h)
        o_moba = moba_attention(qa, ka, va)

        qb, kb, vb = [to_heads(t, N_HEADS_FOX) for t in jnp.split(z_fox, 3, axis=-1)]
        log_f = jax.nn.log_sigmoid(z_fg.astype(jnp.float32) + b_forget[l].astype(jnp.float32))
        cum = jnp.cumsum(log_f, axis=1).transpose(0, 2, 1)
        o_fox = blocked_causal_attention(qb, kb, vb, HEAD_DIM ** -0.5, cum)

        c_q = rmsnorm(z_cq, g_mla_q[l])
        q_c = to_heads(jnp.einsum('bsr,re->bse', c_q, w_mla_q_up[l]), N_HEADS_MLA)
        q_nope, q_rope = q_c[..., :MLA_NOPE_DIM], q_c[..., MLA_NOPE_DIM:]
        c_kv = rmsnorm(z_ckv, g_mla_kv[l])
        kv = to_heads(jnp.einsum('bsr,re->bse', c_kv, w_mla_kv_up[l]), N_HEADS_MLA)
        k_nope, v_c = kv[..., :MLA_NOPE_DIM], kv[..., MLA_NOPE_DIM:]
        k_rope = apply_rope(z_kr[:, None], cos_r, sin_r)
        q_full = jnp.concatenate([q_nope, apply_rope(q_rope, cos_r, sin_r)], axis=-1)
        k_full = jnp.concatenate(
            [k_nope, jnp.broadcast_to(k_rope, (b, N_HEADS_MLA, s, MLA_ROPE_DIM))], axis=-1)
        o_mla = blocked_causal_attention(q_full, k_full, v_c, (MLA_NOPE_DIM + MLA_ROPE_DIM) ** -0.5)

        qd, kd, vd = [to_heads(t, N_HEADS_DIL) for t in jnp.split(z_dil, 3, axis=-1)]
        qd, kd = apply_rope(qd, cos_h, sin_h), apply_rope(kd, cos_h, sin_h)
        o_dil = dilated_mixture(qd, kd, vd)

        mix = jnp.concatenate([from_heads(o_moba), from_heads(o_fox), from_heads(o_mla),
                               from_heads(o_dil)], axis=-1)
        y = jnp.einsum('bse,ed->bsd', mix, w_out[l])
        x = x + rmsnorm(y, g_post_mix[l])

        h = rmsnorm(x, g_pre_ffn[l])
        f = jax.nn.silu(jnp.einsum('bsd,df->bsf', h, w_gate[l])) * jnp.einsum('bsd,df->bsf', h, w_up[l])
        f = jnp.einsum('bsf,fd->bsd', f, w_down[l])
        x = x + rmsnorm(f, g_post_ffn[l])
    return x
```

```python
from contextlib import ExitStack
import numpy as np
import concourse.bass as bass
import concourse.mybir as mybir
from concourse.bass_utils import run_bass_kernel_spmd

F32 = mybir.dt.float32
BF16 = mybir.dt.bfloat16
I32 = mybir.dt.int32
AF = mybir.ActivationFunctionType
ALU = mybir.AluOpType
AX = mybir.AxisListType

D = 1024
DFF = 2816
NEG = -30000.0
EPS = 1e-6
TWO_PI_HI = 6.28125
TWO_PI_LO = 2.0 * np.pi - 6.28125
DIL_CFG = (1, 4, 16)


class Buf:
    def __init__(self, name, t=None, psum=False, multi=False):
        self.name = name
        self.t = t
        self.psum = psum
        self.multi = multi
        self.w = {}
        self.r = {}
        self.rec = None

    def __getitem__(self, idx):
        return self.t[idx]


class SemRec:
    def __init__(self, sem):
        self.sem = sem
        self.n = 0


class Prog:
    def __init__(self, nc, stack):
        self.nc = nc
        self.stack = stack
        self.engs = {"pe": nc.tensor, "act": nc.scalar, "dve": nc.vector, "pool": nc.gpsimd, "sp": nc.sync}
        self.esem = {}
        self.ecnt = {}
        self.waited = {}
        self.nsem = 0
        for k in self.engs:
            self.esem[k] = self._new_sem("e_" + k)
            self.ecnt[k] = 0
            self.waited[k] = {}
        self.pool = []
        self.recs = []
        self.stage_bufs = []
        self.ninstr = 0

    def _new_sem(self, name):
        s = self.stack.enter_context(self.nc.semaphore(name))
        self.nsem += 1
        return s

    def take_rec(self):
        if self.pool:
            return self.pool.pop()
        r = SemRec(self._new_sem("d%d" % len(self.recs)))
        self.recs.append(r)
        return r

    def sbuf(self, st, name, shape, dtype):
        self.uid = getattr(self, "uid", 0) + 1
        name = "%s_%d" % (name, self.uid)
        t = st.enter_context(self.nc.sbuf_tensor(name, list(shape), dtype))
        b = Buf(name, t)
        self.stage_bufs.append(b)
        return b

    def psum(self, st, name):
        self.uid = getattr(self, "uid", 0) + 1
        name = "%s_%d" % (name, self.uid)
        t = st.enter_context(self.nc.psum_tensor(name, [128, 512], F32))
        b = Buf(name, t, psum=True)
        self.stage_bufs.append(b)
        return b

    def dram(self, name, shape, dtype, kind="Internal"):
        t = self.nc.dram_tensor(name, list(shape), dtype, kind=kind)
        return Buf(name, t.ap(), multi=True)

    def _wait(self, e, deps):
        eng = self.engs[e]
        w = self.waited[e]
        best = {}
        for (sem, val) in deps:
            k = id(sem)
            if k not in best or best[k][1] < val:
                best[k] = (sem, val)
        for k, (sem, val) in best.items():
            if sem is self.esem[e] and e == "pe":
                continue
            if w.get(k, 0) >= val:
                continue
            eng.wait_ge(sem, val)
            w[k] = val

    @staticmethod
    def _merge(d, tok):
        k = id(tok[0])
        if k not in d or d[k][1] < tok[1]:
            d[k] = tok

    def _deps(self, e, reads, writes):
        deps = []
        for b in reads:
            deps += list(b.w.values())
            if b.psum:
                deps += [t for t in b.r.values() if t[0] is not self.esem[e]]
        for b in writes:
            if b.multi:
                continue
            deps += list(b.w.values())
            deps += list(b.r.values())
        return deps

    def _post(self, tok, reads, writes):
        for b in reads:
            self._merge(b.r, tok)
        for b in writes:
            if b.multi:
                self._merge(b.w, tok)
            else:
                b.w = {id(tok[0]): tok}
                b.r = {}

    def op(self, e, fn, reads=(), writes=(), inc=True):
        self._wait(e, self._deps(e, reads, writes))
        ins = fn(self.engs[e])
        self.ninstr += 1
        if inc:
            self.ecnt[e] += 1
            ins.then_inc(self.esem[e], 1)
            tok = (self.esem[e], self.ecnt[e])
        else:
            tok = (self.esem[e], self.ecnt[e] + 1)
        self._post(tok, reads, writes)
        return tok

    def dma(self, q, out_ap, in_ap, reads, writes, slot, **kw):
        if slot.rec is None:
            slot.rec = self.take_rec()
        self._wait(q, self._deps(q, reads, writes))
        ins = self.engs[q].dma_start(out=out_ap, in_=in_ap, **kw)
        self.ninstr += 1
        slot.rec.n += 1
        ins.then_inc(slot.rec.sem, 16)
        tok = (slot.rec.sem, 16 * slot.rec.n)
        self._post(tok, reads, writes)
        return tok

    def barrier(self):
        toks = []
        for k in self.engs:
            if self.ecnt[k] > 0:
                toks.append((self.esem[k], self.ecnt[k]))
        for r in self.recs:
            if r.n > 0:
                toks.append((r.sem, 16 * r.n))
        for e in self.engs:
            self._wait(e, toks)

    def end_stage(self):
        self.barrier()
        for b in self.stage_bufs:
            if b.rec is not None:
                self.pool.append(b.rec)
                b.rec = None
        self.stage_bufs = []


def w_in_offsets():
    o = {}
    o["moba_q"], o["moba_k"], o["moba_v"] = 0, 256, 512
    o["fox_q"], o["fox_k"], o["fox_v"] = 768, 1024, 1280
    o["fg"] = 1536
    o["cq"] = 1540
    o["ckv"] = 1732
    o["kr"] = 1860
    o["dil_q"], o["dil_k"], o["dil_v"] = 1892, 2148, 2404
    return o


def make_groups(heads):
    o = w_in_offsets()
    nh = len(heads)
    perm64 = np.concatenate([np.arange(32, 64), np.arange(0, 32)])
    perm32 = np.concatenate([np.arange(16, 32), np.arange(0, 16)])
    groups = []
    for m in ("moba", "dil"):
        for t in ("q", "k"):
            for hp in range(nh // 2):
                ha, hb = heads[2 * hp], heads[2 * hp + 1]
                base = o["%s_%s" % (m, t)]
                ca = np.concatenate([base + ha * 64 + np.arange(64), base + hb * 64 + np.arange(64)])
                cb = np.concatenate([base + ha * 64 + perm64, base + hb * 64 + perm64])
                groups.append(("%s_%s_%d_A" % (m, t, hp), ca))
                groups.append(("%s_%s_%d_B" % (m, t, hp), cb))
    for i, h in enumerate(heads):
        c = np.concatenate([o["fox_q"] + h * 64 + np.arange(64), [o["fg"] + h, o["fg"] + h]])
        groups.append(("fox_q_%d" % i, c))
    for hp in range(nh // 2):
        ha, hb = heads[2 * hp], heads[2 * hp + 1]
        c = np.concatenate([o["fox_k"] + ha * 64 + np.arange(64), o["fox_k"] + hb * 64 + np.arange(64)])
        groups.append(("fox_k_%d" % hp, c))
    groups.append(("cq0", o["cq"] + np.arange(128)))
    groups.append(("cq1_krA", np.concatenate([o["cq"] + 128 + np.arange(64), o["kr"] + np.arange(32)])))
    groups.append(("cq1_krB", np.concatenate([o["cq"] + 128 + np.arange(64), o["kr"] + perm32])))
    groups.append(("ckv", o["ckv"] + np.arange(128)))
    vcols = []
    for m in ("moba", "fox", "dil"):
        for h in heads:
            vcols.append(o["%s_v" % m] + h * 64 + np.arange(64))
    groups.append(("V", np.concatenate(vcols)))
    return groups


def make_consts(S):
    c = {}
    c["c_ident"] = np.eye(128, dtype=np.float32)
    kl = np.arange(128)[:, None]
    ql = np.arange(128)[None, :]
    c["c_tri"] = np.where(kl <= ql, 0.0, NEG).astype(np.float32)
    c["c_triu"] = np.where(kl >= ql, 0.0, NEG).astype(np.float32)
    c["c_negall"] = np.full((128, 128), NEG, np.float32)
    oh = np.zeros((32, S), np.float32)
    for j in range(S // 256):
        oh[j, j * 256:(j + 1) * 256] = 1.0
    c["c_onehot"] = oh
    p = np.arange(128)
    invf64 = (10000.0 ** (-np.arange(0, 64, 2, dtype=np.float32) / 64)).astype(np.float32)
    invf32 = (10000.0 ** (-np.arange(0, 32, 2, dtype=np.float32) / 32)).astype(np.float32)
    cols = np.zeros((128, 8), np.float32)
    cols[:, 0] = invf64[p % 32]
    cols[:, 1] = invf32[p % 16]
    cols[:, 2] = np.where((p % 64) < 32, -1.0, 1.0)
    cols[:, 3] = np.where((p % 32) < 16, -1.0, 1.0)
    cols[:, 4] = -(p % 2).astype(np.float32)
    cols[:, 5] = EPS
    cols[:, 6] = np.pi / 2
    cols[:, 7] = 1.0
    c["c_cols"] = cols
    return c


class Ctx:
    pass


def build_program(S, depth, NH, HS=1, dbg=None, ncores=8):
    assert S % 2048 == 0
    nc = bass.Bass("TRN2", target_bir_lowering=False)
    heads = list(range(NH))
    groups = make_groups(heads)
    goff = {}
    off = 0
    for name, cols in groups:
        goff[name] = (off, len(cols))
        off += len(cols)
    NA = off
    NT = S // 128
    NQ = S // 512
    NB = S // 256
    SO = S // HS
    NTO = SO // 128
    NQO = SO // 512
    NM = NH * 64
    PAIRS = [[2 * i, 2 * i + 1] for i in range(ncores // 2)]
    st = ExitStack()
    with st:
        P = Prog(nc, st)
        C = Ctx()
        x_in = P.dram("x", [S, D], F32, kind="ExternalInput")
        x_own = P.dram("x_own", [SO, D], F32, kind="ExternalInput") if HS > 1 else x_in
        sel_in = P.dram("sel", [128, 2], F32, kind="ExternalInput")
        pos_in = P.dram("pos", [1, S], I32, kind="ExternalInput")
        wA = P.dram("wA", [depth, D, NA], F32, kind="ExternalInput")
        wqu = P.dram("wqu", [depth, 192, NH * 192], F32, kind="ExternalInput")
        wkvu = P.dram("wkvu", [depth, 128, NH * 128], F32, kind="ExternalInput")
        wout = P.dram("wout", [depth, D, D], F32, kind="ExternalInput")
        wg = P.dram("wg", [depth, D, DFF], F32, kind="ExternalInput")
        wu = P.dram("wu", [depth, D, DFF], F32, kind="ExternalInput")
        wd = P.dram("wd", [depth, DFF, D], F32, kind="ExternalInput")
        gv = P.dram("gv", [depth, 4, D], F32, kind="ExternalInput")
        gq = P.dram("gq", [depth, 192, 1], F32, kind="ExternalInput")
        gkv = P.dram("gkv", [depth, 128, 1], F32, kind="ExternalInput")
        bfg = P.dram("bfg", [depth, NH, 1], F32, kind="ExternalInput")
        c_ident = P.dram("c_ident", [128, 128], F32, kind="ExternalInput")
        c_tri = P.dram("c_tri", [128, 128], F32, kind="ExternalInput")
        c_triu = P.dram("c_triu", [128, 128], F32, kind="ExternalInput")
        c_negall = P.dram("c_negall", [128, 128], F32, kind="ExternalInput")
        c_onehot = P.dram("c_onehot", [32, S], F32, kind="ExternalInput")
        c_cols = P.dram("c_cols", [128, 8], F32, kind="ExternalInput")
        out_d = P.dram("out", [SO, D], F32, kind="ExternalOutput")
        hTf = P.dram("hTf", [4 * HS * 256, SO], BF16)
        hTown = P.dram("hTown", [4 * 256, SO], BF16) if HS > 1 else hTf
        xs = P.dram("xs", [SO, D], F32)
        tabs = P.dram("tabs", [4, 128, S], F32)
        QT = {m: P.dram("QT_" + m, [NH, 96 if m == "mla" else 64, S], BF16) for m in ("moba", "fox", "mla", "dil")}
        KT = {m: P.dram("KT_" + m, [NH, 96 if m == "mla" else 64, S], BF16) for m in ("moba", "fox", "mla", "dil")}
        VV = {m: P.dram("V_" + m, [S, NH, 65], BF16) for m in ("moba", "fox", "mla", "dil")}
        FG = P.dram("FG", [NH, 2, S], F32)
        mixX = [P.dram("mixX%d" % j, [4 * NM, SO], BF16) for j in range(HS)]
        mixG = [P.dram("mixG%d" % j, [HS * 4 * NM, SO], BF16) for j in range(HS)] if HS > 1 else mixX
        h2T = P.dram("h2T", [128, 8, SO], BF16)
        dbg_out = None
        if dbg:
            dbg_out = P.dram("dbg", [D, SO], F32, kind="ExternalOutput")

        ident = P.sbuf(st, "ident", [128, 128], BF16)
        tri = P.sbuf(st, "tri", [128, 128], BF16)
        triu = P.sbuf(st, "triu", [128, 128], BF16)
        negall = P.sbuf(st, "negall", [128, 128], BF16)
        cols = P.sbuf(st, "cols", [128, 8], F32)
        ones_f = P.sbuf(st, "ones_f", [128, 128], F32)
        for sb, dr in ((ident, c_ident), (tri, c_tri), (triu, c_triu), (negall, c_negall)):
            P.dma("pool", sb[:, :], dr[:, :], reads=[dr], writes=[sb], slot=sb)
        P.dma("sp", cols[:, :], c_cols[:, :], reads=[c_cols], writes=[cols], slot=cols)
        P.op("dve", lambda e: e.memset(ones_f[:, :], 1.0), writes=[ones_f])
        selc = P.sbuf(st, "selc", [128, 2], F32)
        P.dma("sp", selc[:, :], sel_in[:, :], reads=[sel_in], writes=[selc], slot=selc)

        def hT_tile(n):
            r, ln = n // NQO, n % NQO
            v = hTf[:, ln * 512:(ln + 1) * 512].rearrange("(c r kk p) t -> r p c kk t", c=4, r=HS, kk=2)
            return v[r]

        def collective(src, dst, src_ap, dst_ap):
            sem = P._new_sem("cc%d" % P.nsem)
            P._wait("pool", P._deps("pool", [src], []))
            ins = nc.gpsimd.collective_compute("AllGather", ALU.bypass, replica_groups=PAIRS, ins=[src_ap.opt()], outs=[dst_ap.opt()])
            ins.then_inc(sem)
            P.ninstr += 1
            tok = (sem, 1)
            P._post(tok, [src], [dst])
        EPSC = lambda n=128, b=0: cols[b:b + n, 5:6]

        def stage_tables():
            with ExitStack() as s2:
                posi = P.sbuf(s2, "posi", [128, 512], I32)
                posf = P.sbuf(s2, "posf", [128, 512], F32)
                ang = P.sbuf(s2, "ang", [128, 512], F32)
                kf = P.sbuf(s2, "kf", [128, 512], F32)
                ki = P.sbuf(s2, "ki", [128, 512], I32)
                m1 = P.sbuf(s2, "m1", [128, 512], F32)
                res = [P.sbuf(s2, "tres%d" % i, [128, 512], F32) for i in range(2)]
                for n in range(NQ):
                    sl = slice(n * 512, (n + 1) * 512)
                    P.dma("sp", posi[:, :], pos_in[0:1, sl].to_broadcast([128, 512]), reads=[pos_in], writes=[posi], slot=posi)
                    P.op("dve", lambda e: e.tensor_copy(out=posf[:, :], in_=posi[:, :]), reads=[posi], writes=[posf])
                    for ti in range(2):
                        P.op("dve", lambda e: e.tensor_scalar(out=ang[:, :], in0=posf[:, :], scalar1=cols[:, ti:ti + 1], scalar2=None, op0=ALU.mult),
                             reads=[posf, cols], writes=[ang])
                        P.op("dve", lambda e: e.tensor_scalar(out=kf[:, :], in0=ang[:, :], scalar1=float(1.0 / (2 * np.pi)), scalar2=None, op0=ALU.mult),
                             reads=[ang], writes=[kf])
                        P.op("dve", lambda e: e.tensor_copy(out=ki[:, :], in_=kf[:, :]), reads=[kf], writes=[ki])
                        P.op("dve", lambda e: e.tensor_copy(out=kf[:, :], in_=ki[:, :]), reads=[ki], writes=[kf])
                        P.op("dve", lambda e: e.scalar_tensor_tensor(out=ang[:, :], in0=kf[:, :], scalar=-TWO_PI_HI, in1=ang[:, :], op0=ALU.mult, op1=ALU.add),
                             reads=[kf, ang], writes=[ang])
                        P.op("dve", lambda e: e.scalar_tensor_tensor(out=ang[:, :], in0=kf[:, :], scalar=-TWO_PI_LO, in1=ang[:, :], op0=ALU.mult, op1=ALU.add),
                             reads=[kf, ang], writes=[ang])
                        P.op("dve", lambda e: e.tensor_scalar(out=ang[:, :], in0=ang[:, :], scalar1=float(np.pi), scalar2=float(-np.pi), op0=ALU.min, op1=ALU.max),
                             reads=[ang], writes=[ang])
                        rs = res[0]
                        P.op("act", lambda e: e.activation(out=m1[:, :], in_=ang[:, :], func=AF.Sin), reads=[ang], writes=[m1])
                        P.op("dve", lambda e: e.tensor_scalar(out=rs[:, :], in0=m1[:, :], scalar1=cols[:, 2 + ti:3 + ti], scalar2=None, op0=ALU.mult),
                             reads=[m1, cols], writes=[rs])
                        P.dma("sp", tabs[2 * ti + 1, :, sl], rs[:, :], reads=[rs], writes=[tabs], slot=rs)
                        rc = res[1]
                        P.op("dve", lambda e: e.tensor_scalar(out=m1[:, :], in0=ang[:, :], scalar1=-1.0, scalar2=None, op0=ALU.mult),
                             reads=[ang], writes=[m1])
                        P.op("dve", lambda e: e.tensor_tensor(out=m1[:, :], in0=m1[:, :], in1=ang[:, :], op=ALU.max),
                             reads=[ang, m1], writes=[m1])
                        P.op("act", lambda e: e.activation(out=rc[:, :], in_=m1[:, :], func=AF.Sin, scale=-1.0, bias=cols[:, 6:7]),
                             reads=[m1, cols], writes=[rc])
                        P.dma("sp", tabs[2 * ti, :, sl], rc[:, :], reads=[rc], writes=[tabs], slot=rc)
            P.end_stage()

        def rstd_from_ss(ss, n_feat, rstd, npart=128):
            P.op("act", lambda e: e.activation(out=rstd[0:npart, 0:1], in_=ss[0:npart, 0:1], func=AF.Ln, scale=1.0 / n_feat, bias=EPSC(npart)),
                 reads=[ss, cols], writes=[rstd])
            P.op("act", lambda e: e.activation(out=rstd[0:npart, 0:1], in_=rstd[0:npart, 0:1], func=AF.Exp, scale=-0.5),
                 reads=[rstd], writes=[rstd])

        def transposes_to(hb, dst, dst_sl, pst):
            for half in range(2):
                ps = pst[half]
                for k4 in range(4):
                    k = half * 4 + k4
                    P.op("pe", lambda e: e.matmul(ps[:, k4 * 128:(k4 + 1) * 128], lhsT=hb[:, k * 128:(k + 1) * 128], rhs=ident[:, :], start=True, stop=True),
                         reads=[hb, ident], writes=[ps], inc=(k4 == 3))
                eng = "act" if half == 0 else "dve"
                if eng == "act":
                    P.op("act", lambda e: e.copy(out=dst[:, half * 4:half * 4 + 4, dst_sl], in_=ps[:, :].rearrange("p (k t) -> p k t", k=4)),
                         reads=[ps], writes=[dst])
                else:
                    P.op("dve", lambda e: e.tensor_copy(out=dst[:, half * 4:half * 4 + 4, dst_sl], in_=ps[:, :].rearrange("p (k t) -> p k t", k=4)),
                         reads=[ps], writes=[dst])

        def load_gvec(s2, name, l, idx):
            g = P.sbuf(s2, name, [128, D], F32)
            P.dma("sp", g[:, :], gv[l, idx:idx + 1, :].to_broadcast([128, D]), reads=[gv], writes=[g], slot=g)
            return g

        def stage_h0():
            with ExitStack() as s2:
                g0 = load_gvec(s2, "g0", 0, 0)
                xt = [P.sbuf(s2, "h0x%d" % i, [128, D], F32) for i in range(2)]
                junk = P.sbuf(s2, "h0junk", [128, D], BF16)
                ss = P.sbuf(s2, "h0ss", [128, 1], F32)
                rstd = P.sbuf(s2, "h0rstd", [128, 1], F32)
                hb = [P.sbuf(s2, "h0hb%d" % i, [128, D], BF16) for i in range(2)]
                ho = [P.sbuf(s2, "h0ho%d" % i, [128, 8, 512], BF16) for i in range(2)]
                pst = [P.psum(s2, "h0ps%d" % i) for i in range(4)]
                for t in range(NT):
                    x_t = xt[t % 2]
                    P.dma("sp", x_t[:, :], x_in[t * 128:(t + 1) * 128, :], reads=[x_in], writes=[x_t], slot=x_t)
                    if HS == 1:
                        P.dma("sp", xs[t * 128:(t + 1) * 128, :], x_t[:, :], reads=[x_t], writes=[xs], slot=x_t)
                    P.op("act", lambda e: e.activation(out=junk[:, :], in_=x_t[:, :], func=AF.Square, accum_out=ss[:, 0:1]),
                         reads=[x_t], writes=[junk, ss])
                    rstd_from_ss(ss, D, rstd)
                    h_b = hb[t % 2]
                    P.op("dve", lambda e: e.scalar_tensor_tensor(out=h_b[:, :], in0=x_t[:, :], scalar=rstd[:, 0:1], in1=g0[:, :], op0=ALU.mult, op1=ALU.mult),
                         reads=[x_t, rstd, g0], writes=[h_b])
                    o = ho[(t // 4) % 2]
                    transposes_to(h_b, o, slice((t % 4) * 128, (t % 4 + 1) * 128), pst[2 * (t % 2):2 * (t % 2) + 2])
                    if t % 4 == 3:
                        n = t // 4
                        [P.dma("sp", hT_tile(n)[:, c_, :, :], o[:, 2 * c_:2 * c_ + 2, :], reads=[o], writes=[hTf], slot=o) for c_ in range(4)]
                if HS > 1:
                    for c in range(0, SO, 1024):
                        P.dma("sp", xs[c:c + 1024, :], x_own[c:c + 1024, :], reads=[x_own], writes=[xs], slot=ss)
            P.end_stage()

        def stage_a(l):
            with ExitStack() as s2:
                wa = P.sbuf(s2, "wa", [128, 8, NA], BF16)
                for k in range(8):
                    P.dma("pool", wa[:, k, :], wA[l, k * 128:(k + 1) * 128, :], reads=[wA], writes=[wa], slot=wa)
                wq = P.sbuf(s2, "wq", [128, 2, NH * 192], BF16)
                P.dma("pool", wq[:, 0, :], wqu[l, 0:128, :], reads=[wqu], writes=[wq], slot=wq)
                P.dma("pool", wq[0:64, 1, :], wqu[l, 128:192, :], reads=[wqu], writes=[wq], slot=wq)
                wk = P.sbuf(s2, "wk", [128, NH * 128], BF16)
                P.dma("pool", wk[:, :], wkvu[l, :, :], reads=[wkvu], writes=[wk], slot=wk)
                gqc = P.sbuf(s2, "gqc", [128, 2], F32)
                P.dma("sp", gqc[:, 0:1], gq[l, 0:128, :], reads=[gq], writes=[gqc], slot=gqc)
                P.dma("sp", gqc[0:64, 1:2], gq[l, 128:192, :], reads=[gq], writes=[gqc], slot=gqc)
                gkc = P.sbuf(s2, "gkc", [128, 1], F32)
                P.dma("sp", gkc[:, :], gkv[l, :, :], reads=[gkv], writes=[gkc], slot=gkc)
                ht = [P.sbuf(s2, "a_ht%d" % i, [128, 8, 512], BF16) for i in range(2)]
                tb = [P.sbuf(s2, "a_tb%d" % i, [128, 4, 512], F32) for i in range(2)]
                ps = [P.psum(s2, "a_ps%d" % i) for i in range(8)]
                NO = 6
                ob = [P.sbuf(s2, "a_ob%d" % i, [128, 512], BF16) for i in range(NO)]
                t1 = [P.sbuf(s2, "a_t1%d" % i, [128, 512], F32) for i in range(2)]
                t2 = [P.sbuf(s2, "a_t2%d" % i, [128, 512], F32) for i in range(2)]
                fgb = [P.sbuf(s2, "a_fg%d" % i, [128, 512], F32) for i in range(2)]
                sq = [P.sbuf(s2, "a_sq%d" % i, [128, 512], F32) for i in range(3)]
                rs = [P.sbuf(s2, "a_rs%d" % i, [128, 512], F32) for i in range(2)]
                cn = [P.sbuf(s2, "a_cn%d" % i, [128, 512], BF16) for i in range(3)]
                vb = {m: [P.sbuf(s2, "a_v%s%d" % (m, i), [128, 4, NH * 65], BF16) for i in range(2)] for m in ("moba", "fox", "dil", "mla")}
                for m in vb:
                    for b_ in vb[m]:
                        P.op("pool", lambda e: e.memset(b_[:, :, :], 1.0), writes=[b_])
                state = {"ob": 0, "ps": 0, "t": 0}

                def next_ps():
                    p_ = ps[state["ps"] % 8]
                    state["ps"] += 1
                    return p_

                def next_ob():
                    o_ = ob[state["ob"] % NO]
                    state["ob"] += 1
                    return o_

                def load_tile(n):
                    h_ = ht[n % 2]
                    [P.dma("sp", h_[:, 2 * c_:2 * c_ + 2, :], hT_tile(n)[:, c_, :, :], reads=[hTf], writes=[h_], slot=h_) for c_ in range(4)]
                    t_ = tb[n % 2]
                    P.dma("sp", t_[:, :, :], tabs[:, :, n * 512:(n + 1) * 512].rearrange("f p t -> p f t"), reads=[tabs], writes=[t_], slot=t_)

                def proj(gname, h_, rows=None):
                    o_, m_ = goff[gname]
                    p_ = next_ps()
                    for k in range(8):
                        P.op("pe", lambda e: e.matmul(p_[0:m_, :], lhsT=wa[:, k, o_:o_ + m_], rhs=h_[:, k, :], start=(k == 0), stop=(k == 7)),
                             reads=[wa, h_], writes=[p_], inc=(k == 7))
                    return p_

                def rope_evac(pa, pb, t_, ti, dst, r0, r1):
                    a1 = t1[state["t"] % 2]
                    a2 = t2[state["t"] % 2]
                    state["t"] += 1
                    P.op("dve", lambda e: e.tensor_tensor(out=a1[r0:r1, :], in0=pa[r0:r1, :], in1=t_[r0:r1, 2 * ti, :], op=ALU.mult),
                         reads=[pa, t_], writes=[a1])
                    P.op("dve", lambda e: e.tensor_tensor(out=a2[r0:r1, :], in0=pb[r0:r1, :], in1=t_[r0:r1, 2 * ti + 1, :], op=ALU.mult),
                         reads=[pb, t_], writes=[a2])
                    P.op("pool", lambda e: e.tensor_tensor(out=dst[r0:r1, :], in0=a1[r0:r1, :], in1=a2[r0:r1, :], op=ALU.add),
                         reads=[a1, a2], writes=[dst])

                def store_pair(o_, dt_, hp, sl, rows=64):
                    dst = dt_[2 * hp:2 * hp + 2, 0:rows, sl].rearrange("h r t -> (h r) t")
                    if rows == 64:
                        P.dma("sp", dst, o_[:, :], reads=[o_], writes=[dt_], slot=o_)
                    else:
                        raise NotImplementedError

                load_tile(0)
                for n in range(NQ):
                    if n + 1 < NQ:
                        load_tile(n + 1)
                    h_ = ht[n % 2]
                    t_ = tb[n % 2]
                    sl = slice(n * 512, (n + 1) * 512)
                    for m in ("moba", "dil"):
                        for tname, dt_ in (("q", QT[m]), ("k", KT[m])):
                            for hp in range(NH // 2):
                                pa = proj("%s_%s_%d_A" % (m, tname, hp), h_)
                                pb = proj("%s_%s_%d_B" % (m, tname, hp), h_)
                                o_ = next_ob()
                                rope_evac(pa, pb, t_, 0, o_, 0, 128)
                                store_pair(o_, dt_, hp, sl)
                    for i in range(NH):
                        pq = proj("fox_q_%d" % i, h_)
                        o_ = next_ob()
                        P.op("act", lambda e: e.copy(out=o_[0:64, :], in_=pq[0:64, :]), reads=[pq], writes=[o_])
                        P.dma("sp", QT["fox"][i, 0:64, sl], o_[0:64, :], reads=[o_], writes=[QT["fox"]], slot=o_)
                        f_ = fgb[i % 2]
                        P.op("dve", lambda e: e.tensor_copy(out=f_[64:66, :], in_=pq[64:66, :]), reads=[pq], writes=[f_])
                        P.dma("sp", FG[i, :, sl], f_[64:66, :], reads=[f_], writes=[FG], slot=f_)
                    for hp in range(NH // 2):
                        pk = proj("fox_k_%d" % hp, h_)
                        o_ = next_ob()
                        P.op("act", lambda e: e.copy(out=o_[:, :], in_=pk[:, :]), reads=[pk], writes=[o_])
                        store_pair(o_, KT["fox"], hp, sl)
                    vo, vm = goff["V"]
                    for sub in range(4):
                        for ci, cw in enumerate((0, 1)):
                            w0 = vo + ci * (vm // 2)
                            wn = vm // 2
                            p_ = next_ps()
                            for k in range(8):
                                P.op("pe", lambda e: e.matmul(p_[:, 0:wn], lhsT=h_[:, k, sub * 128:(sub + 1) * 128], rhs=wa[:, k, w0:w0 + wn], start=(k == 0), stop=(k == 7)),
                                     reads=[wa, h_], writes=[p_], inc=(k == 7))
                            c0 = ci * wn
                            done = 0
                            while done < wn:
                                gcol = c0 + done
                                mi = gcol // (NH * 64)
                                m = ("moba", "fox", "dil")[mi]
                                inm = gcol - mi * NH * 64
                                take = min(wn - done, NH * 64 - inm)
                                hh0 = inm // 64
                                nhh = take // 64
                                vt = vb[m][n % 2]
                                eng = "act" if (sub + ci) % 2 == 0 else "dve"
                                src = p_[:, done:done + take].rearrange("p (h c) -> p h c", c=64)
                                dstv = vt[:, sub, :].rearrange("p (h c) -> p h c", c=65)[:, hh0:hh0 + nhh, 0:64]
                                if eng == "act":
                                    P.op("act", lambda e: e.copy(out=dstv, in_=src), reads=[p_], writes=[vt])
                                else:
                                    P.op("dve", lambda e: e.tensor_copy(out=dstv, in_=src), reads=[p_], writes=[vt])
                                done += take
                    for m in ("moba", "fox", "dil"):
                        vt = vb[m][n % 2]
                        P.dma("sp", VV[m][sl, :, :].rearrange("(s p) h c -> p s (h c)", p=128), vt[:, :, :], reads=[vt], writes=[VV[m]], slot=vt)
                    pcq0 = proj("cq0", h_)
                    pcq1 = proj("cq1_krA", h_)
                    pkb = proj("cq1_krB", h_)
                    pckv = proj("ckv", h_)
                    okr = next_ob()
                    rope_evac(pcq1, pkb, t_, 1, okr, 64, 96)
                    for i in range(NH):
                        P.dma("sp", KT["mla"][i, 64:96, sl], okr[64:96, :], reads=[okr], writes=[KT["mla"]], slot=okr)
                    P.op("act", lambda e: e.activation(out=sq[0][:, :], in_=pcq0[:, :], func=AF.Square), reads=[pcq0], writes=[sq[0]])
                    P.op("act", lambda e: e.activation(out=sq[1][0:64, :], in_=pcq1[0:64, :], func=AF.Square), reads=[pcq1], writes=[sq[1]])
                    P.op("act", lambda e: e.activation(out=sq[2][:, :], in_=pckv[:, :], func=AF.Square), reads=[pckv], writes=[sq[2]])
                    pss = next_ps()
                    P.op("pe", lambda e: e.matmul(pss[:, :], lhsT=ones_f[:, :], rhs=sq[0][:, :], start=True, stop=False), reads=[ones_f, sq[0]], writes=[pss], inc=False)
                    P.op("pe", lambda e: e.matmul(pss[:, :], lhsT=ones_f[0:64, :], rhs=sq[1][0:64, :], start=False, stop=True), reads=[ones_f, sq[1]], writes=[pss])
                    pss2 = next_ps()
                    P.op("pe", lambda e: e.matmul(pss2[:, :], lhsT=ones_f[:, :], rhs=sq[2][:, :], start=True, stop=True), reads=[ones_f, sq[2]], writes=[pss2])
                    for (pp, nf, r_) in ((pss, 192, rs[0]), (pss2, 128, rs[1])):
                        P.op("act", lambda e: e.activation(out=r_[:, :], in_=pp[:, :], func=AF.Ln, scale=1.0 / nf, bias=EPSC()), reads=[pp, cols], writes=[r_])
                        P.op("act", lambda e: e.activation(out=r_[:, :], in_=r_[:, :], func=AF.Exp, scale=-0.5), reads=[r_], writes=[r_])
                    P.op("dve", lambda e: e.scalar_tensor_tensor(out=cn[0][:, :], in0=pcq0[:, :], scalar=gqc[:, 0:1], in1=rs[0][:, :], op0=ALU.mult, op1=ALU.mult),
                         reads=[pcq0, gqc, rs[0]], writes=[cn[0]])
                    P.op("dve", lambda e: e.scalar_tensor_tensor(out=cn[1][0:64, :], in0=pcq1[0:64, :], scalar=gqc[0:64, 1:2], in1=rs[0][0:64, :], op0=ALU.mult, op1=ALU.mult),
                         reads=[pcq1, gqc, rs[0]], writes=[cn[1]])
                    P.op("dve", lambda e: e.scalar_tensor_tensor(out=cn[2][:, :], in0=pckv[:, :], scalar=gkc[:, 0:1], in1=rs[1][:, :], op0=ALU.mult, op1=ALU.mult),
                         reads=[pckv, gkc, rs[1]], writes=[cn[2]])
                    for i in range(NH):
                        pa = next_ps()
                        pb = next_ps()
                        for (pp, c0) in ((pa, i * 192), (pb, i * 192 + 96)):
                            P.op("pe", lambda e: e.matmul(pp[0:96, :], lhsT=wq[:, 0, c0:c0 + 96], rhs=cn[0][:, :], start=True, stop=False), reads=[wq, cn[0]], writes=[pp], inc=False)
                            P.op("pe", lambda e: e.matmul(pp[0:96, :], lhsT=wq[0:64, 1, c0:c0 + 96], rhs=cn[1][0:64, :], start=False, stop=True), reads=[wq, cn[1]], writes=[pp])
                        o_ = next_ob()
                        P.op("act", lambda e: e.copy(out=o_[0:64, :], in_=pa[0:64, :]), reads=[pa], writes=[o_])
                        rope_evac(pa, pb, t_, 1, o_, 64, 96)
                        P.dma("sp", QT["mla"][i, 0:96, sl], o_[0:96, :], reads=[o_], writes=[QT["mla"]], slot=o_)
                        pkn = next_ps()
                        P.op("pe", lambda e: e.matmul(pkn[0:64, :], lhsT=wk[:, i * 128:i * 128 + 64], rhs=cn[2][:, :], start=True, stop=True), reads=[wk, cn[2]], writes=[pkn])
                        o2 = next_ob()
                        P.op("act", lambda e: e.copy(out=o2[0:64, :], in_=pkn[0:64, :]), reads=[pkn], writes=[o2])
                        P.dma("sp", KT["mla"][i, 0:64, sl], o2[0:64, :], reads=[o2], writes=[KT["mla"]], slot=o2)
                    vt = vb["mla"][n % 2]
                    for sub in range(4):
                        pv = next_ps()
                        P.op("pe", lambda e: e.matmul(pv[:, 0:NH * 128], lhsT=cn[2][:, sub * 128:(sub + 1) * 128], rhs=wk[:, :], start=True, stop=True), reads=[wk, cn[2]], writes=[pv])
                        src = pv[:, 0:NH * 128].rearrange("p (h c) -> p h c", c=128)[:, :, 64:128]
                        dstv = vt[:, sub, :].rearrange("p (h c) -> p h c", c=65)[:, :, 0:64]
                        P.op("dve", lambda e: e.tensor_copy(out=dstv, in_=src), reads=[pv], writes=[vt])
                    P.dma("sp", VV["mla"][sl, :, :].rearrange("(s p) h c -> p s (h c)", p=128), vt[:, :, :], reads=[vt], writes=[VV["mla"]], slot=vt)
            P.end_stage()

        def finalize_rows(s2res, src_ps_or_sb, is_psum, ncols, dst_rows, tsl, res):
            osb, rec, bcp, outb = res
            if is_psum:
                P.op("dve", lambda e: e.tensor_copy(out=osb[0:65, 0:ncols], in_=src_ps_or_sb[0:65, 0:ncols]), reads=[src_ps_or_sb], writes=[osb])
                srcb = osb
            else:
                srcb = src_ps_or_sb
            P.op("dve", lambda e: e.reciprocal(out=rec[64:65, 0:ncols], in_=srcb[64:65, 0:ncols]), reads=[srcb], writes=[rec])
            return srcb

        def finalize_part2(srcb, ncols, dst_rows, tsl, res, src_off=0):
            osb, rec, bcp, outb = res
            P.op("pe", lambda e: e.matmul(bcp[0:64, 0:ncols], lhsT=ones_f[64:65, 0:64], rhs=rec[64:65, 0:ncols], start=True, stop=True),
                 reads=[ones_f, rec], writes=[bcp])
            P.op("dve", lambda e: e.tensor_tensor(out=outb[0:64, 0:ncols], in0=srcb[0:64, src_off:src_off + ncols], in1=bcp[0:64, 0:ncols], op=ALU.mult),
                 reads=[srcb, bcp], writes=[outb])
            qi_ = tsl.start // 512
            mx = mixX[qi_ // NQO]
            lt = (qi_ % NQO) * 512
            P.dma("sp", mx[dst_rows, lt:lt + ncols], outb[0:64, 0:ncols], reads=[outb], writes=[mx], slot=outb)

        def stage_causal(l, m):
            mix_base = {"moba": 0, "fox": NM, "mla": 2 * NM}[m]
            RQ = {"moba": 96, "fox": 66, "mla": 96}[m]
            scale = {"moba": 0.125, "fox": 0.125, "mla": 96.0 ** -0.5}[m]
            with ExitStack() as s2:
                vall = P.sbuf(s2, "c_vall", [128, NT, NH * 65], BF16)
                for c in range(0, NT, 8):
                    P.dma("sp", vall[:, c:c + 8, :], VV[m][c * 128:(c + 8) * 128, :, :].rearrange("(s p) h c -> p s (h c)", p=128),
                          reads=[VV[m]], writes=[vall], slot=vall)
                qa = [P.sbuf(s2, "c_qa%d" % i, [128, S], BF16) for i in range(2)]
                ka = [P.sbuf(s2, "c_ka%d" % i, [128, S], BF16) for i in range(2)]
                pt = [P.sbuf(s2, "c_pt%d" % i, [128, 1024], BF16) for i in range(2)]
                sps = []
                for i_ in range(2):
                    P.uid += 1
                    t_ = s2.enter_context(nc.psum_tensor("c_sd%d_%d" % (i_, P.uid), [128, 1024], F32))
                    b_ = Buf("c_sd%d" % i_, t_, psum=True)
                    P.stage_bufs.append(b_)
                    sps.append(b_)
                ops_ = [P.psum(s2, "c_o%d" % i) for i in range(2)]
                mps = [P.psum(s2, "c_m%d" % i) for i in range(2)]
                fres = [(P.sbuf(s2, "c_osb%d" % i, [128, 512], F32), P.sbuf(s2, "c_rec%d" % i, [128, 512], F32), mps[0],
                         P.sbuf(s2, "c_out%d" % i, [128, 512], BF16)) for i in range(2)]
                bias = None
                if m == "fox":
                    nbias = sum(4 * i + 4 for i in range(NQ))
                    bias = P.sbuf(s2, "c_bias", [128, nbias], F32)
                    zc = P.sbuf(s2, "f_z", [128, 2048], F32)
                    lf = P.sbuf(s2, "f_lf", [128, 2048], F32)
                    pc = [P.sbuf(s2, "f_pc%d" % i, [128, 2048], F32) for i in range(2)]
                    cq = P.sbuf(s2, "f_cq", [128, 2048], F32)
                    hb_ = P.sbuf(s2, "f_hb", [128, 2048], BF16)
                    cumcol = P.sbuf(s2, "f_cumcol", [128, NT], F32)
                    rb = P.sbuf(s2, "f_rb", [128, NQ], F32)
                    nb_ = P.sbuf(s2, "f_nb", [128, 1], F32)
                if m == "moba":
                    km = P.sbuf(s2, "m_km", [64, NB], F32)
                    kmh = P.sbuf(s2, "m_kmh", [64, NB], BF16)
                    kml = P.sbuf(s2, "m_kml", [64, NB], BF16)
                    kmt = P.sbuf(s2, "m_kmt", [64, NB], F32)
                    gw = [P.sbuf(s2, "m_gw%d" % i, [128, max(NB, 8)], F32) for i in range(4)]
                    m8 = [P.sbuf(s2, "m_m8%d" % i, [128, 8], F32) for i in range(4)]
                    ind = [P.sbuf(s2, "m_ind%d" % i, [128, NB], F32) for i in range(4)]
                    pen = [P.sbuf(s2, "m_pen%d" % i, [128, 96], BF16) for i in range(4)]
                    for p_ in pen:
                        P.op("pool", lambda e: e.memset(p_[:, :], 0.0), writes=[p_])

                def load_head(i):
                    q_ = qa[i % 2]
                    k_ = ka[i % 2]
                    rq = 64 if m != "mla" else 96
                    P.dma("sp", q_[0:rq, :], QT[m][i, 0:rq, :], reads=[QT[m]], writes=[q_], slot=q_)
                    P.dma("sp", k_[0:rq, :], KT[m][i, 0:rq, :], reads=[KT[m]], writes=[k_], slot=k_)
                    if m == "moba":
                        P.dma("pool", k_[64:96, :], c_onehot[:, :], reads=[c_onehot], writes=[k_], slot=k_)
                    if m == "fox":
                        P.op("pool", lambda e: e.memset(k_[64:66, :], 1.0), writes=[k_])

                def prep_fox(i):
                    q_ = qa[i % 2]
                    P.dma("sp", nb_[64:66, :], bfg[l, i:i + 1, :].to_broadcast([2, 1]), reads=[bfg], writes=[nb_], slot=nb_)
                    P.op("dve", lambda e: e.tensor_scalar(out=nb_[64:66, :], in0=nb_[64:66, :], scalar1=-1.0, scalar2=None, op0=ALU.mult), reads=[nb_], writes=[nb_])
                    prev = None
                    for c in range(S // 2048):
                        csl = slice(c * 2048, (c + 1) * 2048)
                        P.dma("sp", zc[64:66, :], FG[i, :, csl], reads=[FG], writes=[zc], slot=zc)
                        P.op("act", lambda e: e.activation(out=lf[64:66, :], in_=zc[64:66, :], func=AF.Exp, scale=-1.0, bias=nb_[64:66, 0:1]), reads=[zc, nb_], writes=[lf])
                        P.op("act", lambda e: e.activation(out=lf[64:66, :], in_=lf[64:66, :], func=AF.Ln, scale=1.0, bias=cols[64:66, 7:8]), reads=[lf, cols], writes=[lf])
                        p_ = pc[c % 2]
                        init = 0.0 if prev is None else prev[64:66, 2047:2048]
                        rd = [lf, ones_f] + ([prev] if prev is not None else [])
                        P.op("dve", lambda e: e.tensor_tensor_scan(out=p_[64:66, :], data0=ones_f[64:66, 0:1].to_broadcast([2, 2048]), data1=lf[64:66, :],
                                                                   initial=init, op0=ALU.mult, op1=ALU.add), reads=rd, writes=[p_])
                        for qi in range(4):
                            qs = slice(qi * 512, (qi + 1) * 512)
                            P.op("dve", lambda e: e.tensor_scalar(out=cq[64:66, qs], in0=p_[64:66, qs], scalar1=p_[64:66, qi * 512:qi * 512 + 1], scalar2=-1.0,
                                                                  op0=ALU.subtract, op1=ALU.mult), reads=[p_], writes=[cq])
                        P.op("dve", lambda e: e.tensor_scalar(out=cq[64:66, :], in0=cq[64:66, :], scalar1=1.0 / scale, scalar2=None, op0=ALU.mult), reads=[cq], writes=[cq])
                        P.op("dve", lambda e: e.tensor_copy(out=hb_[64:66, :], in_=cq[64:66, :]), reads=[cq], writes=[hb_])
                        P.op("dve", lambda e: e.scalar_tensor_tensor(out=q_[64:66, csl], in0=hb_[64:66, :], scalar=cols[64:66, 4:5], in1=cq[64:66, :], op0=ALU.mult, op1=ALU.add),
                             reads=[hb_, cols, cq], writes=[q_])
                        mp = mps[1]
                        for jj in range(16):
                            P.op("pe", lambda e: e.matmul(mp[:, jj:jj + 1], lhsT=p_[64:65, jj * 128:(jj + 1) * 128], rhs=ones_f[64:65, 0:1], start=True, stop=True),
                                 reads=[p_, ones_f], writes=[mp], inc=(jj == 15))
                        P.op("pe", lambda e: e.matmul(mp[:, 16:20], lhsT=ones_f[64:65, 0:128], rhs=p_[64:65, 0:2048:512], start=True, stop=True),
                             reads=[p_, ones_f], writes=[mp])
                        P.op("dve", lambda e: e.tensor_copy(out=cumcol[:, c * 16:(c + 1) * 16], in_=mp[:, 0:16]), reads=[mp], writes=[cumcol])
                        P.op("dve", lambda e: e.tensor_copy(out=rb[:, c * 4:(c + 1) * 4], in_=mp[:, 16:20]), reads=[mp], writes=[rb])
                        prev = p_
                    bo = 0
                    for qi in range(NQ):
                        nj = 4 * qi + 4
                        P.op("dve", lambda e: e.tensor_scalar(out=bias[:, bo:bo + nj], in0=cumcol[:, 0:nj], scalar1=rb[:, qi:qi + 1], scalar2=None, op0=ALU.subtract),
                             reads=[cumcol, rb], writes=[bias])
                        bo += nj

                def prep_moba(i):
                    q_ = qa[i % 2]
                    k_ = ka[i % 2]
                    P.op("dve", lambda e: e.tensor_reduce(out=km[:, :], in_=k_[0:64, :].rearrange("p (j c) -> p j c", c=256), axis=AX.X, op=ALU.add), reads=[k_], writes=[km])
                    P.op("dve", lambda e: e.tensor_scalar(out=km[:, :], in0=km[:, :], scalar1=1.0 / 256, scalar2=None, op0=ALU.mult), reads=[km], writes=[km])
                    P.op("dve", lambda e: e.tensor_copy(out=kmh[:, :], in_=km[:, :]), reads=[km], writes=[kmh])
                    P.op("dve", lambda e: e.tensor_tensor(out=kmt[:, :], in0=km[:, :], in1=kmh[:, :], op=ALU.subtract), reads=[km, kmh], writes=[kmt])
                    P.op("dve", lambda e: e.tensor_copy(out=kml[:, :], in_=kmt[:, :]), reads=[kmt], writes=[kml])
                    def gate_part(t):
                        qb = t // 2
                        tsl = slice(t * 128, (t + 1) * 128)
                        pn = pen[t % 4]
                        P.op("pool", lambda e: e.memset(pn[:, 64:64 + NB], NEG), writes=[pn])
                        if qb <= 3:
                            P.op("pool", lambda e: e.memset(pn[:, 64:64 + qb + 1], 0.0), writes=[pn])
                        else:
                            mp = sps[t % 2]
                            P.op("pe", lambda e: e.matmul(mp[:, 0:qb], lhsT=q_[0:64, tsl], rhs=kmh[:, 0:qb], start=True, stop=False), reads=[q_, kmh], writes=[mp], inc=False)
                            P.op("pe", lambda e: e.matmul(mp[:, 0:qb], lhsT=q_[0:64, tsl], rhs=kml[:, 0:qb], start=False, stop=True), reads=[q_, kml], writes=[mp])
                            g_ = gw[t % 4]
                            w_ = max(qb, 8)
                            if qb < 8:
                                P.op("dve", lambda e: e.memset(g_[:, 0:8], -1e30), writes=[g_])
                            P.op("dve", lambda e: e.tensor_copy(out=g_[:, 0:qb], in_=mp[:, 0:qb]), reads=[mp], writes=[g_])
                            m_ = m8[t % 4]
                            P.op("dve", lambda e: e.max(out=m_[:, :], in_=g_[:, 0:w_]), reads=[g_], writes=[m_])
                            i_ = ind[t % 4]
                            P.op("dve", lambda e: e.tensor_scalar(out=i_[:, 0:qb], in0=g_[:, 0:qb], scalar1=m_[:, 2:3], scalar2=None, op0=ALU.is_ge), reads=[g_, m_], writes=[i_])
                            P.op("dve", lambda e: e.tensor_scalar(out=pn[:, 64:64 + qb], in0=i_[:, 0:qb], scalar1=-NEG, scalar2=NEG, op0=ALU.mult, op1=ALU.add), reads=[i_], writes=[pn])
                            P.op("pool", lambda e: e.memset(pn[:, 64 + qb:64 + qb + 1], 0.0), writes=[pn])

                    def tr_part(t):
                        tsl = slice(t * 128, (t + 1) * 128)
                        pn = pen[t % 4]
                        mp2 = mps[t % 2]
                        P.op("pe", lambda e: e.matmul(mp2[0:96, 0:128], lhsT=pn[:, 0:96], rhs=ident[:, :], start=True, stop=True), reads=[pn, ident], writes=[mp2])
                        P.op("act", lambda e: e.copy(out=q_[64:96, tsl], in_=mp2[64:96, 0:128]), reads=[mp2], writes=[q_])

                    SK = 3
                    for t in range(NT + SK):
                        if t < NT:
                            gate_part(t)
                        if t >= SK:
                            tr_part(t - SK)

                load_head(0)
                for i in range(NH):
                    if i + 1 < NH:
                        load_head(i + 1)
                    q_ = qa[i % 2]
                    k_ = ka[i % 2]
                    if m == "fox":
                        prep_fox(i)
                    if m == "moba":
                        prep_moba(i)
                    rows = slice(mix_base + i * 64, mix_base + i * 64 + 64)
                    merge = (bias is None)
                    units = []
                    for qi in range(NQ):
                        nj = 4 * qi + 4
                        j = 0
                        while j < nj:
                            o = j - 4 * qi
                            if merge and o < 0 and (j + 1) - 4 * qi < 0:
                                units.append((qi, [j, j + 1], nj))
                                j += 2
                            else:
                                units.append((qi, [j], nj))
                                j += 1
                    bias_off = [sum(4 * a + 4 for a in range(qi)) for qi in range(NQ)]
                    pend = []
                    nu = len(units)
                    vcol = slice(i * 65, i * 65 + 65)

                    def qk(ux):
                        qi, js, nj = units[ux]
                        sp_ = sps[ux % 2]
                        q0 = qi * 512
                        for bi, j in enumerate(js):
                            base = bi * 512
                            o = j - 4 * qi
                            ksl = slice(j * 128, (j + 1) * 128)
                            last = (bi == len(js) - 1)
                            if o < 0:
                                P.op("pe", lambda e: e.matmul(sp_[:, base:base + 512], lhsT=k_[0:RQ, ksl], rhs=q_[0:RQ, q0:q0 + 512], start=True, stop=True), reads=[k_, q_], writes=[sp_], inc=last)
                            else:
                                c0 = 128 * o
                                P.op("pe", lambda e: e.matmul(sp_[:, c0:c0 + 128], lhsT=ident[:, :], rhs=tri[:, :], start=True, stop=False), reads=[ident, tri], writes=[sp_], inc=False)
                                P.op("pe", lambda e: e.matmul(sp_[:, c0:c0 + 128], lhsT=k_[0:RQ, ksl], rhs=q_[0:RQ, q0 + c0:q0 + c0 + 128], start=False, stop=True),
                                     reads=[k_, q_], writes=[sp_], inc=(o == 3))
                                if o < 3:
                                    P.op("pe", lambda e: e.matmul(sp_[:, c0 + 128:512], lhsT=k_[0:RQ, ksl], rhs=q_[0:RQ, q0 + c0 + 128:q0 + 512], start=True, stop=True),
                                         reads=[k_, q_], writes=[sp_])

                    def ex(ux):
                        qi, js, nj = units[ux]
                        sp_ = sps[ux % 2]
                        p_ = pt[ux % 2]
                        if len(js) == 2:
                            P.op("act", lambda e: e.activation(out=p_[:, 0:1024], in_=sp_[:, 0:1024], func=AF.Exp, scale=scale), reads=[sp_], writes=[p_])
                            return
                        j = js[0]
                        o = j - 4 * qi
                        c0 = 0 if o < 0 else 128 * o
                        if bias is not None:
                            bcol = bias_off[qi] + j
                            P.op("act", lambda e: e.activation(out=p_[:, c0:512], in_=sp_[:, c0:512], func=AF.Exp, scale=scale, bias=bias[:, bcol:bcol + 1]),
                                 reads=[sp_, bias], writes=[p_])
                        else:
                            P.op("act", lambda e: e.activation(out=p_[:, c0:512], in_=sp_[:, c0:512], func=AF.Exp, scale=scale), reads=[sp_], writes=[p_])

                    def pv(ux):
                        qi, js, nj = units[ux]
                        p_ = pt[ux % 2]
                        o_ = ops_[qi % 2]
                        for bi, j in enumerate(js):
                            base = bi * 512
                            o = j - 4 * qi
                            c0 = 0 if o < 0 else 128 * o
                            P.op("pe", lambda e: e.matmul(o_[0:65, c0:512], lhsT=vall[:, j, vcol], rhs=p_[:, base + c0:base + 512], start=(j == 0), stop=(j == nj - 1)),
                                 reads=[vall, p_], writes=[o_], inc=(j == nj - 1 or bi == len(js) - 1))
                            if j == nj - 1:
                                res = fres[qi % 2]
                                srcb = finalize_rows(None, o_, True, 512, rows, slice(qi * 512, (qi + 1) * 512), res)
                                pend.append([ux + 4, srcb, qi, res])

                    for ux in range(nu + 1):
                        if ux < nu:
                            qk(ux)
                            ex(ux)
                        if ux >= 1:
                            pv(ux - 1)
                        while pend and (pend[0][0] <= ux or ux == nu):
                            _, srcb, qi, res = pend.pop(0)
                            finalize_part2(srcb, 512, rows, slice(qi * 512, (qi + 1) * 512), res)
            P.end_stage()

        def stage_dil(l):
            mix_base = 3 * NM
            scale = 0.125
            with ExitStack() as s2:
                qn = P.sbuf(s2, "d_qn", [64, S], BF16)
                kn = P.sbuf(s2, "d_kn", [64, S], BF16)
                qp = P.sbuf(s2, "d_qp", [64, S], BF16)
                kp = P.sbuf(s2, "d_kp", [64, S], BF16)
                vp = [P.sbuf(s2, "d_vp%d" % i, [128, NT, 65], BF16) for i in range(2)]
                acc = P.sbuf(s2, "d_acc", [128, S], F32)
                pt = [P.sbuf(s2, "d_pt%d" % i, [128, 512], BF16) for i in range(3)]
                sps = [P.psum(s2, "d_s%d" % i) for i in range(3)]
                ops_ = [P.psum(s2, "d_o%d" % i) for i in range(2)]
                mps = [P.psum(s2, "d_m%d" % i) for i in range(1)]
                fres = [(None, P.sbuf(s2, "d_rec%d" % i, [128, 512], F32), mps[0], P.sbuf(s2, "d_out%d" % i, [128, 512], BF16)) for i in range(2)]
                vcnt = 0
                for i in range(NH):
                    P.dma("sp", qn[:, :], QT["dil"][i, 0:64, :], reads=[QT["dil"]], writes=[qn], slot=qn)
                    P.dma("sp", kn[:, :], KT["dil"][i, 0:64, :], reads=[KT["dil"]], writes=[kn], slot=kn)
                    rows = slice(mix_base + i * 64, mix_base + i * 64 + 64)
                    for ci, d in enumerate(DIL_CFG):
                        nbr = S // (128 * d)
                        v_ = vp[vcnt % 2]
                        vcnt += 1
                        vsrc = VV["dil"][:, i, :].rearrange("(b p r) c -> p r b c", p=128, r=d)
                        for r in range(d):
                            P.dma("sp", v_[:, r * nbr:(r + 1) * nbr, :], vsrc[:, r, :, :], reads=[VV["dil"]], writes=[v_], slot=v_)
                        if d == 1:
                            qs_, ks_ = qn, kn
                        else:
                            P.op("dve", lambda e: e.tensor_copy(out=qp[:, :].rearrange("p (r n) -> p r n", r=d), in_=qn[:, :].rearrange("p (n r) -> p r n", r=d)), reads=[qn], writes=[qp])
                            P.op("pool", lambda e: e.tensor_copy(out=kp[:, :].rearrange("p (r n) -> p r n", r=d), in_=kn[:, :].rearrange("p (n r) -> p r n", r=d)), reads=[kn], writes=[kp])
                            qs_, ks_ = qp, kp
                        NG = NT
                        halves = [(g0, half) for g0 in range(0, NG, 4) for half in range(2)]

                        def d_qk(hx):
                            g0, half = halves[hx]
                            sp_ = sps[hx % 3]
                            p_ = pt[hx % 3]
                            for qq in range(2):
                                g = g0 + half * 2 + qq
                                b = g % nbr
                                gsl = slice(g * 128, (g + 1) * 128)
                                for which in range(2):
                                    cs = slice((qq * 2 + which) * 128, (qq * 2 + which + 1) * 128)
                                    if which == 0 and b == 0:
                                        kb, msk = g, negall
                                    elif which == 0:
                                        kb, msk = g - 1, triu
                                    else:
                                        kb, msk = g, tri
                                    P.op("pe", lambda e: e.matmul(sp_[:, cs], lhsT=ident[:, :], rhs=msk[:, :], start=True, stop=False), reads=[ident, msk], writes=[sp_], inc=False)
                                    P.op("pe", lambda e: e.matmul(sp_[:, cs], lhsT=ks_[0:64, kb * 128:(kb + 1) * 128], rhs=qs_[0:64, gsl], start=False, stop=True),
                                         reads=[ks_, qs_], writes=[sp_], inc=(qq == 1 and which == 1))
                            P.op("act", lambda e: e.activation(out=p_[:, :], in_=sp_[:, :], func=AF.Exp, scale=scale), reads=[sp_], writes=[p_])

                        def d_pv(hx):
                            g0, half = halves[hx]
                            p_ = pt[hx % 3]
                            o_ = ops_[(g0 // 4) % 2]
                            for qq in range(2):
                                g = g0 + half * 2 + qq
                                b = g % nbr
                                oc = slice((half * 2 + qq) * 128, (half * 2 + qq + 1) * 128)
                                for which in range(2):
                                    cs = slice((qq * 2 + which) * 128, (qq * 2 + which + 1) * 128)
                                    kb = g if (which == 1 or b == 0) else g - 1
                                    P.op("pe", lambda e: e.matmul(o_[0:65, oc], lhsT=v_[:, kb, :], rhs=p_[:, cs], start=(which == 0), stop=(which == 1)),
                                         reads=[v_, p_], writes=[o_], inc=(which == 1))
                            if half == 1:
                                for qq4 in range(4):
                                    g = g0 + qq4
                                    r, b = g // nbr, g % nbr
                                    if d == 1:
                                        dst = acc[0:65, g * 128:(g + 1) * 128]
                                    else:
                                        st0 = r + d * 128 * b
                                        dst = acc[0:65, st0:st0 + d * 127 + 1:d]
                                    src = o_[0:65, qq4 * 128:(qq4 + 1) * 128]
                                    if ci == 0:
                                        P.op("dve", lambda e: e.tensor_copy(out=dst, in_=src), reads=[o_], writes=[acc])
                                    else:
                                        P.op("dve", lambda e: e.tensor_tensor(out=dst, in0=dst, in1=src, op=ALU.add), reads=[o_, acc], writes=[acc])

                        nh_ = len(halves)
                        for hx in range(nh_ + 1):
                            if hx < nh_:
                                d_qk(hx)
                            if hx >= 1:
                                d_pv(hx - 1)
                    for qi in range(NQ):
                        res = fres[qi % 2]
                        tsl = slice(qi * 512, (qi + 1) * 512)
                        osb, rec, bcp, outb = res
                        P.op("dve", lambda e: e.reciprocal(out=rec[64:65, 0:512], in_=acc[64:65, tsl]), reads=[acc], writes=[rec])
                        finalize_part2(acc, 512, rows, tsl, res, src_off=qi * 512)
            P.end_stage()

        def epilogue(yps, x_t, g_post, g_next, tmp, junk, ssb, hb_, hdst, hdst_sl, tps, want_h):
            ss, rstd, ss2, rstd2 = ssb
            P.op("act", lambda e: e.activation(out=junk[:, 0:512], in_=yps[0][:, :], func=AF.Square, accum_out=ss[:, 0:1]), reads=[yps[0]], writes=[junk, ss])
            P.op("act", lambda e: e.activation(out=junk[:, 512:1024], in_=yps[1][:, :], func=AF.Square, accum_out=ss[:, 1:2]), reads=[yps[1]], writes=[junk, ss])
            P.op("dve", lambda e: e.tensor_tensor(out=ss[:, 0:1], in0=ss[:, 0:1], in1=ss[:, 1:2], op=ALU.add), reads=[ss], writes=[ss])
            rstd_from_ss(ss, D, rstd)
            for hf in range(2):
                P.op("dve", lambda e: e.scalar_tensor_tensor(out=tmp[:, hf * 512:(hf + 1) * 512], in0=yps[hf][:, :], scalar=rstd[:, 0:1], in1=g_post[:, hf * 512:(hf + 1) * 512],
                                                             op0=ALU.mult, op1=ALU.mult), reads=[yps[hf], rstd, g_post], writes=[tmp])
            P.op("dve", lambda e: e.tensor_tensor(out=x_t[:, :], in0=x_t[:, :], in1=tmp[:, :], op=ALU.add), reads=[x_t, tmp], writes=[x_t])
            if want_h:
                P.op("act", lambda e: e.activation(out=junk[:, :], in_=x_t[:, :], func=AF.Square, accum_out=ss2[:, 0:1]), reads=[x_t], writes=[junk, ss2])
                rstd_from_ss(ss2, D, rstd2)
                P.op("dve", lambda e: e.scalar_tensor_tensor(out=hb_[:, :], in0=x_t[:, :], scalar=rstd2[:, 0:1], in1=g_next[:, :], op0=ALU.mult, op1=ALU.mult),
                     reads=[x_t, rstd2, g_next], writes=[hb_])
                transposes_to(hb_, hdst, hdst_sl, tps)

        class EpiPipe:
            def __init__(self, s2, name, g_post, g_next, want_h, tps_pairs, pair=1):
                self.pair = pair
                self.NB = 2 + pair
                self.x = [P.sbuf(s2, "%s_x%d" % (name, i), [128, D], F32) for i in range(self.NB)]
                self.tmp = [P.sbuf(s2, "%s_tmp%d" % (name, i), [128, D], F32) for i in range(self.NB)]
                self.ss = [P.sbuf(s2, "%s_ss%d" % (name, i), [128, 4], F32) for i in range(self.NB)]
                self.s2_ = [P.sbuf(s2, "%s_sq%d" % (name, i), [128, 2], F32) for i in range(self.NB)]
                self.hb = [P.sbuf(s2, "%s_hb%d" % (name, i), [128, D], BF16) for i in range(2 * pair)]
                self.junk = P.sbuf(s2, "%s_junk" % name, [128, D], BF16)
                self.junk2 = [P.sbuf(s2, "%s_junk2%d" % (name, i), [128, D], BF16) for i in range(pair)]
                self.g_post, self.g_next, self.want_h, self.tps_pairs = g_post, g_next, want_h, tps_pairs
                self.pending = []
                self.cnt = 0

            def xbuf(self, t):
                return self.x[t % self.NB]

            def _run_pending(self):
                gens = [self._chain(t_, i_, k_) for k_, (t_, i_) in enumerate(self.pending)]
                self.pending = []
                while gens:
                    for g_ in list(gens):
                        try:
                            next(g_)
                        except StopIteration:
                            gens.remove(g_)

            def push(self, t, yps, info):
                if len(self.pending) >= self.pair:
                    self._run_pending()
                b = t % self.NB
                ss, tmp = self.ss[b], self.tmp[b]
                P.op("act", lambda e: e.activation(out=self.junk[:, 0:512], in_=yps[0][:, :], func=AF.Square, accum_out=ss[:, 0:1]), reads=[yps[0]], writes=[self.junk, ss])
                P.op("act", lambda e: e.activation(out=self.junk[:, 512:1024], in_=yps[1][:, :], func=AF.Square, accum_out=ss[:, 1:2]), reads=[yps[1]], writes=[self.junk, ss])
                for hf in range(2):
                    P.op("dve", lambda e: e.tensor_tensor(out=tmp[:, hf * 512:(hf + 1) * 512], in0=yps[hf][:, :], in1=self.g_post[:, hf * 512:(hf + 1) * 512], op=ALU.mult),
                         reads=[yps[hf], self.g_post], writes=[tmp])
                self.pending.append((t, info))

            def flush(self):
                if self.pending:
                    self._run_pending()

            def _chain(self, t, info, lane):
                b = t % self.NB
                ss, tmp, x_t, sq = self.ss[b], self.tmp[b], self.x[b], self.s2_[b]
                jk = self.junk2[lane]
                P.op("dve", lambda e: e.tensor_tensor(out=ss[:, 2:3], in0=ss[:, 0:1], in1=ss[:, 1:2], op=ALU.add), reads=[ss], writes=[ss])
                yield
                P.op("act", lambda e: e.activation(out=ss[:, 3:4], in_=ss[:, 2:3], func=AF.Ln, scale=1.0 / D, bias=EPSC()), reads=[ss, cols], writes=[ss])
                yield
                P.op("act", lambda e: e.activation(out=ss[:, 3:4], in_=ss[:, 3:4], func=AF.Exp, scale=-0.5), reads=[ss], writes=[ss])
                yield
                P.op("dve", lambda e: e.scalar_tensor_tensor(out=x_t[:, :], in0=tmp[:, :], scalar=ss[:, 3:4], in1=x_t[:, :], op0=ALU.mult, op1=ALU.add),
                     reads=[tmp, ss, x_t], writes=[x_t])
                yield
                if self.want_h:
                    P.op("act", lambda e: e.activation(out=jk[:, :], in_=x_t[:, :], func=AF.Square, accum_out=sq[:, 0:1]), reads=[x_t], writes=[jk, sq])
                    yield
                    P.op("act", lambda e: e.activation(out=sq[:, 1:2], in_=sq[:, 0:1], func=AF.Ln, scale=1.0 / D, bias=EPSC()), reads=[sq, cols], writes=[sq])
                    yield
                    P.op("act", lambda e: e.activation(out=sq[:, 1:2], in_=sq[:, 1:2], func=AF.Exp, scale=-0.5), reads=[sq], writes=[sq])
                    yield
                    h_b = self.hb[self.cnt % len(self.hb)]
                    tp_ = self.tps_pairs[self.cnt % len(self.tps_pairs)]
                    self.cnt += 1
                    P.op("dve", lambda e: e.scalar_tensor_tensor(out=h_b[:, :], in0=x_t[:, :], scalar=sq[:, 1:2], in1=self.g_next[:, :], op0=ALU.mult, op1=ALU.mult),
                         reads=[x_t, sq, self.g_next], writes=[h_b])
                    yield
                    transposes_to(h_b, info["hdst"], info["hsl"], tp_)
                    yield
                info["after"](t, x_t)

        def stage_c1(l):
            with ExitStack() as s2:
                wo = P.sbuf(s2, "wo", [128, 8, D], BF16)
                for k in range(8):
                    P.dma("pool", wo[:, k, :], wout[l, k * 128:(k + 1) * 128, :], reads=[wout], writes=[wo], slot=wo)
                g_post = load_gvec(s2, "c1_gpost", l, 1)
                g_next = load_gvec(s2, "c1_gnext", l, 2)
                mt = [P.sbuf(s2, "c1_mt%d" % i, [128, 8, 512], BF16) for i in range(2)]
                mt2 = [P.sbuf(s2, "c1_mu%d" % i, [128, 8, 512], BF16) for i in range(2)] if HS > 1 else None
                ho = [P.sbuf(s2, "c1_ho%d" % i, [128, 8, 512], BF16) for i in range(2)]
                yps = [P.psum(s2, "c1_y%d" % i) for i in range(4)]
                tps = [P.psum(s2, "c1_t%d" % i) for i in range(4)]
                ep = EpiPipe(s2, "c1e", g_post, g_next, True, [tps[0:2], tps[2:4]], pair=2)

                def load_m(n):
                    m_ = mt[n % 2]
                    P.dma("sp", m_[:, :, :], mixG[0][:, n * 512:(n + 1) * 512].rearrange("(k p) t -> p k t", p=128), reads=[mixG[0]], writes=[m_], slot=m_)
                    if HS > 1:
                        u_ = mt2[n % 2]
                        P.dma("sp", u_[:, :, :], mixG[1][:, n * 512:(n + 1) * 512].rearrange("(k p) t -> p k t", p=128), reads=[mixG[1]], writes=[u_], slot=u_)
                        P.op("dve", lambda e: e.tensor_scalar(out=m_[:, :, :], in0=m_[:, :, :], scalar1=selc[:, 0:1], scalar2=None, op0=ALU.mult), reads=[m_, selc], writes=[m_])
                        P.op("dve", lambda e: e.scalar_tensor_tensor(out=m_[:, :, :], in0=u_[:, :, :], scalar=selc[:, 1:2], in1=m_[:, :, :], op0=ALU.mult, op1=ALU.add),
                             reads=[u_, selc, m_], writes=[m_])

                def after(t, x_t):
                    n, sub = t // 4, t % 4
                    P.dma("sp", xs[t * 128:(t + 1) * 128, :], x_t[:, :], reads=[x_t], writes=[xs], slot=x_t)
                    if sub == 3:
                        o = ho[n % 2]
                        P.dma("sp", h2T[:, :, n * 512:(n + 1) * 512], o[:, :, :], reads=[o], writes=[h2T], slot=o)

                load_m(0)
                for t in range(NTO):
                    n, sub = t // 4, t % 4
                    if sub == 0 and n + 1 < NQO:
                        load_m(n + 1)
                    m_ = mt[n % 2]
                    x_t = ep.xbuf(t)
                    P.dma("sp", x_t[:, :], xs[t * 128:(t + 1) * 128, :], reads=[xs], writes=[x_t], slot=x_t)
                    yp = yps[2 * (t % 2):2 * (t % 2) + 2]
                    for hf in range(2):
                        for k in range(8):
                            P.op("pe", lambda e: e.matmul(yp[hf][:, :], lhsT=m_[:, k, sub * 128:(sub + 1) * 128], rhs=wo[:, k, hf * 512:(hf + 1) * 512], start=(k == 0), stop=(k == 7)),
                                 reads=[m_, wo], writes=[yp[hf]], inc=(k == 7))
                    ep.push(t, yp, {"hdst": ho[n % 2], "hsl": slice(sub * 128, (sub + 1) * 128), "after": after})
                ep.flush()
            P.end_stage()

        def stage_c2(l, last):
            T = 1024
            NTT = SO // T
            NJ = DFF // 128
            with ExitStack() as s2:
                wdn = P.sbuf(s2, "wdn", [128, NJ, D], BF16)
                for j in range(NJ):
                    P.dma("pool", wdn[:, j, :], wd[l, j * 128:(j + 1) * 128, :], reads=[wd], writes=[wdn], slot=wdn)
                g_post = load_gvec(s2, "c2_gpost", l, 3)
                g_next = load_gvec(s2, "c2_gnext", l + 1, 0) if not last else g_post
                h2 = [P.sbuf(s2, "c2_h2%d" % i, [128, 8, T], BF16) for i in range(2)]
                fT = P.sbuf(s2, "c2_fT", [128, NJ, T], BF16)
                NR = 2
                wgr = [P.sbuf(s2, "c2_wg%d" % i, [128, 8, 256], BF16) for i in range(NR)]
                wur = [P.sbuf(s2, "c2_wu%d" % i, [128, 8, 256], BF16) for i in range(NR)]
                sg = [P.sbuf(s2, "c2_sg%d" % i, [128, 512], F32) for i in range(2)]
                ho = [P.sbuf(s2, "c2_ho%d" % i, [128, 8, 512], BF16) for i in range(2)]
                gps = [P.psum(s2, "c2_g%d" % i) for i in range(2)]
                ups = [P.psum(s2, "c2_u%d" % i) for i in range(2)]
                yps = [P.psum(s2, "c2_y%d" % i) for i in range(2)]
                tps = [P.psum(s2, "c2_t%d" % i) for i in range(2)]
                ep = EpiPipe(s2, "c2e", g_post, g_next, not last, [tps])

                def after(t, x_t):
                    dst = out_d if last else xs
                    P.dma("sp", dst[t * 128:(t + 1) * 128, :], x_t[:, :], reads=[x_t], writes=[dst], slot=x_t)
                    if (not last) and t % 4 == 3:
                        n = t // 4
                        o = ho[n % 2]
                        P.dma("sp", hTown[:, n * 512:(n + 1) * 512].rearrange("(k p) t -> p k t", p=128), o[:, :, :], reads=[o], writes=[hTown], slot=o)

                def load_w(jp, slot_i):
                    for (ring, src) in ((wgr, wg), (wur, wu)):
                        r_ = ring[slot_i % NR]
                        P.dma("pool", r_[:, :, :], src[l, :, jp * 256:(jp + 1) * 256].rearrange("(k p) c -> p k c", p=128), reads=[src], writes=[r_], slot=r_)

                def load_h(tt):
                    h_ = h2[tt % 2]
                    P.dma("sp", h_[:, :, :], h2T[:, :, tt * T:(tt + 1) * T], reads=[h2T], writes=[h_], slot=h_)

                load_h(0)
                load_w(0, 0)
                for tt in range(NTT):
                    if tt + 1 < NTT:
                        load_h(tt + 1)
                    h_ = h2[tt % 2]
                    for jp in range(NJ // 2):
                        nxt = (tt * (NJ // 2) + jp + 1)
                        if nxt < NTT * (NJ // 2):
                            load_w(nxt % (NJ // 2), nxt)
                        cur = tt * (NJ // 2) + jp
                        wg_, wu_ = wgr[cur % NR], wur[cur % NR]
                        for jj in range(2):
                            j = jp * 2 + jj
                            for hf in range(T // 512):
                                gp = gps[(j * 2 + hf) % 2]
                                up = ups[(j * 2 + hf) % 2]
                                for (pp, w_) in ((gp, wg_), (up, wu_)):
                                    for k in range(8):
                                        P.op("pe", lambda e: e.matmul(pp[:, :], lhsT=w_[:, k, jj * 128:(jj + 1) * 128], rhs=h_[:, k, hf * 512:(hf + 1) * 512], start=(k == 0), stop=(k == 7)),
                                             reads=[w_, h_], writes=[pp], inc=(k == 7))
                                s_ = sg[(j * 2 + hf) % 2]
                                P.op("act", lambda e: e.activation(out=s_[:, :], in_=gp[:, :], func=AF.Silu), reads=[gp], writes=[s_])
                                P.op("dve", lambda e: e.tensor_tensor(out=fT[:, j, hf * 512:(hf + 1) * 512], in0=s_[:, :], in1=up[:, :], op=ALU.mult), reads=[s_, up], writes=[fT])
                    for sub in range(T // 128):
                        t = tt * (T // 128) + sub
                        x_t = ep.xbuf(t)
                        P.dma("sp", x_t[:, :], xs[t * 128:(t + 1) * 128, :], reads=[xs], writes=[x_t], slot=x_t)
                        for hf in range(2):
                            for j in range(NJ):
                                P.op("pe", lambda e: e.matmul(yps[hf][:, :], lhsT=fT[:, j, sub * 128:(sub + 1) * 128], rhs=wdn[:, j, hf * 512:(hf + 1) * 512], start=(j == 0), stop=(j == NJ - 1)),
                                     reads=[fT, wdn], writes=[yps[hf]], inc=(j == NJ - 1))
                        ep.push(t, yps, {"hdst": ho[(t // 4) % 2], "hsl": slice((t % 4) * 128, (t % 4 + 1) * 128), "after": after})
                ep.flush()
            P.end_stage()
            if HS > 1 and not last:
                for c in range(4):
                    collective(hTown, hTf, hTown[c * 256:(c + 1) * 256, :], hTf[c * HS * 256:(c + 1) * HS * 256, :])

        def dump_bf16(src, rows):
            with ExitStack() as s2:
                a = P.sbuf(s2, "dbg_a", [128, SO], BF16)
                b = P.sbuf(s2, "dbg_b", [128, SO], F32)
                for r0 in range(0, rows, 128):
                    P.dma("sp", a[:, :], src[r0:r0 + 128, :], reads=[src], writes=[a], slot=a)
                    P.op("dve", lambda e: e.tensor_copy(out=b[:, :], in_=a[:, :]), reads=[a], writes=[b])
                    P.dma("sp", dbg_out[r0:r0 + 128, :], b[:, :], reads=[b], writes=[dbg_out], slot=b)
            P.end_stage()

        P.end_stage()
        stage_tables()
        stage_h0()
        for l in range(depth):
            stage_a(l)
            def gather_mix(m_):
                if HS > 1:
                    for j in range(HS):
                        collective(mixX[j], mixG[j], mixX[j][m_ * NM:(m_ + 1) * NM, :], mixG[j][m_ * HS * NM:(m_ + 1) * HS * NM, :])
            stage_causal(l, "moba")
            gather_mix(0)
            stage_causal(l, "fox")
            gather_mix(1)
            stage_causal(l, "mla")
            gather_mix(2)
            stage_dil(l)
            gather_mix(3)
            if dbg == "mix" and l == 0:
                dump_bf16(mixG[0], HS * 4 * NM)
            stage_c1(l)
            stage_c2(l, l == depth - 1)
        P.barrier()
        print("program built: instrs=%d sems=%d" % (P.ninstr, P.nsem))
    return nc, groups


_CACHE = {}


def prepare_weights(inp, depth, heads, groups):
    w_in = np.asarray(inp["w_in"], np.float32)
    NH = len(heads)
    cols = np.concatenate([c for _, c in groups]).astype(np.int64)
    wA = np.ascontiguousarray(w_in[:depth][:, :, cols])
    perm32 = np.concatenate([np.arange(16, 32), np.arange(0, 16)])
    wq = np.asarray(inp["w_mla_q_up"], np.float32)[:depth]
    qcols = []
    for h in heads:
        qcols.append(h * 96 + np.arange(96))
        qcols.append(np.concatenate([h * 96 + np.arange(64), h * 96 + 64 + perm32]))
    wqu = np.ascontiguousarray(wq[:, :, np.concatenate(qcols)])
    wkv = np.asarray(inp["w_mla_kv_up"], np.float32)[:depth]
    kcols = np.concatenate([h * 128 + np.arange(128) for h in heads])
    wkvu = np.ascontiguousarray(wkv[:, :, kcols])
    gvv = np.stack([np.asarray(inp[k], np.float32)[:depth] for k in ("g_pre_mix", "g_post_mix", "g_pre_ffn", "g_post_ffn")], axis=1)
    d = {
        "wA": wA, "wqu": wqu, "wkvu": wkvu,
        "wout": np.ascontiguousarray(np.asarray(inp["w_out"], np.float32)[:depth]),
        "wg": np.ascontiguousarray(np.asarray(inp["w_gate"], np.float32)[:depth]),
        "wu": np.ascontiguousarray(np.asarray(inp["w_up"], np.float32)[:depth]),
        "wd": np.ascontiguousarray(np.asarray(inp["w_down"], np.float32)[:depth]),
        "gv": np.ascontiguousarray(gvv),
        "gq": np.ascontiguousarray(np.asarray(inp["g_mla_q"], np.float32)[:depth, :, None]),
        "gkv": np.ascontiguousarray(np.asarray(inp["g_mla_kv"], np.float32)[:depth, :, None]),
        "bfg": np.ascontiguousarray(np.asarray(inp["b_forget"], np.float32)[:depth][:, heads, None]),
    }
    return d


def run(inp, S, depth, B, dbg=None, HS=2):
    NH = 4 // HS
    ncores = B * HS
    key = (S, depth, NH, HS, ncores, dbg)
    if key not in _CACHE:
        _CACHE[key] = build_program(S, depth, NH, HS, dbg, ncores)
    nc, _ = _CACHE[key]
    consts = make_consts(S)
    x = np.asarray(inp["x"], np.float32)
    pos = np.asarray(inp["positions"], np.int32)
    SO = S // HS
    perm = []
    for m in range(4):
        for r in range(HS):
            for i in range(NH):
                perm.append(m * 256 + (r * NH + i) * 64 + np.arange(64))
    perm = np.concatenate(perm)
    per_half = []
    for hf in range(HS):
        heads = [hf * NH + i for i in range(NH)]
        wd_ = prepare_weights(inp, depth, heads, make_groups(heads))
        wd_["wout"] = np.ascontiguousarray(wd_["wout"][:, perm, :])
        sel = np.zeros((128, 2), np.float32)
        sel[:, hf] = 1.0
        wd_["sel"] = sel
        per_half.append(wd_)
    in_maps = []
    for b in range(B):
        for hf in range(HS):
            m = dict(per_half[hf])
            m.update(consts)
            m["x"] = np.ascontiguousarray(x[b])
            if HS > 1:
                m["x_own"] = np.ascontiguousarray(x[b, hf * SO:(hf + 1) * SO])
            m["pos"] = np.ascontiguousarray(pos[b][None, :])
            in_maps.append(m)
    res = run_bass_kernel_spmd(nc, in_maps, core_ids=list(range(ncores)))
    out = np.empty((B, S, D), np.float32)
    for b in range(B):
        for hf in range(HS):
            out[b, hf * SO:(hf + 1) * SO] = np.asarray(res.results[b * HS + hf]["out"], np.float32)
    if dbg:
        return out, [np.asarray(r["dbg"]) for r in res.results]
    return out


def kernel(**inputs):
    return run(inputs, 8192, 4, 4, HS=2)
```

```python
from contextlib import ExitStack
import numpy as np
import concourse.bass as bass
import concourse.mybir as mybir
from concourse.bass_utils import run_bass_kernel_spmd

F32 = mybir.dt.float32
BF16 = mybir.dt.bfloat16
I32 = mybir.dt.int32
AF = mybir.ActivationFunctionType
ALU = mybir.AluOpType
AX = mybir.AxisListType

D = 1024
DFF = 2816
NEG = -30000.0
EPS = 1e-6
TWO_PI_HI = 6.28125
TWO_PI_LO = 2.0 * np.pi - 6.28125
DIL_CFG = (1, 4, 16)


class Buf:
    def __init__(self, name, t=None, psum=False, multi=False):
        self.name = name
        self.t = t
        self.psum = psum
        self.multi = multi
        self.w = {}
        self.r = {}
        self.rec = None

    def __getitem__(self, idx):
        return self.t[idx]


class SemRec:
    def __init__(self, sem):
        self.sem = sem
        self.n = 0


class Prog:
    def __init__(self, nc, stack):
        self.nc = nc
        self.stack = stack
        self.engs = {"pe": nc.tensor, "act": nc.scalar, "dve": nc.vector, "pool": nc.gpsimd, "sp": nc.sync}
        self.esem = {}
        self.ecnt = {}
        self.waited = {}
        self.nsem = 0
        for k in self.engs:
            self.esem[k] = self._new_sem("e_" + k)
            self.ecnt[k] = 0
            self.waited[k] = {}
        self.pool = []
        self.recs = []
        self.stage_bufs = []
        self.ninstr = 0

    def _new_sem(self, name):
        s = self.stack.enter_context(self.nc.semaphore(name))
        self.nsem += 1
        return s

    def take_rec(self):
        if self.pool:
            return self.pool.pop()
        r = SemRec(self._new_sem("d%d" % len(self.recs)))
        self.recs.append(r)
        return r

    def sbuf(self, st, name, shape, dtype):
        self.uid = getattr(self, "uid", 0) + 1
        name = "%s_%d" % (name, self.uid)
        t = st.enter_context(self.nc.sbuf_tensor(name, list(shape), dtype))
        b = Buf(name, t)
        self.stage_bufs.append(b)
        return b

    def psum(self, st, name):
        self.uid = getattr(self, "uid", 0) + 1
        name = "%s_%d" % (name, self.uid)
        t = st.enter_context(self.nc.psum_tensor(name, [128, 512], F32))
        b = Buf(name, t, psum=True)
        self.stage_bufs.append(b)
        return b

    def dram(self, name, shape, dtype, kind="Internal"):
        t = self.nc.dram_tensor(name, list(shape), dtype, kind=kind)
        return Buf(name, t.ap(), multi=True)

    def _wait(self, e, deps):
        eng = self.engs[e]
        w = self.waited[e]
        best = {}
        for (sem, val) in deps:
            k = id(sem)
            if k not in best or best[k][1] < val:
                best[k] = (sem, val)
        for k, (sem, val) in best.items():
            if sem is self.esem[e] and e == "pe":
                continue
            if w.get(k, 0) >= val:
                continue
            eng.wait_ge(sem, val)
            w[k] = val

    @staticmethod
    def _merge(d, tok):
        k = id(tok[0])
        if k not in d or d[k][1] < tok[1]:
            d[k] = tok

    def _deps(self, e, reads, writes):
        deps = []
        for b in reads:
            deps += list(b.w.values())
            if b.psum:
                deps += [t for t in b.r.values() if t[0] is not self.esem[e]]
        for b in writes:
            if b.multi:
                continue
            deps += list(b.w.values())
            deps += list(b.r.values())
        return deps

    def _post(self, tok, reads, writes):
        for b in reads:
            self._merge(b.r, tok)
        for b in writes:
            if b.multi:
                self._merge(b.w, tok)
            else:
                b.w = {id(tok[0]): tok}
                b.r = {}

    def op(self, e, fn, reads=(), writes=(), inc=True):
        self._wait(e, self._deps(e, reads, writes))
        ins = fn(self.engs[e])
        self.ninstr += 1
        if inc:
            self.ecnt[e] += 1
            ins.then_inc(self.esem[e], 1)
            tok = (self.esem[e], self.ecnt[e])
        else:
            tok = (self.esem[e], self.ecnt[e] + 1)
        self._post(tok, reads, writes)
        return tok

    def dma(self, q, out_ap, in_ap, reads, writes, slot, **kw):
        if slot.rec is None:
            slot.rec = self.take_rec()
        self._wait(q, self._deps(q, reads, writes))
        ins = self.engs[q].dma_start(out=out_ap, in_=in_ap, **kw)
        self.ninstr += 1
        slot.rec.n += 1
        ins.then_inc(slot.rec.sem, 16)
        tok = (slot.rec.sem, 16 * slot.rec.n)
        self._post(tok, reads, writes)
        return tok

    def barrier(self):
        toks = []
        for k in self.engs:
            if self.ecnt[k] > 0:
                toks.append((self.esem[k], self.ecnt[k]))
        for r in self.recs:
            if r.n > 0:
                toks.append((r.sem, 16 * r.n))
        for e in self.engs:
            self._wait(e, toks)

    def end_stage(self):
        self.barrier()
        for b in self.stage_bufs:
            if b.rec is not None:
                self.pool.append(b.rec)
                b.rec = None
        self.stage_bufs = []


def w_in_offsets():
    o = {}
    o["moba_q"], o["moba_k"], o["moba_v"] = 0, 256, 512
    o["fox_q"], o["fox_k"], o["fox_v"] = 768, 1024, 1280
    o["fg"] = 1536
    o["cq"] = 1540
    o["ckv"] = 1732
    o["kr"] = 1860
    o["dil_q"], o["dil_k"], o["dil_v"] = 1892, 2148, 2404
    return o


def make_groups(heads):
    o = w_in_offsets()
    nh = len(heads)
    perm64 = np.concatenate([np.arange(32, 64), np.arange(0, 32)])
    perm32 = np.concatenate([np.arange(16, 32), np.arange(0, 16)])
    groups = []
    for m in ("moba", "dil"):
        for t in ("q", "k"):
            for hp in range(nh // 2):
                ha, hb = heads[2 * hp], heads[2 * hp + 1]
                base = o["%s_%s" % (m, t)]
                ca = np.concatenate([base + ha * 64 + np.arange(64), base + hb * 64 + np.arange(64)])
                cb = np.concatenate([base + ha * 64 + perm64, base + hb * 64 + perm64])
                groups.append(("%s_%s_%d_A" % (m, t, hp), ca))
                groups.append(("%s_%s_%d_B" % (m, t, hp), cb))
    for i, h in enumerate(heads):
        c = np.concatenate([o["fox_q"] + h * 64 + np.arange(64), [o["fg"] + h, o["fg"] + h]])
        groups.append(("fox_q_%d" % i, c))
    for hp in range(nh // 2):
        ha, hb = heads[2 * hp], heads[2 * hp + 1]
        c = np.concatenate([o["fox_k"] + ha * 64 + np.arange(64), o["fox_k"] + hb * 64 + np.arange(64)])
        groups.append(("fox_k_%d" % hp, c))
    groups.append(("cq0", o["cq"] + np.arange(128)))
    groups.append(("cq1_krA", np.concatenate([o["cq"] + 128 + np.arange(64), o["kr"] + np.arange(32)])))
    groups.append(("cq1_krB", np.concatenate([o["cq"] + 128 + np.arange(64), o["kr"] + perm32])))
    groups.append(("ckv", o["ckv"] + np.arange(128)))
    vcols = []
    for m in ("moba", "fox", "dil"):
        for h in heads:
            vcols.append(o["%s_v" % m] + h * 64 + np.arange(64))
    groups.append(("V", np.concatenate(vcols)))
    return groups


def make_consts(S):
    c = {}
    c["c_ident"] = np.eye(128, dtype=np.float32)
    kl = np.arange(128)[:, None]
    ql = np.arange(128)[None, :]
    c["c_tri"] = np.where(kl <= ql, 0.0, NEG).astype(np.float32)
    c["c_triu"] = np.where(kl >= ql, 0.0, NEG).astype(np.float32)
    c["c_negall"] = np.full((128, 128), NEG, np.float32)
    oh = np.zeros((32, S), np.float32)
    for j in range(S // 256):
        oh[j, j * 256:(j + 1) * 256] = 1.0
    c["c_onehot"] = oh
    p = np.arange(128)
    invf64 = (10000.0 ** (-np.arange(0, 64, 2, dtype=np.float32) / 64)).astype(np.float32)
    invf32 = (10000.0 ** (-np.arange(0, 32, 2, dtype=np.float32) / 32)).astype(np.float32)
    cols = np.zeros((128, 8), np.float32)
    cols[:, 0] = invf64[p % 32]
    cols[:, 1] = invf32[p % 16]
    cols[:, 2] = np.where((p % 64) < 32, -1.0, 1.0)
    cols[:, 3] = np.where((p % 32) < 16, -1.0, 1.0)
    cols[:, 4] = -(p % 2).astype(np.float32)
    cols[:, 5] = EPS
    cols[:, 6] = np.pi / 2
    cols[:, 7] = 1.0
    c["c_cols"] = cols
    return c


class Ctx:
    pass


def build_program(S, depth, NH, HS=1, dbg=None, ncores=8):
    assert S % 2048 == 0
    nc = bass.Bass("TRN2", target_bir_lowering=False)
    heads = list(range(NH))
    groups = make_groups(heads)
    goff = {}
    off = 0
    for name, cols in groups:
        goff[name] = (off, len(cols))
        off += len(cols)
    NA = off
    NT = S // 128
    NQ = S // 512
    NB = S // 256
    SO = S // HS
    NTO = SO // 128
    NQO = SO // 512
    NM = NH * 64
    PAIRS = [[2 * i, 2 * i + 1] for i in range(ncores // 2)]
    st = ExitStack()
    with st:
        P = Prog(nc, st)
        C = Ctx()
        x_in = P.dram("x", [S, D], F32, kind="ExternalInput")
        x_own = P.dram("x_own", [SO, D], F32, kind="ExternalInput") if HS > 1 else x_in
        sel_in = P.dram("sel", [128, 2], F32, kind="ExternalInput")
        pos_in = P.dram("pos", [1, S], I32, kind="ExternalInput")
        wA = P.dram("wA", [depth, D, NA], F32, kind="ExternalInput")
        wqu = P.dram("wqu", [depth, 192, NH * 192], F32, kind="ExternalInput")
        wkvu = P.dram("wkvu", [depth, 128, NH * 128], F32, kind="ExternalInput")
        wout = P.dram("wout", [depth, D, D], F32, kind="ExternalInput")
        wg = P.dram("wg", [depth, D, DFF], F32, kind="ExternalInput")
        wu = P.dram("wu", [depth, D, DFF], F32, kind="ExternalInput")
        wd = P.dram("wd", [depth, DFF, D], F32, kind="ExternalInput")
        gv = P.dram("gv", [depth, 4, D], F32, kind="ExternalInput")
        gq = P.dram("gq", [depth, 192, 1], F32, kind="ExternalInput")
        gkv = P.dram("gkv", [depth, 128, 1], F32, kind="ExternalInput")
        bfg = P.dram("bfg", [depth, NH, 1], F32, kind="ExternalInput")
        c_ident = P.dram("c_ident", [128, 128], F32, kind="ExternalInput")
        c_tri = P.dram("c_tri", [128, 128], F32, kind="ExternalInput")
        c_triu = P.dram("c_triu", [128, 128], F32, kind="ExternalInput")
        c_negall = P.dram("c_negall", [128, 128], F32, kind="ExternalInput")
        c_onehot = P.dram("c_onehot", [32, S], F32, kind="ExternalInput")
        c_cols = P.dram("c_cols", [128, 8], F32, kind="ExternalInput")
        out_d = P.dram("out", [SO, D], F32, kind="ExternalOutput")
        hTf = P.dram("hTf", [4 * HS * 256, SO], BF16)
        hTown = P.dram("hTown", [4 * 256, SO], BF16) if HS > 1 else hTf
        xs = P.dram("xs", [SO, D], F32)
        tabs = P.dram("tabs", [4, 128, S], F32)
        QT = {m: P.dram("QT_" + m, [NH, 96 if m == "mla" else 64, S], BF16) for m in ("moba", "fox", "mla", "dil")}
        KT = {m: P.dram("KT_" + m, [NH, 96 if m == "mla" else 64, S], BF16) for m in ("moba", "fox", "mla", "dil")}
        VV = {m: P.dram("V_" + m, [S, NH, 65], BF16) for m in ("moba", "fox", "mla", "dil")}
        FG = P.dram("FG", [NH, 2, S], F32)
        mixX = [P.dram("mixX%d" % j, [4 * NM, SO], BF16) for j in range(HS)]
        mixG = [P.dram("mixG%d" % j, [HS * 4 * NM, SO], BF16) for j in range(HS)] if HS > 1 else mixX
        h2T = P.dram("h2T", [128, 8, SO], BF16)
        dbg_out = None
        if dbg:
            dbg_out = P.dram("dbg", [D, SO], F32, kind="ExternalOutput")

        ident = P.sbuf(st, "ident", [128, 128], BF16)
        tri = P.sbuf(st, "tri", [128, 128], BF16)
        triu = P.sbuf(st, "triu", [128, 128], BF16)
        negall = P.sbuf(st, "negall", [128, 128], BF16)
        cols = P.sbuf(st, "cols", [128, 8], F32)
        ones_f = P.sbuf(st, "ones_f", [128, 128], F32)
        for sb, dr in ((ident, c_ident), (tri, c_tri), (triu, c_triu), (negall, c_negall)):
            P.dma("pool", sb[:, :], dr[:, :], reads=[dr], writes=[sb], slot=sb)
        P.dma("sp", cols[:, :], c_cols[:, :], reads=[c_cols], writes=[cols], slot=cols)
        P.op("dve", lambda e: e.memset(ones_f[:, :], 1.0), writes=[ones_f])
        selc = P.sbuf(st, "selc", [128, 2], F32)
        P.dma("sp", selc[:, :], sel_in[:, :], reads=[sel_in], writes=[selc], slot=selc)

        def hT_tile(n):
            r, ln = n // NQO, n % NQO
            v = hTf[:, ln * 512:(ln + 1) * 512].rearrange("(c r kk p) t -> r p c kk t", c=4, r=HS, kk=2)
            return v[r]

        def collective(src, dst, src_ap, dst_ap):
            sem = P._new_sem("cc%d" % P.nsem)
            P._wait("pool", P._deps("pool", [src], []))
            ins = nc.gpsimd.collective_compute("AllGather", ALU.bypass, replica_groups=PAIRS, ins=[src_ap.opt()], outs=[dst_ap.opt()])
            ins.then_inc(sem)
            P.ninstr += 1
            tok = (sem, 1)
            P._post(tok, [src], [dst])
        EPSC = lambda n=128, b=0: cols[b:b + n, 5:6]

        def stage_tables():
            with ExitStack() as s2:
                posi = P.sbuf(s2, "posi", [128, 512], I32)
                posf = P.sbuf(s2, "posf", [128, 512], F32)
                ang = P.sbuf(s2, "ang", [128, 512], F32)
                kf = P.sbuf(s2, "kf", [128, 512], F32)
                ki = P.sbuf(s2, "ki", [128, 512], I32)
                m1 = P.sbuf(s2, "m1", [128, 512], F32)
                res = [P.sbuf(s2, "tres%d" % i, [128, 512], F32) for i in range(2)]
                for n in range(NQ):
                    sl = slice(n * 512, (n + 1) * 512)
                    P.dma("sp", posi[:, :], pos_in[0:1, sl].to_broadcast([128, 512]), reads=[pos_in], writes=[posi], slot=posi)
                    P.op("dve", lambda e: e.tensor_copy(out=posf[:, :], in_=posi[:, :]), reads=[posi], writes=[posf])
                    for ti in range(2):
                        P.op("dve", lambda e: e.tensor_scalar(out=ang[:, :], in0=posf[:, :], scalar1=cols[:, ti:ti + 1], scalar2=None, op0=ALU.mult),
                             reads=[posf, cols], writes=[ang])
                        P.op("dve", lambda e: e.tensor_scalar(out=kf[:, :], in0=ang[:, :], scalar1=float(1.0 / (2 * np.pi)), scalar2=None, op0=ALU.mult),
                             reads=[ang], writes=[kf])
                        P.op("dve", lambda e: e.tensor_copy(out=ki[:, :], in_=kf[:, :]), reads=[kf], writes=[ki])
                        P.op("dve", lambda e: e.tensor_copy(out=kf[:, :], in_=ki[:, :]), reads=[ki], writes=[kf])
                        P.op("dve", lambda e: e.scalar_tensor_tensor(out=ang[:, :], in0=kf[:, :], scalar=-TWO_PI_HI, in1=ang[:, :], op0=ALU.mult, op1=ALU.add),
                             reads=[kf, ang], writes=[ang])
                        P.op("dve", lambda e: e.scalar_tensor_tensor(out=ang[:, :], in0=kf[:, :], scalar=-TWO_PI_LO, in1=ang[:, :], op0=ALU.mult, op1=ALU.add),
                             reads=[kf, ang], writes=[ang])
                        P.op("dve", lambda e: e.tensor_scalar(out=ang[:, :], in0=ang[:, :], scalar1=float(np.pi), scalar2=float(-np.pi), op0=ALU.min, op1=ALU.max),
                             reads=[ang], writes=[ang])
                        rs = res[0]
                        P.op("act", lambda e: e.activation(out=m1[:, :], in_=ang[:, :], func=AF.Sin), reads=[ang], writes=[m1])
                        P.op("dve", lambda e: e.tensor_scalar(out=rs[:, :], in0=m1[:, :], scalar1=cols[:, 2 + ti:3 + ti], scalar2=None, op0=ALU.mult),
                             reads=[m1, cols], writes=[rs])
                        P.dma("sp", tabs[2 * ti + 1, :, sl], rs[:, :], reads=[rs], writes=[tabs], slot=rs)
                        rc = res[1]
                        P.op("dve", lambda e: e.tensor_scalar(out=m1[:, :], in0=ang[:, :], scalar1=-1.0, scalar2=None, op0=ALU.mult),
                             reads=[ang], writes=[m1])
                        P.op("dve", lambda e: e.tensor_tensor(out=m1[:, :], in0=m1[:, :], in1=ang[:, :], op=ALU.max),
                             reads=[ang, m1], writes=[m1])
                        P.op("act", lambda e: e.activation(out=rc[:, :], in_=m1[:, :], func=AF.Sin, scale=-1.0, bias=cols[:, 6:7]),
                             reads=[m1, cols], writes=[rc])
                        P.dma("sp", tabs[2 * ti, :, sl], rc[:, :], reads=[rc], writes=[tabs], slot=rc)
            P.end_stage()

        def rstd_from_ss(ss, n_feat, rstd, npart=128):
            P.op("act", lambda e: e.activation(out=rstd[0:npart, 0:1], in_=ss[0:npart, 0:1], func=AF.Ln, scale=1.0 / n_feat, bias=EPSC(npart)),
                 reads=[ss, cols], writes=[rstd])
            P.op("act", lambda e: e.activation(out=rstd[0:npart, 0:1], in_=rstd[0:npart, 0:1], func=AF.Exp, scale=-0.5),
                 reads=[rstd], writes=[rstd])

        def transposes_to(hb, dst, dst_sl, pst):
            for half in range(2):
                ps = pst[half]
                for k4 in range(4):
                    k = half * 4 + k4
                    P.op("pe", lambda e: e.matmul(ps[:, k4 * 128:(k4 + 1) * 128], lhsT=hb[:, k * 128:(k + 1) * 128], rhs=ident[:, :], start=True, stop=True),
                         reads=[hb, ident], writes=[ps], inc=(k4 == 3))
                eng = "act" if half == 0 else "dve"
                if eng == "act":
                    P.op("act", lambda e: e.copy(out=dst[:, half * 4:half * 4 + 4, dst_sl], in_=ps[:, :].rearrange("p (k t) -> p k t", k=4)),
                         reads=[ps], writes=[dst])
                else:
                    P.op("dve", lambda e: e.tensor_copy(out=dst[:, half * 4:half * 4 + 4, dst_sl], in_=ps[:, :].rearrange("p (k t) -> p k t", k=4)),
                         reads=[ps], writes=[dst])

        def load_gvec(s2, name, l, idx):
            g = P.sbuf(s2, name, [128, D], F32)
            P.dma("sp", g[:, :], gv[l, idx:idx + 1, :].to_broadcast([128, D]), reads=[gv], writes=[g], slot=g)
            return g

        def stage_h0():
            with ExitStack() as s2:
                g0 = load_gvec(s2, "g0", 0, 0)
                xt = [P.sbuf(s2, "h0x%d" % i, [128, D], F32) for i in range(2)]
                junk = P.sbuf(s2, "h0junk", [128, D], BF16)
                ss = P.sbuf(s2, "h0ss", [128, 1], F32)
                rstd = P.sbuf(s2, "h0rstd", [128, 1], F32)
                hb = [P.sbuf(s2, "h0hb%d" % i, [128, D], BF16) for i in range(2)]
                ho = [P.sbuf(s2, "h0ho%d" % i, [128, 8, 512], BF16) for i in range(2)]
                pst = [P.psum(s2, "h0ps%d" % i) for i in range(4)]
                for t in range(NT):
                    x_t = xt[t % 2]
                    P.dma("sp", x_t[:, :], x_in[t * 128:(t + 1) * 128, :], reads=[x_in], writes=[x_t], slot=x_t)
                    if HS == 1:
                        P.dma("sp", xs[t * 128:(t + 1) * 128, :], x_t[:, :], reads=[x_t], writes=[xs], slot=x_t)
                    P.op("act", lambda e: e.activation(out=junk[:, :], in_=x_t[:, :], func=AF.Square, accum_out=ss[:, 0:1]),
                         reads=[x_t], writes=[junk, ss])
                    rstd_from_ss(ss, D, rstd)
                    h_b = hb[t % 2]
                    P.op("dve", lambda e: e.scalar_tensor_tensor(out=h_b[:, :], in0=x_t[:, :], scalar=rstd[:, 0:1], in1=g0[:, :], op0=ALU.mult, op1=ALU.mult),
                         reads=[x_t, rstd, g0], writes=[h_b])
                    o = ho[(t // 4) % 2]
                    transposes_to(h_b, o, slice((t % 4) * 128, (t % 4 + 1) * 128), pst[2 * (t % 2):2 * (t % 2) + 2])
                    if t % 4 == 3:
                        n = t // 4
                        [P.dma("sp", hT_tile(n)[:, c_, :, :], o[:, 2 * c_:2 * c_ + 2, :], reads=[o], writes=[hTf], slot=o) for c_ in range(4)]
                if HS > 1:
                    for c in range(0, SO, 1024):
                        P.dma("sp", xs[c:c + 1024, :], x_own[c:c + 1024, :], reads=[x_own], writes=[xs], slot=ss)
            P.end_stage()

        def stage_a(l):
            with ExitStack() as s2:
                wa = P.sbuf(s2, "wa", [128, 8, NA], BF16)
                for k in range(8):
                    P.dma("pool", wa[:, k, :], wA[l, k * 128:(k + 1) * 128, :], reads=[wA], writes=[wa], slot=wa)
                wq = P.sbuf(s2, "wq", [128, 2, NH * 192], BF16)
                P.dma("pool", wq[:, 0, :], wqu[l, 0:128, :], reads=[wqu], writes=[wq], slot=wq)
                P.dma("pool", wq[0:64, 1, :], wqu[l, 128:192, :], reads=[wqu], writes=[wq], slot=wq)
                wk = P.sbuf(s2, "wk", [128, NH * 128], BF16)
                P.dma("pool", wk[:, :], wkvu[l, :, :], reads=[wkvu], writes=[wk], slot=wk)
                gqc = P.sbuf(s2, "gqc", [128, 2], F32)
                P.dma("sp", gqc[:, 0:1], gq[l, 0:128, :], reads=[gq], writes=[gqc], slot=gqc)
                P.dma("sp", gqc[0:64, 1:2], gq[l, 128:192, :], reads=[gq], writes=[gqc], slot=gqc)
                gkc = P.sbuf(s2, "gkc", [128, 1], F32)
                P.dma("sp", gkc[:, :], gkv[l, :, :], reads=[gkv], writes=[gkc], slot=gkc)
                ht = [P.sbuf(s2, "a_ht%d" % i, [128, 8, 512], BF16) for i in range(2)]
                tb = [P.sbuf(s2, "a_tb%d" % i, [128, 4, 512], F32) for i in range(2)]
                ps = [P.psum(s2, "a_ps%d" % i) for i in range(8)]
                NO = 6
                ob = [P.sbuf(s2, "a_ob%d" % i, [128, 512], BF16) for i in range(NO)]
                t1 = [P.sbuf(s2, "a_t1%d" % i, [128, 512], F32) for i in range(2)]
                t2 = [P.sbuf(s2, "a_t2%d" % i, [128, 512], F32) for i in range(2)]
                fgb = [P.sbuf(s2, "a_fg%d" % i, [128, 512], F32) for i in range(2)]
                sq = [P.sbuf(s2, "a_sq%d" % i, [128, 512], F32) for i in range(3)]
                rs = [P.sbuf(s2, "a_rs%d" % i, [128, 512], F32) for i in range(2)]
                cn = [P.sbuf(s2, "a_cn%d" % i, [128, 512], BF16) for i in range(3)]
                vb = {m: [P.sbuf(s2, "a_v%s%d" % (m, i), [128, 4, NH * 65], BF16) for i in range(2)] for m in ("moba", "fox", "dil", "mla")}
                for m in vb:
                    for b_ in vb[m]:
                        P.op("pool", lambda e: e.memset(b_[:, :, :], 1.0), writes=[b_])
                state = {"ob": 0, "ps": 0, "t": 0}

                def next_ps():
                    p_ = ps[state["ps"] % 8]
                    state["ps"] += 1
                    return p_

                def next_ob():
                    o_ = ob[state["ob"] % NO]
                    state["ob"] += 1
                    return o_

                def load_tile(n):
                    h_ = ht[n % 2]
                    [P.dma("sp", h_[:, 2 * c_:2 * c_ + 2, :], hT_tile(n)[:, c_, :, :], reads=[hTf], writes=[h_], slot=h_) for c_ in range(4)]
                    t_ = tb[n % 2]
                    P.dma("sp", t_[:, :, :], tabs[:, :, n * 512:(n + 1) * 512].rearrange("f p t -> p f t"), reads=[tabs], writes=[t_], slot=t_)

                def proj(gname, h_, rows=None):
                    o_, m_ = goff[gname]
                    p_ = next_ps()
                    for k in range(8):
                        P.op("pe", lambda e: e.matmul(p_[0:m_, :], lhsT=wa[:, k, o_:o_ + m_], rhs=h_[:, k, :], start=(k == 0), stop=(k == 7)),
                             reads=[wa, h_], writes=[p_], inc=(k == 7))
                    return p_

                def rope_evac(pa, pb, t_, ti, dst, r0, r1):
                    a1 = t1[state["t"] % 2]
                    a2 = t2[state["t"] % 2]
                    state["t"] += 1
                    P.op("dve", lambda e: e.tensor_tensor(out=a1[r0:r1, :], in0=pa[r0:r1, :], in1=t_[r0:r1, 2 * ti, :], op=ALU.mult),
                         reads=[pa, t_], writes=[a1])
                    P.op("dve", lambda e: e.tensor_tensor(out=a2[r0:r1, :], in0=pb[r0:r1, :], in1=t_[r0:r1, 2 * ti + 1, :], op=ALU.mult),
                         reads=[pb, t_], writes=[a2])
                    P.op("pool", lambda e: e.tensor_tensor(out=dst[r0:r1, :], in0=a1[r0:r1, :], in1=a2[r0:r1, :], op=ALU.add),
                         reads=[a1, a2], writes=[dst])

                def store_pair(o_, dt_, hp, sl, rows=64):
                    dst = dt_[2 * hp:2 * hp + 2, 0:rows, sl].rearrange("h r t -> (h r) t")
                    if rows == 64:
                        P.dma("sp", dst, o_[:, :], reads=[o_], writes=[dt_], slot=o_)
                    else:
                        raise NotImplementedError

                load_tile(0)
                for n in range(NQ):
                    if n + 1 < NQ:
                        load_tile(n + 1)
                    h_ = ht[n % 2]
                    t_ = tb[n % 2]
                    sl = slice(n * 512, (n + 1) * 512)
                    for m in ("moba", "dil"):
                        for tname, dt_ in (("q", QT[m]), ("k", KT[m])):
                            for hp in range(NH // 2):
                                pa = proj("%s_%s_%d_A" % (m, tname, hp), h_)
                                pb = proj("%s_%s_%d_B" % (m, tname, hp), h_)
                                o_ = next_ob()
                                rope_evac(pa, pb, t_, 0, o_, 0, 128)
                                store_pair(o_, dt_, hp, sl)
                    for i in range(NH):
                        pq = proj("fox_q_%d" % i, h_)
                        o_ = next_ob()
                        P.op("act", lambda e: e.copy(out=o_[0:64, :], in_=pq[0:64, :]), reads=[pq], writes=[o_])
                        P.dma("sp", QT["fox"][i, 0:64, sl], o_[0:64, :], reads=[o_], writes=[QT["fox"]], slot=o_)
                        f_ = fgb[i % 2]
                        P.op("dve", lambda e: e.tensor_copy(out=f_[64:66, :], in_=pq[64:66, :]), reads=[pq], writes=[f_])
                        P.dma("sp", FG[i, :, sl], f_[64:66, :], reads=[f_], writes=[FG], slot=f_)
                    for hp in range(NH // 2):
                        pk = proj("fox_k_%d" % hp, h_)
                        o_ = next_ob()
                        P.op("act", lambda e: e.copy(out=o_[:, :], in_=pk[:, :]), reads=[pk], writes=[o_])
                        store_pair(o_, KT["fox"], hp, sl)
                    vo, vm = goff["V"]
                    for sub in range(4):
                        for ci, cw in enumerate((0, 1)):
                            w0 = vo + ci * (vm // 2)
                            wn = vm // 2
                            p_ = next_ps()
                            for k in range(8):
                                P.op("pe", lambda e: e.matmul(p_[:, 0:wn], lhsT=h_[:, k, sub * 128:(sub + 1) * 128], rhs=wa[:, k, w0:w0 + wn], start=(k == 0), stop=(k == 7)),
                                     reads=[wa, h_], writes=[p_], inc=(k == 7))
                            c0 = ci * wn
                            done = 0
                            while done < wn:
                                gcol = c0 + done
                                mi = gcol // (NH * 64)
                                m = ("moba", "fox", "dil")[mi]
                                inm = gcol - mi * NH * 64
                                take = min(wn - done, NH * 64 - inm)
                                hh0 = inm // 64
                                nhh = take // 64
                                vt = vb[m][n % 2]
                                eng = "act" if (sub + ci) % 2 == 0 else "dve"
                                src = p_[:, done:done + take].rearrange("p (h c) -> p h c", c=64)
                                dstv = vt[:, sub, :].rearrange("p (h c) -> p h c", c=65)[:, hh0:hh0 + nhh, 0:64]
                                if eng == "act":
                                    P.op("act", lambda e: e.copy(out=dstv, in_=src), reads=[p_], writes=[vt])
                                else:
                                    P.op("dve", lambda e: e.tensor_copy(out=dstv, in_=src), reads=[p_], writes=[vt])
                                done += take
                    for m in ("moba", "fox", "dil"):
                        vt = vb[m][n % 2]
                        P.dma("sp", VV[m][sl, :, :].rearrange("(s p) h c -> p s (h c)", p=128), vt[:, :, :], reads=[vt], writes=[VV[m]], slot=vt)
                    pcq0 = proj("cq0", h_)
                    pcq1 = proj("cq1_krA", h_)
                    pkb = proj("cq1_krB", h_)
                    pckv = proj("ckv", h_)
                    okr = next_ob()
                    rope_evac(pcq1, pkb, t_, 1, okr, 64, 96)
                    for i in range(NH):
                        P.dma("sp", KT["mla"][i, 64:96, sl], okr[64:96, :], reads=[okr], writes=[KT["mla"]], slot=okr)
                    P.op("act", lambda e: e.activation(out=sq[0][:, :], in_=pcq0[:, :], func=AF.Square), reads=[pcq0], writes=[sq[0]])
                    P.op("act", lambda e: e.activation(out=sq[1][0:64, :], in_=pcq1[0:64, :], func=AF.Square), reads=[pcq1], writes=[sq[1]])
                    P.op("act", lambda e: e.activation(out=sq[2][:, :], in_=pckv[:, :], func=AF.Square), reads=[pckv], writes=[sq[2]])
                    pss = next_ps()
                    P.op("pe", lambda e: e.matmul(pss[:, :], lhsT=ones_f[:, :], rhs=sq[0][:, :], start=True, stop=False), reads=[ones_f, sq[0]], writes=[pss], inc=False)
                    P.op("pe", lambda e: e.matmul(pss[:, :], lhsT=ones_f[0:64, :], rhs=sq[1][0:64, :], start=False, stop=True), reads=[ones_f, sq[1]], writes=[pss])
                    pss2 = next_ps()
                    P.op("pe", lambda e: e.matmul(pss2[:, :], lhsT=ones_f[:, :], rhs=sq[2][:, :], start=True, stop=True), reads=[ones_f, sq[2]], writes=[pss2])
                    for (pp, nf, r_) in ((pss, 192, rs[0]), (pss2, 128, rs[1])):
                        P.op("act", lambda e: e.activation(out=r_[:, :], in_=pp[:, :], func=AF.Ln, scale=1.0 / nf, bias=EPSC()), reads=[pp, cols], writes=[r_])
                        P.op("act", lambda e: e.activation(out=r_[:, :], in_=r_[:, :], func=AF.Exp, scale=-0.5), reads=[r_], writes=[r_])
                    P.op("dve", lambda e: e.scalar_tensor_tensor(out=cn[0][:, :], in0=pcq0[:, :], scalar=gqc[:, 0:1], in1=rs[0][:, :], op0=ALU.mult, op1=ALU.mult),
                         reads=[pcq0, gqc, rs[0]], writes=[cn[0]])
                    P.op("dve", lambda e: e.scalar_tensor_tensor(out=cn[1][0:64, :], in0=pcq1[0:64, :], scalar=gqc[0:64, 1:2], in1=rs[0][0:64, :], op0=ALU.mult, op1=ALU.mult),
                         reads=[pcq1, gqc, rs[0]], writes=[cn[1]])
                    P.op("dve", lambda e: e.scalar_tensor_tensor(out=cn[2][:, :], in0=pckv[:, :], scalar=gkc[:, 0:1], in1=rs[1][:, :], op0=ALU.mult, op1=ALU.mult),
                         reads=[pckv, gkc, rs[1]], writes=[cn[2]])
                    for i in range(NH):
                        pa = next_ps()
                        pb = next_ps()
                        for (pp, c0) in ((pa, i * 192), (pb, i * 192 + 96)):
                            P.op("pe", lambda e: e.matmul(pp[0:96, :], lhsT=wq[:, 0, c0:c0 + 96], rhs=cn[0][:, :], start=True, stop=False), reads=[wq, cn[0]], writes=[pp], inc=False)
                            P.op("pe", lambda e: e.matmul(pp[0:96, :], lhsT=wq[0:64, 1, c0:c0 + 96], rhs=cn[1][0:64, :], start=False, stop=True), reads=[wq, cn[1]], writes=[pp])
                        o_ = next_ob()
                        P.op("act", lambda e: e.copy(out=o_[0:64, :], in_=pa[0:64, :]), reads=[pa], writes=[o_])
                        rope_evac(pa, pb, t_, 1, o_, 64, 96)
                        P.dma("sp", QT["mla"][i, 0:96, sl], o_[0:96, :], reads=[o_], writes=[QT["mla"]], slot=o_)
                        pkn = next_ps()
                        P.op("pe", lambda e: e.matmul(pkn[0:64, :], lhsT=wk[:, i * 128:i * 128 + 64], rhs=cn[2][:, :], start=True, stop=True), reads=[wk, cn[2]], writes=[pkn])
                        o2 = next_ob()
                        P.op("act", lambda e: e.copy(out=o2[0:64, :], in_=pkn[0:64, :]), reads=[pkn], writes=[o2])
                        P.dma("sp", KT["mla"][i, 0:64, sl], o2[0:64, :], reads=[o2], writes=[KT["mla"]], slot=o2)
                    vt = vb["mla"][n % 2]
                    for sub in range(4):
                        pv = next_ps()
                        P.op("pe", lambda e: e.matmul(pv[:, 0:NH * 128], lhsT=cn[2][:, sub * 128:(sub + 1) * 128], rhs=wk[:, :], start=True, stop=True), reads=[wk, cn[2]], writes=[pv])
                        src = pv[:, 0:NH * 128].rearrange("p (h c) -> p h c", c=128)[:, :, 64:128]
                        dstv = vt[:, sub, :].rearrange("p (h c) -> p h c", c=65)[:, :, 0:64]
                        P.op("dve", lambda e: e.tensor_copy(out=dstv, in_=src), reads=[pv], writes=[vt])
                    P.dma("sp", VV["mla"][sl, :, :].rearrange("(s p) h c -> p s (h c)", p=128), vt[:, :, :], reads=[vt], writes=[VV["mla"]], slot=vt)
            P.end_stage()

        def finalize_rows(s2res, src_ps_or_sb, is_psum, ncols, dst_rows, tsl, res):
            osb, rec, bcp, outb = res
            if is_psum:
                P.op("dve", lambda e: e.tensor_copy(out=osb[0:65, 0:ncols], in_=src_ps_or_sb[0:65, 0:ncols]), reads=[src_ps_or_sb], writes=[osb])
                srcb = osb
            else:
                srcb = src_ps_or_sb
            P.op("dve", lambda e: e.reciprocal(out=rec[64:65, 0:ncols], in_=srcb[64:65, 0:ncols]), reads=[srcb], writes=[rec])
            return srcb

        def finalize_part2(srcb, ncols, dst_rows, tsl, res, src_off=0):
            osb, rec, bcp, outb = res
            P.op("pe", lambda e: e.matmul(bcp[0:64, 0:ncols], lhsT=ones_f[64:65, 0:64], rhs=rec[64:65, 0:ncols], start=True, stop=True),
                 reads=[ones_f, rec], writes=[bcp])
            P.op("dve", lambda e: e.tensor_tensor(out=outb[0:64, 0:ncols], in0=srcb[0:64, src_off:src_off + ncols], in1=bcp[0:64, 0:ncols], op=ALU.mult),
                 reads=[srcb, bcp], writes=[outb])
            qi_ = tsl.start // 512
            mx = mixX[qi_ // NQO]
            lt = (qi_ % NQO) * 512
            P.dma("sp", mx[dst_rows, lt:lt + ncols], outb[0:64, 0:ncols], reads=[outb], writes=[mx], slot=outb)

        def stage_causal(l, m):
            mix_base = {"moba": 0, "fox": NM, "mla": 2 * NM}[m]
            RQ = {"moba": 96, "fox": 66, "mla": 96}[m]
            scale = {"moba": 0.125, "fox": 0.125, "mla": 96.0 ** -0.5}[m]
            with ExitStack() as s2:
                vall = P.sbuf(s2, "c_vall", [128, NT, NH * 65], BF16)
                for c in range(0, NT, 8):
                    P.dma("sp", vall[:, c:c + 8, :], VV[m][c * 128:(c + 8) * 128, :, :].rearrange("(s p) h c -> p s (h c)", p=128),
                          reads=[VV[m]], writes=[vall], slot=vall)
                qa = [P.sbuf(s2, "c_qa%d" % i, [128, S], BF16) for i in range(2)]
                ka = [P.sbuf(s2, "c_ka%d" % i, [128, S], BF16) for i in range(2)]
                merge = (m != "fox")
                if merge:
                    NSL = 3
                    pt = [P.sbuf(s2, "c_pt%d" % i, [128, 1024], BF16) for i in range(NSL)]
                    sps = []
                    for i_ in range(NSL):
                        P.uid += 1
                        t_ = s2.enter_context(nc.psum_tensor("c_sd%d_%d" % (i_, P.uid), [128, 1024], F32))
                        b_ = Buf("c_sd%d" % i_, t_, psum=True)
                        P.stage_bufs.append(b_)
                        sps.append(b_)
                    SKEW = 2
                else:
                    NSL = 6
                    pt = [P.sbuf(s2, "c_pt%d" % i, [128, 512], BF16) for i in range(NSL)]
                    sps = [P.psum(s2, "c_s%d" % i) for i in range(NSL)]
                    SKEW = 3
                ops_ = [P.psum(s2, "c_o%d" % i) for i in range(1)]
                mps = [P.psum(s2, "c_m%d" % i) for i in range(1)] * 2
                fres = [(P.sbuf(s2, "c_osb%d" % i, [128, 512], F32), P.sbuf(s2, "c_rec%d" % i, [128, 512], F32), mps[0],
                         P.sbuf(s2, "c_out%d" % i, [128, 512], BF16)) for i in range(2)]
                bias = None
                if m == "fox":
                    nbias = sum(4 * i + 4 for i in range(NQ))
                    bias = P.sbuf(s2, "c_bias", [128, nbias], F32)
                    zc = P.sbuf(s2, "f_z", [128, 2048], F32)
                    lf = P.sbuf(s2, "f_lf", [128, 2048], F32)
                    pc = [P.sbuf(s2, "f_pc%d" % i, [128, 2048], F32) for i in range(2)]
                    cq = P.sbuf(s2, "f_cq", [128, 2048], F32)
                    hb_ = P.sbuf(s2, "f_hb", [128, 2048], BF16)
                    cumcol = P.sbuf(s2, "f_cumcol", [128, NT], F32)
                    rb = P.sbuf(s2, "f_rb", [128, NQ], F32)
                    nb_ = P.sbuf(s2, "f_nb", [128, 1], F32)
                if m == "moba":
                    km = P.sbuf(s2, "m_km", [64, NB], F32)
                    kmh = P.sbuf(s2, "m_kmh", [64, NB], BF16)
                    kml = P.sbuf(s2, "m_kml", [64, NB], BF16)
                    kmt = P.sbuf(s2, "m_kmt", [64, NB], F32)
                    gw = [P.sbuf(s2, "m_gw%d" % i, [128, max(NB, 8)], F32) for i in range(4)]
                    m8 = [P.sbuf(s2, "m_m8%d" % i, [128, 8], F32) for i in range(4)]
                    ind = [P.sbuf(s2, "m_ind%d" % i, [128, NB], F32) for i in range(4)]
                    pen = [P.sbuf(s2, "m_pen%d" % i, [128, 96], BF16) for i in range(4)]
                    for p_ in pen:
                        P.op("pool", lambda e: e.memset(p_[:, :], 0.0), writes=[p_])

                def load_head(i):
                    q_ = qa[i % 2]
                    k_ = ka[i % 2]
                    rq = 64 if m != "mla" else 96
                    P.dma("sp", q_[0:rq, :], QT[m][i, 0:rq, :], reads=[QT[m]], writes=[q_], slot=q_)
                    P.dma("sp", k_[0:rq, :], KT[m][i, 0:rq, :], reads=[KT[m]], writes=[k_], slot=k_)
                    if m == "moba":
                        P.dma("pool", k_[64:96, :], c_onehot[:, :], reads=[c_onehot], writes=[k_], slot=k_)
                    if m == "fox":
                        P.op("pool", lambda e: e.memset(k_[64:66, :], 1.0), writes=[k_])

                def prep_fox(i):
                    q_ = qa[i % 2]
                    P.dma("sp", nb_[64:66, :], bfg[l, i:i + 1, :].to_broadcast([2, 1]), reads=[bfg], writes=[nb_], slot=nb_)
                    P.op("dve", lambda e: e.tensor_scalar(out=nb_[64:66, :], in0=nb_[64:66, :], scalar1=-1.0, scalar2=None, op0=ALU.mult), reads=[nb_], writes=[nb_])
                    prev = None
                    for c in range(S // 2048):
                        csl = slice(c * 2048, (c + 1) * 2048)
                        P.dma("sp", zc[64:66, :], FG[i, :, csl], reads=[FG], writes=[zc], slot=zc)
                        P.op("act", lambda e: e.activation(out=lf[64:66, :], in_=zc[64:66, :], func=AF.Exp, scale=-1.0, bias=nb_[64:66, 0:1]), reads=[zc, nb_], writes=[lf])
                        P.op("act", lambda e: e.activation(out=lf[64:66, :], in_=lf[64:66, :], func=AF.Ln, scale=1.0, bias=cols[64:66, 7:8]), reads=[lf, cols], writes=[lf])
                        p_ = pc[c % 2]
                        init = 0.0 if prev is None else prev[64:66, 2047:2048]
                        rd = [lf, ones_f] + ([prev] if prev is not None else [])
                        P.op("dve", lambda e: e.tensor_tensor_scan(out=p_[64:66, :], data0=ones_f[64:66, 0:1].to_broadcast([2, 2048]), data1=lf[64:66, :],
                                                                   initial=init, op0=ALU.mult, op1=ALU.add), reads=rd, writes=[p_])
                        for qi in range(4):
                            qs = slice(qi * 512, (qi + 1) * 512)
                            P.op("dve", lambda e: e.tensor_scalar(out=cq[64:66, qs], in0=p_[64:66, qs], scalar1=p_[64:66, qi * 512:qi * 512 + 1], scalar2=-1.0,
                                                                  op0=ALU.subtract, op1=ALU.mult), reads=[p_], writes=[cq])
                        P.op("dve", lambda e: e.tensor_scalar(out=cq[64:66, :], in0=cq[64:66, :], scalar1=1.0 / scale, scalar2=None, op0=ALU.mult), reads=[cq], writes=[cq])
                        P.op("dve", lambda e: e.tensor_copy(out=hb_[64:66, :], in_=cq[64:66, :]), reads=[cq], writes=[hb_])
                        P.op("dve", lambda e: e.scalar_tensor_tensor(out=q_[64:66, csl], in0=hb_[64:66, :], scalar=cols[64:66, 4:5], in1=cq[64:66, :], op0=ALU.mult, op1=ALU.add),
                             reads=[hb_, cols, cq], writes=[q_])
                        mp = mps[1]
                        for jj in range(16):
                            P.op("pe", lambda e: e.matmul(mp[:, jj:jj + 1], lhsT=p_[64:65, jj * 128:(jj + 1) * 128], rhs=ones_f[64:65, 0:1], start=True, stop=True),
                                 reads=[p_, ones_f], writes=[mp], inc=(jj == 15))
                        P.op("pe", lambda e: e.matmul(mp[:, 16:20], lhsT=ones_f[64:65, 0:128], rhs=p_[64:65, 0:2048:512], start=True, stop=True),
                             reads=[p_, ones_f], writes=[mp])
                        P.op("dve", lambda e: e.tensor_copy(out=cumcol[:, c * 16:(c + 1) * 16], in_=mp[:, 0:16]), reads=[mp], writes=[cumcol])
                        P.op("dve", lambda e: e.tensor_copy(out=rb[:, c * 4:(c + 1) * 4], in_=mp[:, 16:20]), reads=[mp], writes=[rb])
                        prev = p_
                    bo = 0
                    for qi in range(NQ):
                        nj = 4 * qi + 4
                        P.op("dve", lambda e: e.tensor_scalar(out=bias[:, bo:bo + nj], in0=cumcol[:, 0:nj], scalar1=rb[:, qi:qi + 1], scalar2=None, op0=ALU.subtract),
                             reads=[cumcol, rb], writes=[bias])
                        bo += nj

                def prep_moba(i):
                    q_ = qa[i % 2]
                    k_ = ka[i % 2]
                    P.op("dve", lambda e: e.tensor_reduce(out=km[:, :], in_=k_[0:64, :].rearrange("p (j c) -> p j c", c=256), axis=AX.X, op=ALU.add), reads=[k_], writes=[km])
                    P.op("dve", lambda e: e.tensor_scalar(out=km[:, :], in0=km[:, :], scalar1=1.0 / 256, scalar2=None, op0=ALU.mult), reads=[km], writes=[km])
                    P.op("dve", lambda e: e.tensor_copy(out=kmh[:, :], in_=km[:, :]), reads=[km], writes=[kmh])
                    P.op("dve", lambda e: e.tensor_tensor(out=kmt[:, :], in0=km[:, :], in1=kmh[:, :], op=ALU.subtract), reads=[km, kmh], writes=[kmt])
                    P.op("dve", lambda e: e.tensor_copy(out=kml[:, :], in_=kmt[:, :]), reads=[kmt], writes=[kml])
                    def gate_part(t):
                        qb = t // 2
                        tsl = slice(t * 128, (t + 1) * 128)
                        pn = pen[t % 4]
                        P.op("pool", lambda e: e.memset(pn[:, 64:64 + NB], NEG), writes=[pn])
                        if qb <= 3:
                            P.op("pool", lambda e: e.memset(pn[:, 64:64 + qb + 1], 0.0), writes=[pn])
                        else:
                            mp = sps[t % 3]
                            P.op("pe", lambda e: e.matmul(mp[:, 0:qb], lhsT=q_[0:64, tsl], rhs=kmh[:, 0:qb], start=True, stop=False), reads=[q_, kmh], writes=[mp], inc=False)
                            P.op("pe", lambda e: e.matmul(mp[:, 0:qb], lhsT=q_[0:64, tsl], rhs=kml[:, 0:qb], start=False, stop=True), reads=[q_, kml], writes=[mp])
                            g_ = gw[t % 4]
                            w_ = max(qb, 8)
                            if qb < 8:
                                P.op("dve", lambda e: e.memset(g_[:, 0:8], -1e30), writes=[g_])
                            P.op("dve", lambda e: e.tensor_copy(out=g_[:, 0:qb], in_=mp[:, 0:qb]), reads=[mp], writes=[g_])
                            m_ = m8[t % 4]
                            P.op("dve", lambda e: e.max(out=m_[:, :], in_=g_[:, 0:w_]), reads=[g_], writes=[m_])
                            i_ = ind[t % 4]
                            P.op("dve", lambda e: e.tensor_scalar(out=i_[:, 0:qb], in0=g_[:, 0:qb], scalar1=m_[:, 2:3], scalar2=None, op0=ALU.is_ge), reads=[g_, m_], writes=[i_])
                            P.op("dve", lambda e: e.tensor_scalar(out=pn[:, 64:64 + qb], in0=i_[:, 0:qb], scalar1=-NEG, scalar2=NEG, op0=ALU.mult, op1=ALU.add), reads=[i_], writes=[pn])
                            P.op("pool", lambda e: e.memset(pn[:, 64 + qb:64 + qb + 1], 0.0), writes=[pn])

                    def tr_part(t):
                        tsl = slice(t * 128, (t + 1) * 128)
                        pn = pen[t % 4]
                        mp2 = mps[t % 2]
                        P.op("pe", lambda e: e.matmul(mp2[0:96, 0:128], lhsT=pn[:, 0:96], rhs=ident[:, :], start=True, stop=True), reads=[pn, ident], writes=[mp2])
                        P.op("act", lambda e: e.copy(out=q_[64:96, tsl], in_=mp2[64:96, 0:128]), reads=[mp2], writes=[q_])

                    SK = 3
                    for t in range(NT + SK):
                        if t < NT:
                            gate_part(t)
                        if t >= SK:
                            tr_part(t - SK)

                load_head(0)
                for i in range(NH):
                    if i + 1 < NH:
                        load_head(i + 1)
                    q_ = qa[i % 2]
                    k_ = ka[i % 2]
                    if m == "fox":
                        prep_fox(i)
                    if m == "moba":
                        prep_moba(i)
                    rows = slice(mix_base + i * 64, mix_base + i * 64 + 64)
                    units = []
                    for qi in range(NQ):
                        nj = 4 * qi + 4
                        j = 0
                        while j < nj:
                            o = j - 4 * qi
                            if merge and o < 0 and (j + 1) - 4 * qi < 0:
                                units.append((qi, [j, j + 1], nj))
                                j += 2
                            else:
                                units.append((qi, [j], nj))
                                j += 1
                    bias_off = [sum(4 * a + 4 for a in range(qi)) for qi in range(NQ)]
                    pend = []
                    nu = len(units)
                    vcol = slice(i * 65, i * 65 + 65)

                    def qk(ux):
                        qi, js, nj = units[ux]
                        sp_ = sps[ux % NSL]
                        q0 = qi * 512
                        for bi, j in enumerate(js):
                            base = bi * 512
                            o = j - 4 * qi
                            ksl = slice(j * 128, (j + 1) * 128)
                            last = (bi == len(js) - 1)
                            if o < 0:
                                P.op("pe", lambda e: e.matmul(sp_[:, base:base + 512], lhsT=k_[0:RQ, ksl], rhs=q_[0:RQ, q0:q0 + 512], start=True, stop=True), reads=[k_, q_], writes=[sp_], inc=last)
                            else:
                                c0 = 128 * o
                                P.op("pe", lambda e: e.matmul(sp_[:, c0:c0 + 128], lhsT=ident[:, :], rhs=tri[:, :], start=True, stop=False), reads=[ident, tri], writes=[sp_], inc=False)
                                P.op("pe", lambda e: e.matmul(sp_[:, c0:c0 + 128], lhsT=k_[0:RQ, ksl], rhs=q_[0:RQ, q0 + c0:q0 + c0 + 128], start=False, stop=True),
                                     reads=[k_, q_], writes=[sp_], inc=(o == 3))
                                if o < 3:
                                    P.op("pe", lambda e: e.matmul(sp_[:, c0 + 128:512], lhsT=k_[0:RQ, ksl], rhs=q_[0:RQ, q0 + c0 + 128:q0 + 512], start=True, stop=True),
                                         reads=[k_, q_], writes=[sp_])

                    def ex(ux):
                        qi, js, nj = units[ux]
                        sp_ = sps[ux % NSL]
                        p_ = pt[ux % NSL]
                        if len(js) == 2:
                            P.op("act", lambda e: e.activation(out=p_[:, 0:1024], in_=sp_[:, 0:1024], func=AF.Exp, scale=scale), reads=[sp_], writes=[p_])
                            return
                        j = js[0]
                        o = j - 4 * qi
                        c0 = 0 if o < 0 else 128 * o
                        if bias is not None:
                            bcol = bias_off[qi] + j
                            P.op("act", lambda e: e.activation(out=p_[:, c0:512], in_=sp_[:, c0:512], func=AF.Exp, scale=scale, bias=bias[:, bcol:bcol + 1]),
                                 reads=[sp_, bias], writes=[p_])
                        else:
                            P.op("act", lambda e: e.activation(out=p_[:, c0:512], in_=sp_[:, c0:512], func=AF.Exp, scale=scale), reads=[sp_], writes=[p_])

                    def pv(ux):
                        qi, js, nj = units[ux]
                        p_ = pt[ux % NSL]
                        o_ = ops_[0]
                        for bi, j in enumerate(js):
                            base = bi * 512
                            o = j - 4 * qi
                            c0 = 0 if o < 0 else 128 * o
                            P.op("pe", lambda e: e.matmul(o_[0:65, c0:512], lhsT=vall[:, j, vcol], rhs=p_[:, base + c0:base + 512], start=(j == 0), stop=(j == nj - 1)),
                                 reads=[vall, p_], writes=[o_], inc=(j == nj - 1 or bi == len(js) - 1))
                            if j == nj - 1:
                                res = fres[qi % 2]
                                srcb = finalize_rows(None, o_, True, 512, rows, slice(qi * 512, (qi + 1) * 512), res)
                                pend.append([ux + 10, srcb, qi, res])

                    for ux in range(nu + SKEW):
                        if ux < nu:
                            qk(ux)
                            ex(ux)
                        if ux >= SKEW:
                            pv(ux - SKEW)
                        while pend and (pend[0][0] <= ux or ux == nu + SKEW - 1):
                            _, srcb, qi, res = pend.pop(0)
                            finalize_part2(srcb, 512, rows, slice(qi * 512, (qi + 1) * 512), res)
            P.end_stage()

        def stage_dil(l):
            mix_base = 3 * NM
            scale = 0.125
            with ExitStack() as s2:
                qn = P.sbuf(s2, "d_qn", [64, S], BF16)
                kn = P.sbuf(s2, "d_kn", [64, S], BF16)
                qp = P.sbuf(s2, "d_qp", [64, S], BF16)
                kp = P.sbuf(s2, "d_kp", [64, S], BF16)
                vp = [P.sbuf(s2, "d_vp%d" % i, [128, NT, 65], BF16) for i in range(2)]
                acc = P.sbuf(s2, "d_acc", [128, S], F32)
                pt = [P.sbuf(s2, "d_pt%d" % i, [128, 512], BF16) for i in range(3)]
                sps = [P.psum(s2, "d_s%d" % i) for i in range(3)]
                ops_ = [P.psum(s2, "d_o%d" % i) for i in range(2)]
                mps = [P.psum(s2, "d_m%d" % i) for i in range(1)]
                fres = [(None, P.sbuf(s2, "d_rec%d" % i, [128, 512], F32), mps[0], P.sbuf(s2, "d_out%d" % i, [128, 512], BF16)) for i in range(2)]
                vcnt = 0
                for i in range(NH):
                    P.dma("sp", qn[:, :], QT["dil"][i, 0:64, :], reads=[QT["dil"]], writes=[qn], slot=qn)
                    P.dma("sp", kn[:, :], KT["dil"][i, 0:64, :], reads=[KT["dil"]], writes=[kn], slot=kn)
                    rows = slice(mix_base + i * 64, mix_base + i * 64 + 64)
                    for ci, d in enumerate(DIL_CFG):
                        nbr = S // (128 * d)
                        v_ = vp[vcnt % 2]
                        vcnt += 1
                        vsrc = VV["dil"][:, i, :].rearrange("(b p r) c -> p r b c", p=128, r=d)
                        for r in range(d):
                            P.dma("sp", v_[:, r * nbr:(r + 1) * nbr, :], vsrc[:, r, :, :], reads=[VV["dil"]], writes=[v_], slot=v_)
                        if d == 1:
                            qs_, ks_ = qn, kn
                        else:
                            P.op("dve", lambda e: e.tensor_copy(out=qp[:, :].rearrange("p (r n) -> p r n", r=d), in_=qn[:, :].rearrange("p (n r) -> p r n", r=d)), reads=[qn], writes=[qp])
                            P.op("pool", lambda e: e.tensor_copy(out=kp[:, :].rearrange("p (r n) -> p r n", r=d), in_=kn[:, :].rearrange("p (n r) -> p r n", r=d)), reads=[kn], writes=[kp])
                            qs_, ks_ = qp, kp
                        NG = NT
                        halves = [(g0, half) for g0 in range(0, NG, 4) for half in range(2)]

                        def d_qk(hx):
                            g0, half = halves[hx]
                            sp_ = sps[hx % 3]
                            p_ = pt[hx % 3]
                            for qq in range(2):
                                g = g0 + half * 2 + qq
                                b = g % nbr
                                gsl = slice(g * 128, (g + 1) * 128)
                                for which in range(2):
                                    cs = slice((qq * 2 + which) * 128, (qq * 2 + which + 1) * 128)
                                    if which == 0 and b == 0:
                                        kb, msk = g, negall
                                    elif which == 0:
                                        kb, msk = g - 1, triu
                                    else:
                                        kb, msk = g, tri
                                    P.op("pe", lambda e: e.matmul(sp_[:, cs], lhsT=ident[:, :], rhs=msk[:, :], start=True, stop=False), reads=[ident, msk], writes=[sp_], inc=False)
                                    P.op("pe", lambda e: e.matmul(sp_[:, cs], lhsT=ks_[0:64, kb * 128:(kb + 1) * 128], rhs=qs_[0:64, gsl], start=False, stop=True),
                                         reads=[ks_, qs_], writes=[sp_], inc=(qq == 1 and which == 1))
                            P.op("act", lambda e: e.activation(out=p_[:, :], in_=sp_[:, :], func=AF.Exp, scale=scale), reads=[sp_], writes=[p_])

                        def d_pv(hx):
                            g0, half = halves[hx]
                            p_ = pt[hx % 3]
                            o_ = ops_[(g0 // 4) % 2]
                            for qq in range(2):
                                g = g0 + half * 2 + qq
                                b = g % nbr
                                oc = slice((half * 2 + qq) * 128, (half * 2 + qq + 1) * 128)
                                for which in range(2):
                                    cs = slice((qq * 2 + which) * 128, (qq * 2 + which + 1) * 128)
                                    kb = g if (which == 1 or b == 0) else g - 1
                                    P.op("pe", lambda e: e.matmul(o_[0:65, oc], lhsT=v_[:, kb, :], rhs=p_[:, cs], start=(which == 0), stop=(which == 1)),
                                         reads=[v_, p_], writes=[o_], inc=(which == 1))
                            if half == 1:
                                for qq4 in range(4):
                                    g = g0 + qq4
                                    r, b = g // nbr, g % nbr
                                    if d == 1:
                                        dst = acc[0:65, g * 128:(g + 1) * 128]
                                    else:
                                        st0 = r + d * 128 * b
                                        dst = acc[0:65, st0:st0 + d * 127 + 1:d]
                                    src = o_[0:65, qq4 * 128:(qq4 + 1) * 128]
                                    if ci == 0:
                                        P.op("dve", lambda e: e.tensor_copy(out=dst, in_=src), reads=[o_], writes=[acc])
                                    else:
                                        P.op("dve", lambda e: e.tensor_tensor(out=dst, in0=dst, in1=src, op=ALU.add), reads=[o_, acc], writes=[acc])

                        nh_ = len(halves)
                        for hx in range(nh_ + 1):
                            if hx < nh_:
                                d_qk(hx)
                            if hx >= 1:
                                d_pv(hx - 1)
                    for qi in range(NQ):
                        res = fres[qi % 2]
                        tsl = slice(qi * 512, (qi + 1) * 512)
                        osb, rec, bcp, outb = res
                        P.op("dve", lambda e: e.reciprocal(out=rec[64:65, 0:512], in_=acc[64:65, tsl]), reads=[acc], writes=[rec])
                        finalize_part2(acc, 512, rows, tsl, res, src_off=qi * 512)
            P.end_stage()

        def epilogue(yps, x_t, g_post, g_next, tmp, junk, ssb, hb_, hdst, hdst_sl, tps, want_h):
            ss, rstd, ss2, rstd2 = ssb
            P.op("act", lambda e: e.activation(out=junk[:, 0:512], in_=yps[0][:, :], func=AF.Square, accum_out=ss[:, 0:1]), reads=[yps[0]], writes=[junk, ss])
            P.op("act", lambda e: e.activation(out=junk[:, 512:1024], in_=yps[1][:, :], func=AF.Square, accum_out=ss[:, 1:2]), reads=[yps[1]], writes=[junk, ss])
            P.op("dve", lambda e: e.tensor_tensor(out=ss[:, 0:1], in0=ss[:, 0:1], in1=ss[:, 1:2], op=ALU.add), reads=[ss], writes=[ss])
            rstd_from_ss(ss, D, rstd)
            for hf in range(2):
                P.op("dve", lambda e: e.scalar_tensor_tensor(out=tmp[:, hf * 512:(hf + 1) * 512], in0=yps[hf][:, :], scalar=rstd[:, 0:1], in1=g_post[:, hf * 512:(hf + 1) * 512],
                                                             op0=ALU.mult, op1=ALU.mult), reads=[yps[hf], rstd, g_post], writes=[tmp])
            P.op("dve", lambda e: e.tensor_tensor(out=x_t[:, :], in0=x_t[:, :], in1=tmp[:, :], op=ALU.add), reads=[x_t, tmp], writes=[x_t])
            if want_h:
                P.op("act", lambda e: e.activation(out=junk[:, :], in_=x_t[:, :], func=AF.Square, accum_out=ss2[:, 0:1]), reads=[x_t], writes=[junk, ss2])
                rstd_from_ss(ss2, D, rstd2)
                P.op("dve", lambda e: e.scalar_tensor_tensor(out=hb_[:, :], in0=x_t[:, :], scalar=rstd2[:, 0:1], in1=g_next[:, :], op0=ALU.mult, op1=ALU.mult),
                     reads=[x_t, rstd2, g_next], writes=[hb_])
                transposes_to(hb_, hdst, hdst_sl, tps)

        class EpiPipe:
            def __init__(self, s2, name, g_post, g_next, want_h, tps_pairs, pair=1):
                self.pair = pair
                self.NB = 2 + pair
                self.x = [P.sbuf(s2, "%s_x%d" % (name, i), [128, D], F32) for i in range(self.NB)]
                self.tmp = [P.sbuf(s2, "%s_tmp%d" % (name, i), [128, D], F32) for i in range(self.NB)]
                self.ss = [P.sbuf(s2, "%s_ss%d" % (name, i), [128, 4], F32) for i in range(self.NB)]
                self.s2_ = [P.sbuf(s2, "%s_sq%d" % (name, i), [128, 2], F32) for i in range(self.NB)]
                self.hb = [P.sbuf(s2, "%s_hb%d" % (name, i), [128, D], BF16) for i in range(2 * pair)]
                self.junk = P.sbuf(s2, "%s_junk" % name, [128, D], BF16)
                self.junk2 = [P.sbuf(s2, "%s_junk2%d" % (name, i), [128, D], BF16) for i in range(pair)]
                self.g_post, self.g_next, self.want_h, self.tps_pairs = g_post, g_next, want_h, tps_pairs
                self.pending = []
                self.cnt = 0

            def xbuf(self, t):
                return self.x[t % self.NB]

            def _run_pending(self):
                gens = [self._chain(t_, i_, k_) for k_, (t_, i_) in enumerate(self.pending)]
                self.pending = []
                while gens:
                    for g_ in list(gens):
                        try:
                            next(g_)
                        except StopIteration:
                            gens.remove(g_)

            def push(self, t, yps, info):
                if len(self.pending) >= self.pair:
                    self._run_pending()
                b = t % self.NB
                ss, tmp = self.ss[b], self.tmp[b]
                P.op("act", lambda e: e.activation(out=self.junk[:, 0:512], in_=yps[0][:, :], func=AF.Square, accum_out=ss[:, 0:1]), reads=[yps[0]], writes=[self.junk, ss])
                P.op("act", lambda e: e.activation(out=self.junk[:, 512:1024], in_=yps[1][:, :], func=AF.Square, accum_out=ss[:, 1:2]), reads=[yps[1]], writes=[self.junk, ss])
                for hf in range(2):
                    P.op("dve", lambda e: e.tensor_tensor(out=tmp[:, hf * 512:(hf + 1) * 512], in0=yps[hf][:, :], in1=self.g_post[:, hf * 512:(hf + 1) * 512], op=ALU.mult),
                         reads=[yps[hf], self.g_post], writes=[tmp])
                self.pending.append((t, info))

            def flush(self):
                if self.pending:
                    self._run_pending()

            def _chain(self, t, info, lane):
                b = t % self.NB
                ss, tmp, x_t, sq = self.ss[b], self.tmp[b], self.x[b], self.s2_[b]
                jk = self.junk2[lane]
                P.op("dve", lambda e: e.tensor_tensor(out=ss[:, 2:3], in0=ss[:, 0:1], in1=ss[:, 1:2], op=ALU.add), reads=[ss], writes=[ss])
                yield
                P.op("act", lambda e: e.activation(out=ss[:, 3:4], in_=ss[:, 2:3], func=AF.Ln, scale=1.0 / D, bias=EPSC()), reads=[ss, cols], writes=[ss])
                yield
                P.op("act", lambda e: e.activation(out=ss[:, 3:4], in_=ss[:, 3:4], func=AF.Exp, scale=-0.5), reads=[ss], writes=[ss])
                yield
                P.op("dve", lambda e: e.scalar_tensor_tensor(out=x_t[:, :], in0=tmp[:, :], scalar=ss[:, 3:4], in1=x_t[:, :], op0=ALU.mult, op1=ALU.add),
                     reads=[tmp, ss, x_t], writes=[x_t])
                yield
                if self.want_h:
                    P.op("act", lambda e: e.activation(out=jk[:, :], in_=x_t[:, :], func=AF.Square, accum_out=sq[:, 0:1]), reads=[x_t], writes=[jk, sq])
                    yield
                    P.op("act", lambda e: e.activation(out=sq[:, 1:2], in_=sq[:, 0:1], func=AF.Ln, scale=1.0 / D, bias=EPSC()), reads=[sq, cols], writes=[sq])
                    yield
                    P.op("act", lambda e: e.activation(out=sq[:, 1:2], in_=sq[:, 1:2], func=AF.Exp, scale=-0.5), reads=[sq], writes=[sq])
                    yield
                    h_b = self.hb[self.cnt % len(self.hb)]
                    tp_ = self.tps_pairs[self.cnt % len(self.tps_pairs)]
                    self.cnt += 1
                    P.op("dve", lambda e: e.scalar_tensor_tensor(out=h_b[:, :], in0=x_t[:, :], scalar=sq[:, 1:2], in1=self.g_next[:, :], op0=ALU.mult, op1=ALU.mult),
                         reads=[x_t, sq, self.g_next], writes=[h_b])
                    yield
                    transposes_to(h_b, info["hdst"], info["hsl"], tp_)
                    yield
                info["after"](t, x_t)

        def stage_c1(l):
            with ExitStack() as s2:
                wo = P.sbuf(s2, "wo", [128, 8, D], BF16)
                for k in range(8):
                    P.dma("pool", wo[:, k, :], wout[l, k * 128:(k + 1) * 128, :], reads=[wout], writes=[wo], slot=wo)
                g_post = load_gvec(s2, "c1_gpost", l, 1)
                g_next = load_gvec(s2, "c1_gnext", l, 2)
                mt = [P.sbuf(s2, "c1_mt%d" % i, [128, 8, 512], BF16) for i in range(2)]
                mt2 = [P.sbuf(s2, "c1_mu%d" % i, [128, 8, 512], BF16) for i in range(2)] if HS > 1 else None
                ho = [P.sbuf(s2, "c1_ho%d" % i, [128, 8, 512], BF16) for i in range(2)]
                yps = [P.psum(s2, "c1_y%d" % i) for i in range(4)]
                tps = [P.psum(s2, "c1_t%d" % i) for i in range(4)]
                ep = EpiPipe(s2, "c1e", g_post, g_next, True, [tps[0:2], tps[2:4]], pair=2)

                def load_m(n):
                    m_ = mt[n % 2]
                    P.dma("sp", m_[:, :, :], mixG[0][:, n * 512:(n + 1) * 512].rearrange("(k p) t -> p k t", p=128), reads=[mixG[0]], writes=[m_], slot=m_)
                    if HS > 1:
                        u_ = mt2[n % 2]
                        P.dma("sp", u_[:, :, :], mixG[1][:, n * 512:(n + 1) * 512].rearrange("(k p) t -> p k t", p=128), reads=[mixG[1]], writes=[u_], slot=u_)
                        P.op("dve", lambda e: e.tensor_scalar(out=m_[:, :, :], in0=m_[:, :, :], scalar1=selc[:, 0:1], scalar2=None, op0=ALU.mult), reads=[m_, selc], writes=[m_])
                        P.op("dve", lambda e: e.scalar_tensor_tensor(out=m_[:, :, :], in0=u_[:, :, :], scalar=selc[:, 1:2], in1=m_[:, :, :], op0=ALU.mult, op1=ALU.add),
                             reads=[u_, selc, m_], writes=[m_])

                def after(t, x_t):
                    n, sub = t // 4, t % 4
                    P.dma("sp", xs[t * 128:(t + 1) * 128, :], x_t[:, :], reads=[x_t], writes=[xs], slot=x_t)
                    if sub == 3:
                        o = ho[n % 2]
                        P.dma("sp", h2T[:, :, n * 512:(n + 1) * 512], o[:, :, :], reads=[o], writes=[h2T], slot=o)

                load_m(0)
                for t in range(NTO):
                    n, sub = t // 4, t % 4
                    if sub == 0 and n + 1 < NQO:
                        load_m(n + 1)
                    m_ = mt[n % 2]
                    x_t = ep.xbuf(t)
                    P.dma("sp", x_t[:, :], xs[t * 128:(t + 1) * 128, :], reads=[xs], writes=[x_t], slot=x_t)
                    yp = yps[2 * (t % 2):2 * (t % 2) + 2]
                    for hf in range(2):
                        for k in range(8):
                            P.op("pe", lambda e: e.matmul(yp[hf][:, :], lhsT=m_[:, k, sub * 128:(sub + 1) * 128], rhs=wo[:, k, hf * 512:(hf + 1) * 512], start=(k == 0), stop=(k == 7)),
                                 reads=[m_, wo], writes=[yp[hf]], inc=(k == 7))
                    ep.push(t, yp, {"hdst": ho[n % 2], "hsl": slice(sub * 128, (sub + 1) * 128), "after": after})
                ep.flush()
            P.end_stage()

        def stage_c2(l, last):
            T = 1024
            NTT = SO // T
            NJ = DFF // 128
            with ExitStack() as s2:
                wdn = P.sbuf(s2, "wdn", [128, NJ, D], BF16)
                for j in range(NJ):
                    P.dma("pool", wdn[:, j, :], wd[l, j * 128:(j + 1) * 128, :], reads=[wd], writes=[wdn], slot=wdn)
                g_post = load_gvec(s2, "c2_gpost", l, 3)
                g_next = load_gvec(s2, "c2_gnext", l + 1, 0) if not last else g_post
                h2 = [P.sbuf(s2, "c2_h2%d" % i, [128, 8, T], BF16) for i in range(2)]
                fT = P.sbuf(s2, "c2_fT", [128, NJ, T], BF16)
                NR = 2
                wgr = [P.sbuf(s2, "c2_wg%d" % i, [128, 8, 256], BF16) for i in range(NR)]
                wur = [P.sbuf(s2, "c2_wu%d" % i, [128, 8, 256], BF16) for i in range(NR)]
                sg = [P.sbuf(s2, "c2_sg%d" % i, [128, 512], F32) for i in range(2)]
                ho = [P.sbuf(s2, "c2_ho%d" % i, [128, 8, 512], BF16) for i in range(2)]
                gps = [P.psum(s2, "c2_g%d" % i) for i in range(2)]
                ups = [P.psum(s2, "c2_u%d" % i) for i in range(2)]
                yps = [P.psum(s2, "c2_y%d" % i) for i in range(2)]
                tps = [P.psum(s2, "c2_t%d" % i) for i in range(2)]
                ep = EpiPipe(s2, "c2e", g_post, g_next, not last, [tps])

                def after(t, x_t):
                    dst = out_d if last else xs
                    P.dma("sp", dst[t * 128:(t + 1) * 128, :], x_t[:, :], reads=[x_t], writes=[dst], slot=x_t)
                    if (not last) and t % 4 == 3:
                        n = t // 4
                        o = ho[n % 2]
                        P.dma("sp", hTown[:, n * 512:(n + 1) * 512].rearrange("(k p) t -> p k t", p=128), o[:, :, :], reads=[o], writes=[hTown], slot=o)

                def load_w(jp, slot_i):
                    for (ring, src) in ((wgr, wg), (wur, wu)):
                        r_ = ring[slot_i % NR]
                        P.dma("pool", r_[:, :, :], src[l, :, jp * 256:(jp + 1) * 256].rearrange("(k p) c -> p k c", p=128), reads=[src], writes=[r_], slot=r_)

                def load_h(tt):
                    h_ = h2[tt % 2]
                    P.dma("sp", h_[:, :, :], h2T[:, :, tt * T:(tt + 1) * T], reads=[h2T], writes=[h_], slot=h_)

                load_h(0)
                load_w(0, 0)
                for tt in range(NTT):
                    if tt + 1 < NTT:
                        load_h(tt + 1)
                    h_ = h2[tt % 2]
                    for jp in range(NJ // 2):
                        nxt = (tt * (NJ // 2) + jp + 1)
                        if nxt < NTT * (NJ // 2):
                            load_w(nxt % (NJ // 2), nxt)
                        cur = tt * (NJ // 2) + jp
                        wg_, wu_ = wgr[cur % NR], wur[cur % NR]
                        for jj in range(2):
                            j = jp * 2 + jj
                            for hf in range(T // 512):
                                gp = gps[(j * 2 + hf) % 2]
                                up = ups[(j * 2 + hf) % 2]
                                for (pp, w_) in ((gp, wg_), (up, wu_)):
                                    for k in range(8):
                                        P.op("pe", lambda e: e.matmul(pp[:, :], lhsT=w_[:, k, jj * 128:(jj + 1) * 128], rhs=h_[:, k, hf * 512:(hf + 1) * 512], start=(k == 0), stop=(k == 7)),
                                             reads=[w_, h_], writes=[pp], inc=(k == 7))
                                s_ = sg[(j * 2 + hf) % 2]
                                P.op("act", lambda e: e.activation(out=s_[:, :], in_=gp[:, :], func=AF.Silu), reads=[gp], writes=[s_])
                                P.op("dve", lambda e: e.tensor_tensor(out=fT[:, j, hf * 512:(hf + 1) * 512], in0=s_[:, :], in1=up[:, :], op=ALU.mult), reads=[s_, up], writes=[fT])
                    for sub in range(T // 128):
                        t = tt * (T // 128) + sub
                        x_t = ep.xbuf(t)
                        P.dma("sp", x_t[:, :], xs[t * 128:(t + 1) * 128, :], reads=[xs], writes=[x_t], slot=x_t)
                        for hf in range(2):
                            for j in range(NJ):
                                P.op("pe", lambda e: e.matmul(yps[hf][:, :], lhsT=fT[:, j, sub * 128:(sub + 1) * 128], rhs=wdn[:, j, hf * 512:(hf + 1) * 512], start=(j == 0), stop=(j == NJ - 1)),
                                     reads=[fT, wdn], writes=[yps[hf]], inc=(j == NJ - 1))
                        ep.push(t, yps, {"hdst": ho[(t // 4) % 2], "hsl": slice((t % 4) * 128, (t % 4 + 1) * 128), "after": after})
                ep.flush()
            P.end_stage()
            if HS > 1 and not last:
                for c in range(4):
                    collective(hTown, hTf, hTown[c * 256:(c + 1) * 256, :], hTf[c * HS * 256:(c + 1) * HS * 256, :])

        def dump_bf16(src, rows):
            with ExitStack() as s2:
                a = P.sbuf(s2, "dbg_a", [128, SO], BF16)
                b = P.sbuf(s2, "dbg_b", [128, SO], F32)
                for r0 in range(0, rows, 128):
                    P.dma("sp", a[:, :], src[r0:r0 + 128, :], reads=[src], writes=[a], slot=a)
                    P.op("dve", lambda e: e.tensor_copy(out=b[:, :], in_=a[:, :]), reads=[a], writes=[b])
                    P.dma("sp", dbg_out[r0:r0 + 128, :], b[:, :], reads=[b], writes=[dbg_out], slot=b)
            P.end_stage()

        P.end_stage()
        stage_tables()
        stage_h0()
        for l in range(depth):
            stage_a(l)
            def gather_mix(m_):
                if HS > 1:
                    for j in range(HS):
                        collective(mixX[j], mixG[j], mixX[j][m_ * NM:(m_ + 1) * NM, :], mixG[j][m_ * HS * NM:(m_ + 1) * HS * NM, :])
            stage_causal(l, "moba")
            gather_mix(0)
            stage_causal(l, "fox")
            gather_mix(1)
            stage_causal(l, "mla")
            gather_mix(2)
            stage_dil(l)
            gather_mix(3)
            if dbg == "mix" and l == 0:
                dump_bf16(mixG[0], HS * 4 * NM)
            stage_c1(l)
            stage_c2(l, l == depth - 1)
        P.barrier()
        print("program built: instrs=%d sems=%d" % (P.ninstr, P.nsem))
    return nc, groups


_CACHE = {}


def prepare_weights(inp, depth, heads, groups):
    w_in = np.asarray(inp["w_in"], np.float32)
    NH = len(heads)
    cols = np.concatenate([c for _, c in groups]).astype(np.int64)
    wA = np.ascontiguousarray(w_in[:depth][:, :, cols])
    perm32 = np.concatenate([np.arange(16, 32), np.arange(0, 16)])
    wq = np.asarray(inp["w_mla_q_up"], np.float32)[:depth]
    qcols = []
    for h in heads:
        qcols.append(h * 96 + np.arange(96))
        qcols.append(np.concatenate([h * 96 + np.arange(64), h * 96 + 64 + perm32]))
    wqu = np.ascontiguousarray(wq[:, :, np.concatenate(qcols)])
    wkv = np.asarray(inp["w_mla_kv_up"], np.float32)[:depth]
    kcols = np.concatenate([h * 128 + np.arange(128) for h in heads])
    wkvu = np.ascontiguousarray(wkv[:, :, kcols])
    gvv = np.stack([np.asarray(inp[k], np.float32)[:depth] for k in ("g_pre_mix", "g_post_mix", "g_pre_ffn", "g_post_ffn")], axis=1)
    d = {
        "wA": wA, "wqu": wqu, "wkvu": wkvu,
        "wout": np.ascontiguousarray(np.asarray(inp["w_out"], np.float32)[:depth]),
        "wg": np.ascontiguousarray(np.asarray(inp["w_gate"], np.float32)[:depth]),
        "wu": np.ascontiguousarray(np.asarray(inp["w_up"], np.float32)[:depth]),
        "wd": np.ascontiguousarray(np.asarray(inp["w_down"], np.float32)[:depth]),
        "gv": np.ascontiguousarray(gvv),
        "gq": np.ascontiguousarray(np.asarray(inp["g_mla_q"], np.float32)[:depth, :, None]),
        "gkv": np.ascontiguousarray(np.asarray(inp["g_mla_kv"], np.float32)[:depth, :, None]),
        "bfg": np.ascontiguousarray(np.asarray(inp["b_forget"], np.float32)[:depth][:, heads, None]),
    }
    return d


def run(inp, S, depth, B, dbg=None, HS=2):
    NH = 4 // HS
    ncores = B * HS
    key = (S, depth, NH, HS, ncores, dbg)
    if key not in _CACHE:
        _CACHE[key] = build_program(S, depth, NH, HS, dbg, ncores)
    nc, _ = _CACHE[key]
    consts = make_consts(S)
    x = np.asarray(inp["x"], np.float32)
    pos = np.asarray(inp["positions"], np.int32)
    SO = S // HS
    perm = []
    for m in range(4):
        for r in range(HS):
            for i in range(NH):
                perm.append(m * 256 + (r * NH + i) * 64 + np.arange(64))
    perm = np.concatenate(perm)
    per_half = []
    for hf in range(HS):
        heads = [hf * NH + i for i in range(NH)]
        wd_ = prepare_weights(inp, depth, heads, make_groups(heads))
        wd_["wout"] = np.ascontiguousarray(wd_["wout"][:, perm, :])
        sel = np.zeros((128, 2), np.float32)
        sel[:, hf] = 1.0
        wd_["sel"] = sel
        per_half.append(wd_)
    in_maps = []
    for b in range(B):
        for hf in range(HS):
            m = dict(per_half[hf])
            m.update(consts)
            m["x"] = np.ascontiguousarray(x[b])
            if HS > 1:
                m["x_own"] = np.ascontiguousarray(x[b, hf * SO:(hf + 1) * SO])
            m["pos"] = np.ascontiguousarray(pos[b][None, :])
            in_maps.append(m)
    res = run_bass_kernel_spmd(nc, in_maps, core_ids=list(range(ncores)))
    out = np.empty((B, S, D), np.float32)
    for b in range(B):
        for hf in range(HS):
            out[b, hf * SO:(hf + 1) * SO] = np.asarray(res.results[b * HS + hf]["out"], np.float32)
    if dbg:
        return out, [np.asarray(r["dbg"]) for r in res.results]
    return out


def kernel(**inputs):
    return run(inputs, 8192, 4, 4, HS=2)
```

```python
from contextlib import ExitStack
import numpy as np
import concourse.bass as bass
import concourse.mybir as mybir
from concourse.bass_utils import run_bass_kernel_spmd

F32 = mybir.dt.float32
BF16 = mybir.dt.bfloat16
I32 = mybir.dt.int32
AF = mybir.ActivationFunctionType
ALU = mybir.AluOpType
AX = mybir.AxisListType

D = 1024
DFF = 2816
NEG = -30000.0
EPS = 1e-6
TWO_PI_HI = 6.28125
TWO_PI_LO = 2.0 * np.pi - 6.28125
DIL_CFG = (1, 4, 16)


class Buf:
    def __init__(self, name, t=None, psum=False, multi=False):
        self.name = name
        self.t = t
        self.psum = psum
        self.multi = multi
        self.w = {}
        self.r = {}
        self.rec = None

    def __getitem__(self, idx):
        return self.t[idx]


class SemRec:
    def __init__(self, sem):
        self.sem = sem
        self.n = 0


class Prog:
    def __init__(self, nc, stack):
        self.nc = nc
        self.stack = stack
        self.engs = {"pe": nc.tensor, "act": nc.scalar, "dve": nc.vector, "pool": nc.gpsimd, "sp": nc.sync}
        self.esem = {}
        self.ecnt = {}
        self.waited = {}
        self.nsem = 0
        for k in self.engs:
            self.esem[k] = self._new_sem("e_" + k)
            self.ecnt[k] = 0
            self.waited[k] = {}
        self.pool = []
        self.recs = []
        self.stage_bufs = []
        self.ninstr = 0

    def _new_sem(self, name):
        s = self.stack.enter_context(self.nc.semaphore(name))
        self.nsem += 1
        return s

    def take_rec(self):
        if self.pool:
            return self.pool.pop()
        r = SemRec(self._new_sem("d%d" % len(self.recs)))
        self.recs.append(r)
        return r

    def sbuf(self, st, name, shape, dtype):
        self.uid = getattr(self, "uid", 0) + 1
        name = "%s_%d" % (name, self.uid)
        t = st.enter_context(self.nc.sbuf_tensor(name, list(shape), dtype))
        b = Buf(name, t)
        self.stage_bufs.append(b)
        return b

    def psum(self, st, name):
        self.uid = getattr(self, "uid", 0) + 1
        name = "%s_%d" % (name, self.uid)
        t = st.enter_context(self.nc.psum_tensor(name, [128, 512], F32))
        b = Buf(name, t, psum=True)
        self.stage_bufs.append(b)
        return b

    def dram(self, name, shape, dtype, kind="Internal"):
        t = self.nc.dram_tensor(name, list(shape), dtype, kind=kind)
        return Buf(name, t.ap(), multi=True)

    def _wait(self, e, deps):
        eng = self.engs[e]
        w = self.waited[e]
        best = {}
        for (sem, val) in deps:
            k = id(sem)
            if k not in best or best[k][1] < val:
                best[k] = (sem, val)
        for k, (sem, val) in best.items():
            if sem is self.esem[e] and e == "pe":
                continue
            if w.get(k, 0) >= val:
                continue
            eng.wait_ge(sem, val)
            w[k] = val

    @staticmethod
    def _merge(d, tok):
        k = id(tok[0])
        if k not in d or d[k][1] < tok[1]:
            d[k] = tok

    def _deps(self, e, reads, writes):
        deps = []
        for b in reads:
            deps += list(b.w.values())
            if b.psum:
                deps += [t for t in b.r.values() if t[0] is not self.esem[e]]
        for b in writes:
            if b.multi:
                continue
            deps += list(b.w.values())
            deps += list(b.r.values())
        return deps

    def _post(self, tok, reads, writes):
        for b in reads:
            self._merge(b.r, tok)
        for b in writes:
            if b.multi:
                self._merge(b.w, tok)
            else:
                b.w = {id(tok[0]): tok}
                b.r = {}

    def op(self, e, fn, reads=(), writes=(), inc=True):
        self._wait(e, self._deps(e, reads, writes))
        ins = fn(self.engs[e])
        self.ninstr += 1
        if inc:
            self.ecnt[e] += 1
            ins.then_inc(self.esem[e], 1)
            tok = (self.esem[e], self.ecnt[e])
        else:
            tok = (self.esem[e], self.ecnt[e] + 1)
        self._post(tok, reads, writes)
        return tok

    def dma(self, q, out_ap, in_ap, reads, writes, slot, **kw):
        if slot.rec is None:
            slot.rec = self.take_rec()
        self._wait(q, self._deps(q, reads, writes))
        ins = self.engs[q].dma_start(out=out_ap, in_=in_ap, **kw)
        self.ninstr += 1
        slot.rec.n += 1
        ins.then_inc(slot.rec.sem, 16)
        tok = (slot.rec.sem, 16 * slot.rec.n)
        self._post(tok, reads, writes)
        return tok

    def barrier(self):
        toks = []
        for k in self.engs:
            if self.ecnt[k] > 0:
                toks.append((self.esem[k], self.ecnt[k]))
        for r in self.recs:
            if r.n > 0:
                toks.append((r.sem, 16 * r.n))
        for e in self.engs:
            self._wait(e, toks)

    def end_stage(self):
        self.barrier()
        for b in self.stage_bufs:
            if b.rec is not None:
                self.pool.append(b.rec)
                b.rec = None
        self.stage_bufs = []


def w_in_offsets():
    o = {}
    o["moba_q"], o["moba_k"], o["moba_v"] = 0, 256, 512
    o["fox_q"], o["fox_k"], o["fox_v"] = 768, 1024, 1280
    o["fg"] = 1536
    o["cq"] = 1540
    o["ckv"] = 1732
    o["kr"] = 1860
    o["dil_q"], o["dil_k"], o["dil_v"] = 1892, 2148, 2404
    return o


def make_groups(heads):
    o = w_in_offsets()
    nh = len(heads)
    perm64 = np.concatenate([np.arange(32, 64), np.arange(0, 32)])
    perm32 = np.concatenate([np.arange(16, 32), np.arange(0, 16)])
    groups = []
    for m in ("moba", "dil"):
        for t in ("q", "k"):
            for hp in range(nh // 2):
                ha, hb = heads[2 * hp], heads[2 * hp + 1]
                base = o["%s_%s" % (m, t)]
                ca = np.concatenate([base + ha * 64 + np.arange(64), base + hb * 64 + np.arange(64)])
                cb = np.concatenate([base + ha * 64 + perm64, base + hb * 64 + perm64])
                groups.append(("%s_%s_%d_A" % (m, t, hp), ca))
                groups.append(("%s_%s_%d_B" % (m, t, hp), cb))
    for i, h in enumerate(heads):
        c = np.concatenate([o["fox_q"] + h * 64 + np.arange(64), [o["fg"] + h, o["fg"] + h]])
        groups.append(("fox_q_%d" % i, c))
    for hp in range(nh // 2):
        ha, hb = heads[2 * hp], heads[2 * hp + 1]
        c = np.concatenate([o["fox_k"] + ha * 64 + np.arange(64), o["fox_k"] + hb * 64 + np.arange(64)])
        groups.append(("fox_k_%d" % hp, c))
    groups.append(("cq0", o["cq"] + np.arange(128)))
    groups.append(("cq1_krA", np.concatenate([o["cq"] + 128 + np.arange(64), o["kr"] + np.arange(32)])))
    groups.append(("cq1_krB", np.concatenate([o["cq"] + 128 + np.arange(64), o["kr"] + perm32])))
    groups.append(("ckv", o["ckv"] + np.arange(128)))
    vcols = []
    for m in ("moba", "fox", "dil"):
        for h in heads:
            vcols.append(o["%s_v" % m] + h * 64 + np.arange(64))
    groups.append(("V", np.concatenate(vcols)))
    return groups


def make_consts(S):
    c = {}
    c["c_ident"] = np.eye(128, dtype=np.float32)
    kl = np.arange(128)[:, None]
    ql = np.arange(128)[None, :]
    c["c_tri"] = np.where(kl <= ql, 0.0, NEG).astype(np.float32)
    c["c_triu"] = np.where(kl >= ql, 0.0, NEG).astype(np.float32)
    c["c_negall"] = np.full((128, 128), NEG, np.float32)
    oh = np.zeros((32, S), np.float32)
    for j in range(S // 256):
        oh[j, j * 256:(j + 1) * 256] = 1.0
    c["c_onehot"] = oh
    p = np.arange(128)
    invf64 = (10000.0 ** (-np.arange(0, 64, 2, dtype=np.float32) / 64)).astype(np.float32)
    invf32 = (10000.0 ** (-np.arange(0, 32, 2, dtype=np.float32) / 32)).astype(np.float32)
    cols = np.zeros((128, 8), np.float32)
    cols[:, 0] = invf64[p % 32]
    cols[:, 1] = invf32[p % 16]
    cols[:, 2] = np.where((p % 64) < 32, -1.0, 1.0)
    cols[:, 3] = np.where((p % 32) < 16, -1.0, 1.0)
    cols[:, 4] = -(p % 2).astype(np.float32)
    cols[:, 5] = EPS
    cols[:, 6] = np.pi / 2
    cols[:, 7] = 1.0
    c["c_cols"] = cols
    return c


class Ctx:
    pass


def build_program(S, depth, NH, HS=1, dbg=None, ncores=8):
    assert S % 2048 == 0
    nc = bass.Bass("TRN2", target_bir_lowering=False)
    heads = list(range(NH))
    groups = make_groups(heads)
    goff = {}
    off = 0
    for name, cols in groups:
        goff[name] = (off, len(cols))
        off += len(cols)
    NA = off
    NT = S // 128
    NQ = S // 512
    NB = S // 256
    SO = S // HS
    NTO = SO // 128
    NQO = SO // 512
    NM = NH * 64
    PAIRS = [[2 * i, 2 * i + 1] for i in range(ncores // 2)]
    st = ExitStack()
    with st:
        P = Prog(nc, st)
        C = Ctx()
        x_in = P.dram("x", [S, D], F32, kind="ExternalInput")
        x_own = P.dram("x_own", [SO, D], F32, kind="ExternalInput") if HS > 1 else x_in
        sel_in = P.dram("sel", [128, 2], F32, kind="ExternalInput")
        pos_in = P.dram("pos", [1, S], I32, kind="ExternalInput")
        wA = P.dram("wA", [depth, D, NA], F32, kind="ExternalInput")
        wqu = P.dram("wqu", [depth, 192, NH * 192], F32, kind="ExternalInput")
        wkvu = P.dram("wkvu", [depth, 128, NH * 128], F32, kind="ExternalInput")
        wout = P.dram("wout", [depth, D, D], F32, kind="ExternalInput")
        wg = P.dram("wg", [depth, D, DFF], F32, kind="ExternalInput")
        wu = P.dram("wu", [depth, D, DFF], F32, kind="ExternalInput")
        wd = P.dram("wd", [depth, DFF, D], F32, kind="ExternalInput")
        gv = P.dram("gv", [depth, 4, D], F32, kind="ExternalInput")
        gq = P.dram("gq", [depth, 192, 1], F32, kind="ExternalInput")
        gkv = P.dram("gkv", [depth, 128, 1], F32, kind="ExternalInput")
        bfg = P.dram("bfg", [depth, NH, 1], F32, kind="ExternalInput")
        c_ident = P.dram("c_ident", [128, 128], F32, kind="ExternalInput")
        c_tri = P.dram("c_tri", [128, 128], F32, kind="ExternalInput")
        c_triu = P.dram("c_triu", [128, 128], F32, kind="ExternalInput")
        c_negall = P.dram("c_negall", [128, 128], F32, kind="ExternalInput")
        c_onehot = P.dram("c_onehot", [32, S], F32, kind="ExternalInput")
        c_cols = P.dram("c_cols", [128, 8], F32, kind="ExternalInput")
        out_d = P.dram("out", [SO, D], F32, kind="ExternalOutput")
        hTf = P.dram("hTf", [4 * HS * 256, SO], BF16)
        hTown = P.dram("hTown", [4 * 256, SO], BF16) if HS > 1 else hTf
        xs = P.dram("xs", [SO, D], F32)
        tabs = P.dram("tabs", [4, 128, S], F32)
        QT = {m: P.dram("QT_" + m, [NH, 96 if m == "mla" else 64, S], BF16) for m in ("moba", "fox", "mla", "dil")}
        KT = {m: P.dram("KT_" + m, [NH, 96 if m == "mla" else 64, S], BF16) for m in ("moba", "fox", "mla", "dil")}
        VV = {m: P.dram("V_" + m, [S, NH, 65], BF16) for m in ("moba", "fox", "mla", "dil")}
        FG = P.dram("FG", [NH, 2, S], F32)
        mixX = [P.dram("mixX%d" % j, [4 * NM, SO], BF16) for j in range(HS)]
        mixG = [P.dram("mixG%d" % j, [HS * 4 * NM, SO], BF16) for j in range(HS)] if HS > 1 else mixX
        h2T = P.dram("h2T", [128, 8, SO], BF16)
        dbg_out = None
        if dbg:
            dbg_out = P.dram("dbg", [D, SO], F32, kind="ExternalOutput")

        ident = P.sbuf(st, "ident", [128, 128], BF16)
        tri = P.sbuf(st, "tri", [128, 128], BF16)
        triu = P.sbuf(st, "triu", [128, 128], BF16)
        negall = P.sbuf(st, "negall", [128, 128], BF16)
        cols = P.sbuf(st, "cols", [128, 8], F32)
        ones_f = P.sbuf(st, "ones_f", [128, 128], F32)
        for sb, dr in ((ident, c_ident), (tri, c_tri), (triu, c_triu), (negall, c_negall)):
            P.dma("pool", sb[:, :], dr[:, :], reads=[dr], writes=[sb], slot=sb)
        P.dma("sp", cols[:, :], c_cols[:, :], reads=[c_cols], writes=[cols], slot=cols)
        P.op("dve", lambda e: e.memset(ones_f[:, :], 1.0), writes=[ones_f])
        mask4 = P.sbuf(st, "mask4", [128, 4, 512], BF16)
        for vi, pat in enumerate(((c_triu, c_tri, c_triu, c_tri), (c_negall, c_tri, c_triu, c_tri), (c_triu, c_tri, c_negall, c_tri), (c_negall, c_tri, c_negall, c_tri))):
            for qi_, src_ in enumerate(pat):
                P.dma("pool", mask4[:, vi, qi_ * 128:(qi_ + 1) * 128], src_[:, :], reads=[src_], writes=[mask4], slot=mask4)
        selc = P.sbuf(st, "selc", [128, 2], F32)
        P.dma("sp", selc[:, :], sel_in[:, :], reads=[sel_in], writes=[selc], slot=selc)

        def hT_tile(n):
            r, ln = n // NQO, n % NQO
            v = hTf[:, ln * 512:(ln + 1) * 512].rearrange("(c r kk p) t -> r p c kk t", c=4, r=HS, kk=2)
            return v[r]

        def collective(src, dst, src_ap, dst_ap):
            sem = P._new_sem("cc%d" % P.nsem)
            P._wait("pool", P._deps("pool", [src], []))
            ins = nc.gpsimd.collective_compute("AllGather", ALU.bypass, replica_groups=PAIRS, ins=[src_ap.opt()], outs=[dst_ap.opt()])
            ins.then_inc(sem)
            P.ninstr += 1
            tok = (sem, 1)
            P._post(tok, [src], [dst])
        EPSC = lambda n=128, b=0: cols[b:b + n, 5:6]

        def stage_tables():
            with ExitStack() as s2:
                posi = P.sbuf(s2, "posi", [128, 512], I32)
                posf = P.sbuf(s2, "posf", [128, 512], F32)
                ang = P.sbuf(s2, "ang", [128, 512], F32)
                kf = P.sbuf(s2, "kf", [128, 512], F32)
                ki = P.sbuf(s2, "ki", [128, 512], I32)
                m1 = P.sbuf(s2, "m1", [128, 512], F32)
                res = [P.sbuf(s2, "tres%d" % i, [128, 512], F32) for i in range(2)]
                for n in range(NQ):
                    sl = slice(n * 512, (n + 1) * 512)
                    P.dma("sp", posi[:, :], pos_in[0:1, sl].to_broadcast([128, 512]), reads=[pos_in], writes=[posi], slot=posi)
                    P.op("dve", lambda e: e.tensor_copy(out=posf[:, :], in_=posi[:, :]), reads=[posi], writes=[posf])
                    for ti in range(2):
                        P.op("dve", lambda e: e.tensor_scalar(out=ang[:, :], in0=posf[:, :], scalar1=cols[:, ti:ti + 1], scalar2=None, op0=ALU.mult),
                             reads=[posf, cols], writes=[ang])
                        P.op("dve", lambda e: e.tensor_scalar(out=kf[:, :], in0=ang[:, :], scalar1=float(1.0 / (2 * np.pi)), scalar2=None, op0=ALU.mult),
                             reads=[ang], writes=[kf])
                        P.op("dve", lambda e: e.tensor_copy(out=ki[:, :], in_=kf[:, :]), reads=[kf], writes=[ki])
                        P.op("dve", lambda e: e.tensor_copy(out=kf[:, :], in_=ki[:, :]), reads=[ki], writes=[kf])
                        P.op("dve", lambda e: e.scalar_tensor_tensor(out=ang[:, :], in0=kf[:, :], scalar=-TWO_PI_HI, in1=ang[:, :], op0=ALU.mult, op1=ALU.add),
                             reads=[kf, ang], writes=[ang])
                        P.op("dve", lambda e: e.scalar_tensor_tensor(out=ang[:, :], in0=kf[:, :], scalar=-TWO_PI_LO, in1=ang[:, :], op0=ALU.mult, op1=ALU.add),
                             reads=[kf, ang], writes=[ang])
                        P.op("dve", lambda e: e.tensor_scalar(out=ang[:, :], in0=ang[:, :], scalar1=float(np.pi), scalar2=float(-np.pi), op0=ALU.min, op1=ALU.max),
                             reads=[ang], writes=[ang])
                        rs = res[0]
                        P.op("act", lambda e: e.activation(out=m1[:, :], in_=ang[:, :], func=AF.Sin), reads=[ang], writes=[m1])
                        P.op("dve", lambda e: e.tensor_scalar(out=rs[:, :], in0=m1[:, :], scalar1=cols[:, 2 + ti:3 + ti], scalar2=None, op0=ALU.mult),
                             reads=[m1, cols], writes=[rs])
                        P.dma("sp", tabs[2 * ti + 1, :, sl], rs[:, :], reads=[rs], writes=[tabs], slot=rs)
                        rc = res[1]
                        P.op("dve", lambda e: e.tensor_scalar(out=m1[:, :], in0=ang[:, :], scalar1=-1.0, scalar2=None, op0=ALU.mult),
                             reads=[ang], writes=[m1])
                        P.op("dve", lambda e: e.tensor_tensor(out=m1[:, :], in0=m1[:, :], in1=ang[:, :], op=ALU.max),
                             reads=[ang, m1], writes=[m1])
                        P.op("act", lambda e: e.activation(out=rc[:, :], in_=m1[:, :], func=AF.Sin, scale=-1.0, bias=cols[:, 6:7]),
                             reads=[m1, cols], writes=[rc])
                        P.dma("sp", tabs[2 * ti, :, sl], rc[:, :], reads=[rc], writes=[tabs], slot=rc)
            P.end_stage()

        def rstd_from_ss(ss, n_feat, rstd, npart=128):
            P.op("act", lambda e: e.activation(out=rstd[0:npart, 0:1], in_=ss[0:npart, 0:1], func=AF.Ln, scale=1.0 / n_feat, bias=EPSC(npart)),
                 reads=[ss, cols], writes=[rstd])
            P.op("act", lambda e: e.activation(out=rstd[0:npart, 0:1], in_=rstd[0:npart, 0:1], func=AF.Exp, scale=-0.5),
                 reads=[rstd], writes=[rstd])

        def transposes_to(hb, dst, dst_sl, pst):
            for half in range(2):
                ps = pst[half]
                for k4 in range(4):
                    k = half * 4 + k4
                    P.op("pe", lambda e: e.matmul(ps[:, k4 * 128:(k4 + 1) * 128], lhsT=hb[:, k * 128:(k + 1) * 128], rhs=ident[:, :], start=True, stop=True),
                         reads=[hb, ident], writes=[ps], inc=(k4 == 3))
                eng = "act" if half == 0 else "dve"
                if eng == "act":
                    P.op("act", lambda e: e.copy(out=dst[:, half * 4:half * 4 + 4, dst_sl], in_=ps[:, :].rearrange("p (k t) -> p k t", k=4)),
                         reads=[ps], writes=[dst])
                else:
                    P.op("dve", lambda e: e.tensor_copy(out=dst[:, half * 4:half * 4 + 4, dst_sl], in_=ps[:, :].rearrange("p (k t) -> p k t", k=4)),
                         reads=[ps], writes=[dst])

        def load_gvec(s2, name, l, idx):
            g = P.sbuf(s2, name, [128, D], F32)
            P.dma("sp", g[:, :], gv[l, idx:idx + 1, :].to_broadcast([128, D]), reads=[gv], writes=[g], slot=g)
            return g

        def stage_h0():
            with ExitStack() as s2:
                g0 = load_gvec(s2, "g0", 0, 0)
                xt = [P.sbuf(s2, "h0x%d" % i, [128, D], F32) for i in range(8)]
                junk = [P.sbuf(s2, "h0junk%d" % i, [128, D], BF16) for i in range(4)]
                ss = P.sbuf(s2, "h0ss", [128, 8], F32)
                rstd = P.sbuf(s2, "h0rstd", [128, 8], F32)
                ssl = [P.sbuf(s2, "h0s%d" % i, [128, 2], F32) for i in range(8)]
                hb = [P.sbuf(s2, "h0hb%d" % i, [128, D], BF16) for i in range(4)]
                ho = [P.sbuf(s2, "h0ho%d" % i, [128, 8, 512], BF16) for i in range(2)]
                pst = [P.psum(s2, "h0ps%d" % i) for i in range(8)]

                def h0_tile(t):
                    lane = t % 4
                    x_t = xt[t % 8]
                    sl_ = ssl[t % 8]
                    P.dma("sp", x_t[:, :], x_in[t * 128:(t + 1) * 128, :], reads=[x_in], writes=[x_t], slot=x_t)
                    if HS == 1:
                        P.dma("sp", xs[t * 128:(t + 1) * 128, :], x_t[:, :], reads=[x_t], writes=[xs], slot=x_t)
                    yield
                    P.op("act", lambda e: e.activation(out=junk[lane][:, :], in_=x_t[:, :], func=AF.Square, accum_out=sl_[:, 0:1]),
                         reads=[x_t], writes=[junk[lane], sl_])
                    yield
                    P.op("act", lambda e: e.activation(out=sl_[:, 1:2], in_=sl_[:, 0:1], func=AF.Ln, scale=1.0 / D, bias=EPSC()), reads=[sl_, cols], writes=[sl_])
                    yield
                    P.op("act", lambda e: e.activation(out=sl_[:, 1:2], in_=sl_[:, 1:2], func=AF.Exp, scale=-0.5), reads=[sl_], writes=[sl_])
                    yield
                    h_b = hb[lane]
                    P.op("dve", lambda e: e.scalar_tensor_tensor(out=h_b[:, :], in0=x_t[:, :], scalar=sl_[:, 1:2], in1=g0[:, :], op0=ALU.mult, op1=ALU.mult),
                         reads=[x_t, sl_, g0], writes=[h_b])
                    yield
                    o = ho[(t // 4) % 2]
                    transposes_to(h_b, o, slice(lane * 128, (lane + 1) * 128), pst[2 * lane:2 * lane + 2])
                    yield

                for n in range(NQ):
                    gens = [h0_tile(4 * n + i_) for i_ in range(4)]
                    while gens:
                        for g_ in list(gens):
                            try:
                                next(g_)
                            except StopIteration:
                                gens.remove(g_)
                    o = ho[n % 2]
                    [P.dma("sp", hT_tile(n)[:, c_, :, :], o[:, 2 * c_:2 * c_ + 2, :], reads=[o], writes=[hTf], slot=o) for c_ in range(4)]
                if HS > 1:
                    for c in range(0, SO, 1024):
                        P.dma("sp", xs[c:c + 1024, :], x_own[c:c + 1024, :], reads=[x_own], writes=[xs], slot=ss)
            P.end_stage()

        def stage_a(l):
            with ExitStack() as s2:
                wa = P.sbuf(s2, "wa", [128, 8, NA], BF16)
                for k in range(8):
                    P.dma("pool", wa[:, k, :], wA[l, k * 128:(k + 1) * 128, :], reads=[wA], writes=[wa], slot=wa)
                wq = P.sbuf(s2, "wq", [128, 2, NH * 192], BF16)
                P.dma("pool", wq[:, 0, :], wqu[l, 0:128, :], reads=[wqu], writes=[wq], slot=wq)
                P.dma("pool", wq[0:64, 1, :], wqu[l, 128:192, :], reads=[wqu], writes=[wq], slot=wq)
                wk = P.sbuf(s2, "wk", [128, NH * 128], BF16)
                P.dma("pool", wk[:, :], wkvu[l, :, :], reads=[wkvu], writes=[wk], slot=wk)
                gqc = P.sbuf(s2, "gqc", [128, 2], F32)
                P.dma("sp", gqc[:, 0:1], gq[l, 0:128, :], reads=[gq], writes=[gqc], slot=gqc)
                P.dma("sp", gqc[0:64, 1:2], gq[l, 128:192, :], reads=[gq], writes=[gqc], slot=gqc)
                gkc = P.sbuf(s2, "gkc", [128, 1], F32)
                P.dma("sp", gkc[:, :], gkv[l, :, :], reads=[gkv], writes=[gkc], slot=gkc)
                ht = [P.sbuf(s2, "a_ht%d" % i, [128, 8, 512], BF16) for i in range(2)]
                tb = [P.sbuf(s2, "a_tb%d" % i, [128, 4, 512], F32) for i in range(2)]
                ps = [P.psum(s2, "a_ps%d" % i) for i in range(8)]
                NO = 6
                ob = [P.sbuf(s2, "a_ob%d" % i, [128, 512], BF16) for i in range(NO)]
                t1 = [P.sbuf(s2, "a_t1%d" % i, [128, 512], F32) for i in range(2)]
                t2 = [P.sbuf(s2, "a_t2%d" % i, [128, 512], F32) for i in range(2)]
                fgb = [P.sbuf(s2, "a_fg%d" % i, [128, 512], F32) for i in range(2)]
                sq = [P.sbuf(s2, "a_sq%d" % i, [128, 512], F32) for i in range(3)]
                rs = [P.sbuf(s2, "a_rs%d" % i, [128, 512], F32) for i in range(2)]
                cn = [P.sbuf(s2, "a_cn%d" % i, [128, 512], BF16) for i in range(3)]
                vb = {m: [P.sbuf(s2, "a_v%s%d" % (m, i), [128, 4, NH * 65], BF16) for i in range(2)] for m in ("moba", "fox", "dil", "mla")}
                for m in vb:
                    for b_ in vb[m]:
                        P.op("pool", lambda e: e.memset(b_[:, :, :], 1.0), writes=[b_])
                state = {"ob": 0, "ps": 0, "t": 0}

                def next_ps():
                    p_ = ps[state["ps"] % 8]
                    state["ps"] += 1
                    return p_

                def next_ob():
                    o_ = ob[state["ob"] % NO]
                    state["ob"] += 1
                    return o_

                def load_tile(n):
                    h_ = ht[n % 2]
                    [P.dma("sp", h_[:, 2 * c_:2 * c_ + 2, :], hT_tile(n)[:, c_, :, :], reads=[hTf], writes=[h_], slot=h_) for c_ in range(4)]
                    t_ = tb[n % 2]
                    P.dma("sp", t_[:, :, :], tabs[:, :, n * 512:(n + 1) * 512].rearrange("f p t -> p f t"), reads=[tabs], writes=[t_], slot=t_)

                def proj(gname, h_, rows=None):
                    o_, m_ = goff[gname]
                    p_ = next_ps()
                    for k in range(8):
                        P.op("pe", lambda e: e.matmul(p_[0:m_, :], lhsT=wa[:, k, o_:o_ + m_], rhs=h_[:, k, :], start=(k == 0), stop=(k == 7)),
                             reads=[wa, h_], writes=[p_], inc=(k == 7))
                    return p_

                def rope_evac(pa, pb, t_, ti, dst, r0, r1):
                    a1 = t1[state["t"] % 2]
                    a2 = t2[state["t"] % 2]
                    state["t"] += 1
                    P.op("dve", lambda e: e.tensor_tensor(out=a1[r0:r1, :], in0=pa[r0:r1, :], in1=t_[r0:r1, 2 * ti, :], op=ALU.mult),
                         reads=[pa, t_], writes=[a1])
                    P.op("dve", lambda e: e.tensor_tensor(out=a2[r0:r1, :], in0=pb[r0:r1, :], in1=t_[r0:r1, 2 * ti + 1, :], op=ALU.mult),
                         reads=[pb, t_], writes=[a2])
                    P.op("pool", lambda e: e.tensor_tensor(out=dst[r0:r1, :], in0=a1[r0:r1, :], in1=a2[r0:r1, :], op=ALU.add),
                         reads=[a1, a2], writes=[dst])

                def store_pair(o_, dt_, hp, sl, rows=64):
                    dst = dt_[2 * hp:2 * hp + 2, 0:rows, sl].rearrange("h r t -> (h r) t")
                    if rows == 64:
                        P.dma("sp", dst, o_[:, :], reads=[o_], writes=[dt_], slot=o_)
                    else:
                        raise NotImplementedError

                load_tile(0)
                for n in range(NQ):
                    if n + 1 < NQ:
                        load_tile(n + 1)
                    h_ = ht[n % 2]
                    t_ = tb[n % 2]
                    sl = slice(n * 512, (n + 1) * 512)
                    for m in ("moba", "dil"):
                        for tname, dt_ in (("q", QT[m]), ("k", KT[m])):
                            for hp in range(NH // 2):
                                pa = proj("%s_%s_%d_A" % (m, tname, hp), h_)
                                pb = proj("%s_%s_%d_B" % (m, tname, hp), h_)
                                o_ = next_ob()
                                rope_evac(pa, pb, t_, 0, o_, 0, 128)
                                store_pair(o_, dt_, hp, sl)
                    for i in range(NH):
                        pq = proj("fox_q_%d" % i, h_)
                        o_ = next_ob()
                        P.op("act", lambda e: e.copy(out=o_[0:64, :], in_=pq[0:64, :]), reads=[pq], writes=[o_])
                        P.dma("sp", QT["fox"][i, 0:64, sl], o_[0:64, :], reads=[o_], writes=[QT["fox"]], slot=o_)
                        f_ = fgb[i % 2]
                        P.op("dve", lambda e: e.tensor_copy(out=f_[64:66, :], in_=pq[64:66, :]), reads=[pq], writes=[f_])
                        P.dma("sp", FG[i, :, sl], f_[64:66, :], reads=[f_], writes=[FG], slot=f_)
                    for hp in range(NH // 2):
                        pk = proj("fox_k_%d" % hp, h_)
                        o_ = next_ob()
                        P.op("act", lambda e: e.copy(out=o_[:, :], in_=pk[:, :]), reads=[pk], writes=[o_])
                        store_pair(o_, KT["fox"], hp, sl)
                    vo, vm = goff["V"]
                    for sub in range(4):
                        for ci, cw in enumerate((0, 1)):
                            w0 = vo + ci * (vm // 2)
                            wn = vm // 2
                            p_ = next_ps()
                            for k in range(8):
                                P.op("pe", lambda e: e.matmul(p_[:, 0:wn], lhsT=h_[:, k, sub * 128:(sub + 1) * 128], rhs=wa[:, k, w0:w0 + wn], start=(k == 0), stop=(k == 7)),
                                     reads=[wa, h_], writes=[p_], inc=(k == 7))
                            c0 = ci * wn
                            done = 0
                            while done < wn:
                                gcol = c0 + done
                                mi = gcol // (NH * 64)
                                m = ("moba", "fox", "dil")[mi]
                                inm = gcol - mi * NH * 64
                                take = min(wn - done, NH * 64 - inm)
                                hh0 = inm // 64
                                nhh = take // 64
                                vt = vb[m][n % 2]
                                eng = "act" if (sub + ci) % 2 == 0 else "dve"
                                src = p_[:, done:done + take].rearrange("p (h c) -> p h c", c=64)
                                dstv = vt[:, sub, :].rearrange("p (h c) -> p h c", c=65)[:, hh0:hh0 + nhh, 0:64]
                                if eng == "act":
                                    P.op("act", lambda e: e.copy(out=dstv, in_=src), reads=[p_], writes=[vt])
                                else:
                                    P.op("dve", lambda e: e.tensor_copy(out=dstv, in_=src), reads=[p_], writes=[vt])
                                done += take
                    for m in ("moba", "fox", "dil"):
                        vt = vb[m][n % 2]
                        P.dma("sp", VV[m][sl, :, :].rearrange("(s p) h c -> p s (h c)", p=128), vt[:, :, :], reads=[vt], writes=[VV[m]], slot=vt)
                    pcq0 = proj("cq0", h_)
                    pcq1 = proj("cq1_krA", h_)
                    pkb = proj("cq1_krB", h_)
                    pckv = proj("ckv", h_)
                    okr = next_ob()
                    rope_evac(pcq1, pkb, t_, 1, okr, 64, 96)
                    for i in range(NH):
                        P.dma("sp", KT["mla"][i, 64:96, sl], okr[64:96, :], reads=[okr], writes=[KT["mla"]], slot=okr)
                    P.op("act", lambda e: e.activation(out=sq[0][:, :], in_=pcq0[:, :], func=AF.Square), reads=[pcq0], writes=[sq[0]])
                    P.op("act", lambda e: e.activation(out=sq[1][0:64, :], in_=pcq1[0:64, :], func=AF.Square), reads=[pcq1], writes=[sq[1]])
                    P.op("act", lambda e: e.activation(out=sq[2][:, :], in_=pckv[:, :], func=AF.Square), reads=[pckv], writes=[sq[2]])
                    pss = next_ps()
                    P.op("pe", lambda e: e.matmul(pss[:, :], lhsT=ones_f[:, :], rhs=sq[0][:, :], start=True, stop=False), reads=[ones_f, sq[0]], writes=[pss], inc=False)
                    P.op("pe", lambda e: e.matmul(pss[:, :], lhsT=ones_f[0:64, :], rhs=sq[1][0:64, :], start=False, stop=True), reads=[ones_f, sq[1]], writes=[pss])
                    pss2 = next_ps()
                    P.op("pe", lambda e: e.matmul(pss2[:, :], lhsT=ones_f[:, :], rhs=sq[2][:, :], start=True, stop=True), reads=[ones_f, sq[2]], writes=[pss2])
                    for (pp, nf, r_) in ((pss, 192, rs[0]), (pss2, 128, rs[1])):
                        P.op("act", lambda e: e.activation(out=r_[:, :], in_=pp[:, :], func=AF.Ln, scale=1.0 / nf, bias=EPSC()), reads=[pp, cols], writes=[r_])
                        P.op("act", lambda e: e.activation(out=r_[:, :], in_=r_[:, :], func=AF.Exp, scale=-0.5), reads=[r_], writes=[r_])
                    P.op("dve", lambda e: e.scalar_tensor_tensor(out=cn[0][:, :], in0=pcq0[:, :], scalar=gqc[:, 0:1], in1=rs[0][:, :], op0=ALU.mult, op1=ALU.mult),
                         reads=[pcq0, gqc, rs[0]], writes=[cn[0]])
                    P.op("dve", lambda e: e.scalar_tensor_tensor(out=cn[1][0:64, :], in0=pcq1[0:64, :], scalar=gqc[0:64, 1:2], in1=rs[0][0:64, :], op0=ALU.mult, op1=ALU.mult),
                         reads=[pcq1, gqc, rs[0]], writes=[cn[1]])
                    P.op("dve", lambda e: e.scalar_tensor_tensor(out=cn[2][:, :], in0=pckv[:, :], scalar=gkc[:, 0:1], in1=rs[1][:, :], op0=ALU.mult, op1=ALU.mult),
                         reads=[pckv, gkc, rs[1]], writes=[cn[2]])
                    for i in range(NH):
                        pa = next_ps()
                        pb = next_ps()
                        for (pp, c0) in ((pa, i * 192), (pb, i * 192 + 96)):
                            P.op("pe", lambda e: e.matmul(pp[0:96, :], lhsT=wq[:, 0, c0:c0 + 96], rhs=cn[0][:, :], start=True, stop=False), reads=[wq, cn[0]], writes=[pp], inc=False)
                            P.op("pe", lambda e: e.matmul(pp[0:96, :], lhsT=wq[0:64, 1, c0:c0 + 96], rhs=cn[1][0:64, :], start=False, stop=True), reads=[wq, cn[1]], writes=[pp])
                        o_ = next_ob()
                        P.op("act", lambda e: e.copy(out=o_[0:64, :], in_=pa[0:64, :]), reads=[pa], writes=[o_])
                        rope_evac(pa, pb, t_, 1, o_, 64, 96)
                        P.dma("sp", QT["mla"][i, 0:96, sl], o_[0:96, :], reads=[o_], writes=[QT["mla"]], slot=o_)
                        pkn = next_ps()
                        P.op("pe", lambda e: e.matmul(pkn[0:64, :], lhsT=wk[:, i * 128:i * 128 + 64], rhs=cn[2][:, :], start=True, stop=True), reads=[wk, cn[2]], writes=[pkn])
                        o2 = next_ob()
                        P.op("act", lambda e: e.copy(out=o2[0:64, :], in_=pkn[0:64, :]), reads=[pkn], writes=[o2])
                        P.dma("sp", KT["mla"][i, 0:64, sl], o2[0:64, :], reads=[o2], writes=[KT["mla"]], slot=o2)
                    vt = vb["mla"][n % 2]
                    for sub in range(4):
                        pv = next_ps()
                        P.op("pe", lambda e: e.matmul(pv[:, 0:NH * 128], lhsT=cn[2][:, sub * 128:(sub + 1) * 128], rhs=wk[:, :], start=True, stop=True), reads=[wk, cn[2]], writes=[pv])
                        src = pv[:, 0:NH * 128].rearrange("p (h c) -> p h c", c=128)[:, :, 64:128]
                        dstv = vt[:, sub, :].rearrange("p (h c) -> p h c", c=65)[:, :, 0:64]
                        P.op("dve", lambda e: e.tensor_copy(out=dstv, in_=src), reads=[pv], writes=[vt])
                    P.dma("sp", VV["mla"][sl, :, :].rearrange("(s p) h c -> p s (h c)", p=128), vt[:, :, :], reads=[vt], writes=[VV["mla"]], slot=vt)
            P.end_stage()

        def finalize_rows(s2res, src_ps_or_sb, is_psum, ncols, dst_rows, tsl, res):
            osb, rec, bcp, outb = res
            if is_psum:
                P.op("dve", lambda e: e.tensor_copy(out=osb[0:65, 0:ncols], in_=src_ps_or_sb[0:65, 0:ncols]), reads=[src_ps_or_sb], writes=[osb])
                srcb = osb
            else:
                srcb = src_ps_or_sb
            P.op("dve", lambda e: e.reciprocal(out=rec[64:65, 0:ncols], in_=srcb[64:65, 0:ncols]), reads=[srcb], writes=[rec])
            return srcb

        def finalize_part2(srcb, ncols, dst_rows, tsl, res, src_off=0):
            osb, rec, bcp, outb = res
            P.op("pe", lambda e: e.matmul(bcp[0:64, 0:ncols], lhsT=ones_f[64:65, 0:64], rhs=rec[64:65, 0:ncols], start=True, stop=True),
                 reads=[ones_f, rec], writes=[bcp])
            P.op("dve", lambda e: e.tensor_tensor(out=outb[0:64, 0:ncols], in0=srcb[0:64, src_off:src_off + ncols], in1=bcp[0:64, 0:ncols], op=ALU.mult),
                 reads=[srcb, bcp], writes=[outb])
            qi_ = tsl.start // 512
            mx = mixX[qi_ // NQO]
            lt = (qi_ % NQO) * 512
            P.dma("sp", mx[dst_rows, lt:lt + ncols], outb[0:64, 0:ncols], reads=[outb], writes=[mx], slot=outb)

        def stage_causal(l, m):
            mix_base = {"moba": 0, "fox": NM, "mla": 2 * NM}[m]
            RQ = {"moba": 96, "fox": 66, "mla": 96}[m]
            scale = {"moba": 0.125, "fox": 0.125, "mla": 96.0 ** -0.5}[m]
            with ExitStack() as s2:
                vall = P.sbuf(s2, "c_vall", [128, NT, NH * 65], BF16)
                for c in range(0, NT, 8):
                    P.dma("sp", vall[:, c:c + 8, :], VV[m][c * 128:(c + 8) * 128, :, :].rearrange("(s p) h c -> p s (h c)", p=128),
                          reads=[VV[m]], writes=[vall], slot=vall)
                qa = [P.sbuf(s2, "c_qa%d" % i, [128, S], BF16) for i in range(2)]
                ka = [P.sbuf(s2, "c_ka%d" % i, [128, S], BF16) for i in range(2)]
                merge = (m != "fox")
                if merge:
                    NSL = 3
                    pt = [P.sbuf(s2, "c_pt%d" % i, [128, 1024], BF16) for i in range(NSL)]
                    sps = []
                    for i_ in range(NSL):
                        P.uid += 1
                        t_ = s2.enter_context(nc.psum_tensor("c_sd%d_%d" % (i_, P.uid), [128, 1024], F32))
                        b_ = Buf("c_sd%d" % i_, t_, psum=True)
                        P.stage_bufs.append(b_)
                        sps.append(b_)
                    SKEW = 2
                else:
                    NSL = 6
                    pt = [P.sbuf(s2, "c_pt%d" % i, [128, 512], BF16) for i in range(NSL)]
                    sps = [P.psum(s2, "c_s%d" % i) for i in range(NSL)]
                    SKEW = 3
                ops_ = [P.psum(s2, "c_o%d" % i) for i in range(1)]
                mps = [P.psum(s2, "c_m%d" % i) for i in range(1)] * 2
                fres = [(P.sbuf(s2, "c_osb%d" % i, [128, 512], F32), P.sbuf(s2, "c_rec%d" % i, [128, 512], F32), mps[0],
                         P.sbuf(s2, "c_out%d" % i, [128, 512], BF16)) for i in range(2)]
                bias = None
                if m == "fox":
                    nbias = sum(4 * i + 4 for i in range(NQ))
                    bias = P.sbuf(s2, "c_bias", [128, nbias], F32)
                    zc = P.sbuf(s2, "f_z", [128, 2048], F32)
                    lf = P.sbuf(s2, "f_lf", [128, 2048], F32)
                    pc = [P.sbuf(s2, "f_pc%d" % i, [128, 2048], F32) for i in range(2)]
                    cq = P.sbuf(s2, "f_cq", [128, 2048], F32)
                    hb_ = P.sbuf(s2, "f_hb", [128, 2048], BF16)
                    cumcol = P.sbuf(s2, "f_cumcol", [128, NT], F32)
                    rb = P.sbuf(s2, "f_rb", [128, NQ], F32)
                    nb_ = P.sbuf(s2, "f_nb", [128, 1], F32)
                if m == "moba":
                    km = P.sbuf(s2, "m_km", [64, NB], F32)
                    kmh = P.sbuf(s2, "m_kmh", [64, NB], BF16)
                    kml = P.sbuf(s2, "m_kml", [64, NB], BF16)
                    kmt = P.sbuf(s2, "m_kmt", [64, NB], F32)
                    gw = [P.sbuf(s2, "m_gw%d" % i, [128, max(NB, 8)], F32) for i in range(4)]
                    m8 = [P.sbuf(s2, "m_m8%d" % i, [128, 8], F32) for i in range(4)]
                    ind = [P.sbuf(s2, "m_ind%d" % i, [128, NB], F32) for i in range(4)]
                    pen = [P.sbuf(s2, "m_pen%d" % i, [128, 96], BF16) for i in range(4)]
                    for p_ in pen:
                        P.op("pool", lambda e: e.memset(p_[:, :], 0.0), writes=[p_])

                def load_head(i):
                    q_ = qa[i % 2]
                    k_ = ka[i % 2]
                    rq = 64 if m != "mla" else 96
                    P.dma("sp", q_[0:rq, :], QT[m][i, 0:rq, :], reads=[QT[m]], writes=[q_], slot=q_)
                    P.dma("sp", k_[0:rq, :], KT[m][i, 0:rq, :], reads=[KT[m]], writes=[k_], slot=k_)
                    if m == "moba":
                        P.dma("pool", k_[64:96, :], c_onehot[:, :], reads=[c_onehot], writes=[k_], slot=k_)
                    if m == "fox":
                        P.op("pool", lambda e: e.memset(k_[64:66, :], 1.0), writes=[k_])

                def prep_fox(i):
                    q_ = qa[i % 2]
                    P.dma("sp", nb_[64:66, :], bfg[l, i:i + 1, :].to_broadcast([2, 1]), reads=[bfg], writes=[nb_], slot=nb_)
                    P.op("dve", lambda e: e.tensor_scalar(out=nb_[64:66, :], in0=nb_[64:66, :], scalar1=-1.0, scalar2=None, op0=ALU.mult), reads=[nb_], writes=[nb_])
                    prev = None
                    for c in range(S // 2048):
                        csl = slice(c * 2048, (c + 1) * 2048)
                        P.dma("sp", zc[64:66, :], FG[i, :, csl], reads=[FG], writes=[zc], slot=zc)
                        P.op("act", lambda e: e.activation(out=lf[64:66, :], in_=zc[64:66, :], func=AF.Exp, scale=-1.0, bias=nb_[64:66, 0:1]), reads=[zc, nb_], writes=[lf])
                        P.op("act", lambda e: e.activation(out=lf[64:66, :], in_=lf[64:66, :], func=AF.Ln, scale=1.0, bias=cols[64:66, 7:8]), reads=[lf, cols], writes=[lf])
                        p_ = pc[c % 2]
                        init = 0.0 if prev is None else prev[64:66, 2047:2048]
                        rd = [lf, ones_f] + ([prev] if prev is not None else [])
                        P.op("dve", lambda e: e.tensor_tensor_scan(out=p_[64:66, :], data0=ones_f[64:66, 0:1].to_broadcast([2, 2048]), data1=lf[64:66, :],
                                                                   initial=init, op0=ALU.mult, op1=ALU.add), reads=rd, writes=[p_])
                        for qi in range(4):
                            qs = slice(qi * 512, (qi + 1) * 512)
                            P.op("dve", lambda e: e.tensor_scalar(out=cq[64:66, qs], in0=p_[64:66, qs], scalar1=p_[64:66, qi * 512:qi * 512 + 1], scalar2=-1.0,
                                                                  op0=ALU.subtract, op1=ALU.mult), reads=[p_], writes=[cq])
                        P.op("dve", lambda e: e.tensor_scalar(out=cq[64:66, :], in0=cq[64:66, :], scalar1=1.0 / scale, scalar2=None, op0=ALU.mult), reads=[cq], writes=[cq])
                        P.op("dve", lambda e: e.tensor_copy(out=hb_[64:66, :], in_=cq[64:66, :]), reads=[cq], writes=[hb_])
                        P.op("dve", lambda e: e.scalar_tensor_tensor(out=q_[64:66, csl], in0=hb_[64:66, :], scalar=cols[64:66, 4:5], in1=cq[64:66, :], op0=ALU.mult, op1=ALU.add),
                             reads=[hb_, cols, cq], writes=[q_])
                        mp = mps[1]
                        for jj in range(16):
                            P.op("pe", lambda e: e.matmul(mp[:, jj:jj + 1], lhsT=p_[64:65, jj * 128:(jj + 1) * 128], rhs=ones_f[64:65, 0:1], start=True, stop=True),
                                 reads=[p_, ones_f], writes=[mp], inc=(jj == 15))
                        P.op("pe", lambda e: e.matmul(mp[:, 16:20], lhsT=ones_f[64:65, 0:128], rhs=p_[64:65, 0:2048:512], start=True, stop=True),
                             reads=[p_, ones_f], writes=[mp])
                        P.op("dve", lambda e: e.tensor_copy(out=cumcol[:, c * 16:(c + 1) * 16], in_=mp[:, 0:16]), reads=[mp], writes=[cumcol])
                        P.op("dve", lambda e: e.tensor_copy(out=rb[:, c * 4:(c + 1) * 4], in_=mp[:, 16:20]), reads=[mp], writes=[rb])
                        prev = p_
                    bo = 0
                    for qi in range(NQ):
                        nj = 4 * qi + 4
                        P.op("dve", lambda e: e.tensor_scalar(out=bias[:, bo:bo + nj], in0=cumcol[:, 0:nj], scalar1=rb[:, qi:qi + 1], scalar2=None, op0=ALU.subtract),
                             reads=[cumcol, rb], writes=[bias])
                        bo += nj

                def prep_moba(i):
                    q_ = qa[i % 2]
                    k_ = ka[i % 2]
                    P.op("dve", lambda e: e.tensor_reduce(out=km[:, :], in_=k_[0:64, :].rearrange("p (j c) -> p j c", c=256), axis=AX.X, op=ALU.add), reads=[k_], writes=[km])
                    P.op("dve", lambda e: e.tensor_scalar(out=km[:, :], in0=km[:, :], scalar1=1.0 / 256, scalar2=None, op0=ALU.mult), reads=[km], writes=[km])
                    P.op("dve", lambda e: e.tensor_copy(out=kmh[:, :], in_=km[:, :]), reads=[km], writes=[kmh])
                    P.op("dve", lambda e: e.tensor_tensor(out=kmt[:, :], in0=km[:, :], in1=kmh[:, :], op=ALU.subtract), reads=[km, kmh], writes=[kmt])
                    P.op("dve", lambda e: e.tensor_copy(out=kml[:, :], in_=kmt[:, :]), reads=[kmt], writes=[kml])
                    def gate_part(t):
                        qb = t // 2
                        tsl = slice(t * 128, (t + 1) * 128)
                        pn = pen[t % 4]
                        P.op("pool", lambda e: e.memset(pn[:, 64:64 + NB], NEG), writes=[pn])
                        if qb <= 3:
                            P.op("pool", lambda e: e.memset(pn[:, 64:64 + qb + 1], 0.0), writes=[pn])
                        else:
                            mp = sps[t % 3]
                            P.op("pe", lambda e: e.matmul(mp[:, 0:qb], lhsT=q_[0:64, tsl], rhs=kmh[:, 0:qb], start=True, stop=False), reads=[q_, kmh], writes=[mp], inc=False)
                            P.op("pe", lambda e: e.matmul(mp[:, 0:qb], lhsT=q_[0:64, tsl], rhs=kml[:, 0:qb], start=False, stop=True), reads=[q_, kml], writes=[mp])
                            g_ = gw[t % 4]
                            w_ = max(qb, 8)
                            if qb < 8:
                                P.op("dve", lambda e: e.memset(g_[:, 0:8], -1e30), writes=[g_])
                            P.op("dve", lambda e: e.tensor_copy(out=g_[:, 0:qb], in_=mp[:, 0:qb]), reads=[mp], writes=[g_])
                            m_ = m8[t % 4]
                            P.op("dve", lambda e: e.max(out=m_[:, :], in_=g_[:, 0:w_]), reads=[g_], writes=[m_])
                            i_ = ind[t % 4]
                            P.op("dve", lambda e: e.tensor_scalar(out=i_[:, 0:qb], in0=g_[:, 0:qb], scalar1=m_[:, 2:3], scalar2=None, op0=ALU.is_ge), reads=[g_, m_], writes=[i_])
                            P.op("dve", lambda e: e.tensor_scalar(out=pn[:, 64:64 + qb], in0=i_[:, 0:qb], scalar1=-NEG, scalar2=NEG, op0=ALU.mult, op1=ALU.add), reads=[i_], writes=[pn])
                            P.op("pool", lambda e: e.memset(pn[:, 64 + qb:64 + qb + 1], 0.0), writes=[pn])

                    def tr_part(t):
                        tsl = slice(t * 128, (t + 1) * 128)
                        pn = pen[t % 4]
                        mp2 = mps[t % 2]
                        P.op("pe", lambda e: e.matmul(mp2[0:96, 0:128], lhsT=pn[:, 0:96], rhs=ident[:, :], start=True, stop=True), reads=[pn, ident], writes=[mp2])
                        P.op("act", lambda e: e.copy(out=q_[64:96, tsl], in_=mp2[64:96, 0:128]), reads=[mp2], writes=[q_])

                    SK = 3
                    for t in range(NT + SK):
                        if t < NT:
                            gate_part(t)
                        if t >= SK:
                            tr_part(t - SK)

                load_head(0)
                for i in range(NH):
                    if i + 1 < NH:
                        load_head(i + 1)
                    q_ = qa[i % 2]
                    k_ = ka[i % 2]
                    if m == "fox":
                        prep_fox(i)
                    if m == "moba":
                        prep_moba(i)
                    rows = slice(mix_base + i * 64, mix_base + i * 64 + 64)
                    units = []
                    for qi in range(NQ):
                        nj = 4 * qi + 4
                        j = 0
                        while j < nj:
                            o = j - 4 * qi
                            if merge and o < 0 and (j + 1) - 4 * qi < 0:
                                units.append((qi, [j, j + 1], nj))
                                j += 2
                            else:
                                units.append((qi, [j], nj))
                                j += 1
                    bias_off = [sum(4 * a + 4 for a in range(qi)) for qi in range(NQ)]
                    pend = []
                    nu = len(units)
                    vcol = slice(i * 65, i * 65 + 65)

                    def qk(ux):
                        qi, js, nj = units[ux]
                        sp_ = sps[ux % NSL]
                        q0 = qi * 512
                        for bi, j in enumerate(js):
                            base = bi * 512
                            o = j - 4 * qi
                            ksl = slice(j * 128, (j + 1) * 128)
                            last = (bi == len(js) - 1)
                            if o < 0:
                                P.op("pe", lambda e: e.matmul(sp_[:, base:base + 512], lhsT=k_[0:RQ, ksl], rhs=q_[0:RQ, q0:q0 + 512], start=True, stop=True), reads=[k_, q_], writes=[sp_], inc=last)
                            else:
                                c0 = 128 * o
                                P.op("pe", lambda e: e.matmul(sp_[:, c0:c0 + 128], lhsT=ident[:, :], rhs=tri[:, :], start=True, stop=False), reads=[ident, tri], writes=[sp_], inc=False)
                                P.op("pe", lambda e: e.matmul(sp_[:, c0:c0 + 128], lhsT=k_[0:RQ, ksl], rhs=q_[0:RQ, q0 + c0:q0 + c0 + 128], start=False, stop=True),
                                     reads=[k_, q_], writes=[sp_], inc=(o == 3))
                                if o < 3:
                                    P.op("pe", lambda e: e.matmul(sp_[:, c0 + 128:512], lhsT=k_[0:RQ, ksl], rhs=q_[0:RQ, q0 + c0 + 128:q0 + 512], start=True, stop=True),
                                         reads=[k_, q_], writes=[sp_])

                    def ex(ux):
                        qi, js, nj = units[ux]
                        sp_ = sps[ux % NSL]
                        p_ = pt[ux % NSL]
                        if len(js) == 2:
                            P.op("act", lambda e: e.activation(out=p_[:, 0:1024], in_=sp_[:, 0:1024], func=AF.Exp, scale=scale), reads=[sp_], writes=[p_])
                            return
                        j = js[0]
                        o = j - 4 * qi
                        c0 = 0 if o < 0 else 128 * o
                        if bias is not None:
                            bcol = bias_off[qi] + j
                            P.op("act", lambda e: e.activation(out=p_[:, c0:512], in_=sp_[:, c0:512], func=AF.Exp, scale=scale, bias=bias[:, bcol:bcol + 1]),
                                 reads=[sp_, bias], writes=[p_])
                        else:
                            P.op("act", lambda e: e.activation(out=p_[:, c0:512], in_=sp_[:, c0:512], func=AF.Exp, scale=scale), reads=[sp_], writes=[p_])

                    def pv(ux):
                        qi, js, nj = units[ux]
                        p_ = pt[ux % NSL]
                        o_ = ops_[0]
                        for bi, j in enumerate(js):
                            base = bi * 512
                            o = j - 4 * qi
                            c0 = 0 if o < 0 else 128 * o
                            P.op("pe", lambda e: e.matmul(o_[0:65, c0:512], lhsT=vall[:, j, vcol], rhs=p_[:, base + c0:base + 512], start=(j == 0), stop=(j == nj - 1)),
                                 reads=[vall, p_], writes=[o_], inc=(j == nj - 1 or bi == len(js) - 1))
                            if j == nj - 1:
                                res = fres[qi % 2]
                                srcb = finalize_rows(None, o_, True, 512, rows, slice(qi * 512, (qi + 1) * 512), res)
                                pend.append([ux + 10, srcb, qi, res])

                    for ux in range(nu + SKEW):
                        if ux < nu:
                            qk(ux)
                            ex(ux)
                        if ux >= SKEW:
                            pv(ux - SKEW)
                        while pend and (pend[0][0] <= ux or ux == nu + SKEW - 1):
                            _, srcb, qi, res = pend.pop(0)
                            finalize_part2(srcb, 512, rows, slice(qi * 512, (qi + 1) * 512), res)
            P.end_stage()

        def stage_dil(l):
            mix_base = 3 * NM
            scale = 0.125
            with ExitStack() as s2:
                qn = P.sbuf(s2, "d_qn", [64, S], BF16)
                kn = P.sbuf(s2, "d_kn", [64, S], BF16)
                qp = P.sbuf(s2, "d_qp", [64, S], BF16)
                kp = P.sbuf(s2, "d_kp", [64, S], BF16)
                vp = [P.sbuf(s2, "d_vp%d" % i, [128, NT, 65], BF16) for i in range(2)]
                acc = P.sbuf(s2, "d_acc", [128, S], F32)
                pt = [P.sbuf(s2, "d_pt%d" % i, [128, 512], BF16) for i in range(3)]
                sps = [P.psum(s2, "d_s%d" % i) for i in range(3)]
                ops_ = [P.psum(s2, "d_o%d" % i) for i in range(2)]
                mps = [P.psum(s2, "d_m%d" % i) for i in range(1)]
                fres = [(None, P.sbuf(s2, "d_rec%d" % i, [128, 512], F32), mps[0], P.sbuf(s2, "d_out%d" % i, [128, 512], BF16)) for i in range(2)]
                vcnt = 0
                for i in range(NH):
                    P.dma("sp", qn[:, :], QT["dil"][i, 0:64, :], reads=[QT["dil"]], writes=[qn], slot=qn)
                    P.dma("sp", kn[:, :], KT["dil"][i, 0:64, :], reads=[KT["dil"]], writes=[kn], slot=kn)
                    rows = slice(mix_base + i * 64, mix_base + i * 64 + 64)
                    for ci, d in enumerate(DIL_CFG):
                        nbr = S // (128 * d)
                        v_ = vp[vcnt % 2]
                        vcnt += 1
                        vsrc = VV["dil"][:, i, :].rearrange("(b p r) c -> p r b c", p=128, r=d)
                        for r in range(d):
                            P.dma("sp", v_[:, r * nbr:(r + 1) * nbr, :], vsrc[:, r, :, :], reads=[VV["dil"]], writes=[v_], slot=v_)
                        if d == 1:
                            qs_, ks_ = qn, kn
                        else:
                            P.op("dve", lambda e: e.tensor_copy(out=qp[:, :].rearrange("p (r n) -> p r n", r=d), in_=qn[:, :].rearrange("p (n r) -> p r n", r=d)), reads=[qn], writes=[qp])
                            P.op("pool", lambda e: e.tensor_copy(out=kp[:, :].rearrange("p (r n) -> p r n", r=d), in_=kn[:, :].rearrange("p (n r) -> p r n", r=d)), reads=[kn], writes=[kp])
                            qs_, ks_ = qp, kp
                        NG = NT
                        halves = [(g0, half) for g0 in range(0, NG, 4) for half in range(2)]

                        def d_qk(hx):
                            g0, half = halves[hx]
                            sp_ = sps[hx % 3]
                            p_ = pt[hx % 3]
                            gA, gB = g0 + half * 2, g0 + half * 2 + 1
                            vi = (1 if gA % nbr == 0 else 0) + (2 if gB % nbr == 0 else 0)
                            P.op("pe", lambda e: e.matmul(sp_[:, :], lhsT=ident[:, :], rhs=mask4[:, vi, :], start=True, stop=False, skip_group_check=True), reads=[ident, mask4], writes=[sp_], inc=False)
                            for qq in range(2):
                                g = g0 + half * 2 + qq
                                b = g % nbr
                                gsl = slice(g * 128, (g + 1) * 128)
                                for which in range(2):
                                    cs = slice((qq * 2 + which) * 128, (qq * 2 + which + 1) * 128)
                                    kb = g if (which == 1 or b == 0) else g - 1
                                    P.op("pe", lambda e: e.matmul(sp_[:, cs], lhsT=ks_[0:64, kb * 128:(kb + 1) * 128], rhs=qs_[0:64, gsl], start=False, stop=(qq == 1 and which == 1), skip_group_check=True),
                                         reads=[ks_, qs_], writes=[sp_], inc=(qq == 1 and which == 1))
                            P.op("act", lambda e: e.activation(out=p_[:, :], in_=sp_[:, :], func=AF.Exp, scale=scale), reads=[sp_], writes=[p_])

                        def d_pv(hx):
                            g0, half = halves[hx]
                            p_ = pt[hx % 3]
                            o_ = ops_[(g0 // 4) % 2]
                            for qq in range(2):
                                g = g0 + half * 2 + qq
                                b = g % nbr
                                oc = slice((half * 2 + qq) * 128, (half * 2 + qq + 1) * 128)
                                for which in range(2):
                                    cs = slice((qq * 2 + which) * 128, (qq * 2 + which + 1) * 128)
                                    kb = g if (which == 1 or b == 0) else g - 1
                                    P.op("pe", lambda e: e.matmul(o_[0:65, oc], lhsT=v_[:, kb, :], rhs=p_[:, cs], start=(which == 0), stop=(which == 1)),
                                         reads=[v_, p_], writes=[o_], inc=(which == 1))
                            if half == 1:
                                for qq4 in range(4):
                                    g = g0 + qq4
                                    r, b = g // nbr, g % nbr
                                    if d == 1:
                                        dst = acc[0:65, g * 128:(g + 1) * 128]
                                    else:
                                        st0 = r + d * 128 * b
                                        dst = acc[0:65, st0:st0 + d * 127 + 1:d]
                                    src = o_[0:65, qq4 * 128:(qq4 + 1) * 128]
                                    if ci == 0:
                                        P.op("dve", lambda e: e.tensor_copy(out=dst, in_=src), reads=[o_], writes=[acc])
                                    else:
                                        P.op("dve", lambda e: e.tensor_tensor(out=dst, in0=dst, in1=src, op=ALU.add), reads=[o_, acc], writes=[acc])

                        nh_ = len(halves)
                        for hx in range(nh_ + 1):
                            if hx < nh_:
                                d_qk(hx)
                            if hx >= 1:
                                d_pv(hx - 1)
                    for qi in range(NQ):
                        res = fres[qi % 2]
                        tsl = slice(qi * 512, (qi + 1) * 512)
                        osb, rec, bcp, outb = res
                        P.op("dve", lambda e: e.reciprocal(out=rec[64:65, 0:512], in_=acc[64:65, tsl]), reads=[acc], writes=[rec])
                        finalize_part2(acc, 512, rows, tsl, res, src_off=qi * 512)
            P.end_stage()

        def epilogue(yps, x_t, g_post, g_next, tmp, junk, ssb, hb_, hdst, hdst_sl, tps, want_h):
            ss, rstd, ss2, rstd2 = ssb
            P.op("act", lambda e: e.activation(out=junk[:, 0:512], in_=yps[0][:, :], func=AF.Square, accum_out=ss[:, 0:1]), reads=[yps[0]], writes=[junk, ss])
            P.op("act", lambda e: e.activation(out=junk[:, 512:1024], in_=yps[1][:, :], func=AF.Square, accum_out=ss[:, 1:2]), reads=[yps[1]], writes=[junk, ss])
            P.op("dve", lambda e: e.tensor_tensor(out=ss[:, 0:1], in0=ss[:, 0:1], in1=ss[:, 1:2], op=ALU.add), reads=[ss], writes=[ss])
            rstd_from_ss(ss, D, rstd)
            for hf in range(2):
                P.op("dve", lambda e: e.scalar_tensor_tensor(out=tmp[:, hf * 512:(hf + 1) * 512], in0=yps[hf][:, :], scalar=rstd[:, 0:1], in1=g_post[:, hf * 512:(hf + 1) * 512],
                                                             op0=ALU.mult, op1=ALU.mult), reads=[yps[hf], rstd, g_post], writes=[tmp])
            P.op("dve", lambda e: e.tensor_tensor(out=x_t[:, :], in0=x_t[:, :], in1=tmp[:, :], op=ALU.add), reads=[x_t, tmp], writes=[x_t])
            if want_h:
                P.op("act", lambda e: e.activation(out=junk[:, :], in_=x_t[:, :], func=AF.Square, accum_out=ss2[:, 0:1]), reads=[x_t], writes=[junk, ss2])
                rstd_from_ss(ss2, D, rstd2)
                P.op("dve", lambda e: e.scalar_tensor_tensor(out=hb_[:, :], in0=x_t[:, :], scalar=rstd2[:, 0:1], in1=g_next[:, :], op0=ALU.mult, op1=ALU.mult),
                     reads=[x_t, rstd2, g_next], writes=[hb_])
                transposes_to(hb_, hdst, hdst_sl, tps)

        class EpiPipe:
            def __init__(self, s2, name, g_post, g_next, want_h, tps_pairs, pair=1):
                self.pair = pair
                self.NB = 2 + pair
                self.x = [P.sbuf(s2, "%s_x%d" % (name, i), [128, D], F32) for i in range(self.NB)]
                self.tmp = [P.sbuf(s2, "%s_tmp%d" % (name, i), [128, D], F32) for i in range(self.NB)]
                self.ss = [P.sbuf(s2, "%s_ss%d" % (name, i), [128, 4], F32) for i in range(self.NB)]
                self.s2_ = [P.sbuf(s2, "%s_sq%d" % (name, i), [128, 2], F32) for i in range(self.NB)]
                self.hb = [P.sbuf(s2, "%s_hb%d" % (name, i), [128, D], BF16) for i in range(2 * pair)]
                self.junk = P.sbuf(s2, "%s_junk" % name, [128, D], BF16)
                self.junk2 = [P.sbuf(s2, "%s_junk2%d" % (name, i), [128, D], BF16) for i in range(pair)]
                self.g_post, self.g_next, self.want_h, self.tps_pairs = g_post, g_next, want_h, tps_pairs
                self.pending = []
                self.cnt = 0

            def xbuf(self, t):
                return self.x[t % self.NB]

            def _run_pending(self):
                gens = [self._chain(t_, i_, k_) for k_, (t_, i_) in enumerate(self.pending)]
                self.pending = []
                while gens:
                    for g_ in list(gens):
                        try:
                            next(g_)
                        except StopIteration:
                            gens.remove(g_)

            def push(self, t, yps, info):
                if len(self.pending) >= self.pair:
                    self._run_pending()
                b = t % self.NB
                ss, tmp = self.ss[b], self.tmp[b]
                P.op("act", lambda e: e.activation(out=self.junk[:, 0:512], in_=yps[0][:, :], func=AF.Square, accum_out=ss[:, 0:1]), reads=[yps[0]], writes=[self.junk, ss])
                P.op("act", lambda e: e.activation(out=self.junk[:, 512:1024], in_=yps[1][:, :], func=AF.Square, accum_out=ss[:, 1:2]), reads=[yps[1]], writes=[self.junk, ss])
                for hf in range(2):
                    P.op("dve", lambda e: e.tensor_tensor(out=tmp[:, hf * 512:(hf + 1) * 512], in0=yps[hf][:, :], in1=self.g_post[:, hf * 512:(hf + 1) * 512], op=ALU.mult),
                         reads=[yps[hf], self.g_post], writes=[tmp])
                self.pending.append((t, info))

            def flush(self):
                if self.pending:
                    self._run_pending()

            def _chain(self, t, info, lane):
                b = t % self.NB
                ss, tmp, x_t, sq = self.ss[b], self.tmp[b], self.x[b], self.s2_[b]
                jk = self.junk2[lane]
                P.op("dve", lambda e: e.tensor_tensor(out=ss[:, 2:3], in0=ss[:, 0:1], in1=ss[:, 1:2], op=ALU.add), reads=[ss], writes=[ss])
                yield
                P.op("act", lambda e: e.activation(out=ss[:, 3:4], in_=ss[:, 2:3], func=AF.Ln, scale=1.0 / D, bias=EPSC()), reads=[ss, cols], writes=[ss])
                yield
                P.op("act", lambda e: e.activation(out=ss[:, 3:4], in_=ss[:, 3:4], func=AF.Exp, scale=-0.5), reads=[ss], writes=[ss])
                yield
                P.op("dve", lambda e: e.scalar_tensor_tensor(out=x_t[:, :], in0=tmp[:, :], scalar=ss[:, 3:4], in1=x_t[:, :], op0=ALU.mult, op1=ALU.add),
                     reads=[tmp, ss, x_t], writes=[x_t])
                yield
                if self.want_h:
                    P.op("act", lambda e: e.activation(out=jk[:, :], in_=x_t[:, :], func=AF.Square, accum_out=sq[:, 0:1]), reads=[x_t], writes=[jk, sq])
                    yield
                    P.op("act", lambda e: e.activation(out=sq[:, 1:2], in_=sq[:, 0:1], func=AF.Ln, scale=1.0 / D, bias=EPSC()), reads=[sq, cols], writes=[sq])
                    yield
                    P.op("act", lambda e: e.activation(out=sq[:, 1:2], in_=sq[:, 1:2], func=AF.Exp, scale=-0.5), reads=[sq], writes=[sq])
                    yield
                    h_b = self.hb[self.cnt % len(self.hb)]
                    tp_ = self.tps_pairs[self.cnt % len(self.tps_pairs)]
                    self.cnt += 1
                    P.op("dve", lambda e: e.scalar_tensor_tensor(out=h_b[:, :], in0=x_t[:, :], scalar=sq[:, 1:2], in1=self.g_next[:, :], op0=ALU.mult, op1=ALU.mult),
                         reads=[x_t, sq, self.g_next], writes=[h_b])
                    yield
                    transposes_to(h_b, info["hdst"], info["hsl"], tp_)
                    yield
                info["after"](t, x_t)

        def stage_c1(l):
            with ExitStack() as s2:
                wo = P.sbuf(s2, "wo", [128, 8, D], BF16)
                for k in range(8):
                    P.dma("pool", wo[:, k, :], wout[l, k * 128:(k + 1) * 128, :], reads=[wout], writes=[wo], slot=wo)
                g_post = load_gvec(s2, "c1_gpost", l, 1)
                g_next = load_gvec(s2, "c1_gnext", l, 2)
                mt = [P.sbuf(s2, "c1_mt%d" % i, [128, 8, 512], BF16) for i in range(2)]
                mt2 = [P.sbuf(s2, "c1_mu%d" % i, [128, 8, 512], BF16) for i in range(2)] if HS > 1 else None
                ho = [P.sbuf(s2, "c1_ho%d" % i, [128, 8, 512], BF16) for i in range(2)]
                yps = [P.psum(s2, "c1_y%d" % i) for i in range(4)]
                tps = [P.psum(s2, "c1_t%d" % i) for i in range(4)]
                ep = EpiPipe(s2, "c1e", g_post, g_next, True, [tps[0:2], tps[2:4]], pair=2)

                def load_m(n):
                    m_ = mt[n % 2]
                    P.dma("sp", m_[:, :, :], mixG[0][:, n * 512:(n + 1) * 512].rearrange("(k p) t -> p k t", p=128), reads=[mixG[0]], writes=[m_], slot=m_)
                    if HS > 1:
                        u_ = mt2[n % 2]
                        P.dma("sp", u_[:, :, :], mixG[1][:, n * 512:(n + 1) * 512].rearrange("(k p) t -> p k t", p=128), reads=[mixG[1]], writes=[u_], slot=u_)
                        P.op("dve", lambda e: e.tensor_scalar(out=m_[:, :, :], in0=m_[:, :, :], scalar1=selc[:, 0:1], scalar2=None, op0=ALU.mult), reads=[m_, selc], writes=[m_])
                        P.op("dve", lambda e: e.scalar_tensor_tensor(out=m_[:, :, :], in0=u_[:, :, :], scalar=selc[:, 1:2], in1=m_[:, :, :], op0=ALU.mult, op1=ALU.add),
                             reads=[u_, selc, m_], writes=[m_])

                def after(t, x_t):
                    n, sub = t // 4, t % 4
                    P.dma("sp", xs[t * 128:(t + 1) * 128, :], x_t[:, :], reads=[x_t], writes=[xs], slot=x_t)
                    if sub == 3:
                        o = ho[n % 2]
                        P.dma("sp", h2T[:, :, n * 512:(n + 1) * 512], o[:, :, :], reads=[o], writes=[h2T], slot=o)

                load_m(0)
                for t in range(NTO):
                    n, sub = t // 4, t % 4
                    if sub == 0 and n + 1 < NQO:
                        load_m(n + 1)
                    m_ = mt[n % 2]
                    x_t = ep.xbuf(t)
                    P.dma("sp", x_t[:, :], xs[t * 128:(t + 1) * 128, :], reads=[xs], writes=[x_t], slot=x_t)
                    yp = yps[2 * (t % 2):2 * (t % 2) + 2]
                    for hf in range(2):
                        for k in range(8):
                            P.op("pe", lambda e: e.matmul(yp[hf][:, :], lhsT=m_[:, k, sub * 128:(sub + 1) * 128], rhs=wo[:, k, hf * 512:(hf + 1) * 512], start=(k == 0), stop=(k == 7)),
                                 reads=[m_, wo], writes=[yp[hf]], inc=(k == 7))
                    ep.push(t, yp, {"hdst": ho[n % 2], "hsl": slice(sub * 128, (sub + 1) * 128), "after": after})
                ep.flush()
            P.end_stage()

        def stage_c2(l, last):
            T = 1024
            NTT = SO // T
            NJ = DFF // 128
            with ExitStack() as s2:
                wdn = P.sbuf(s2, "wdn", [128, NJ, D], BF16)
                for j in range(NJ):
                    P.dma("pool", wdn[:, j, :], wd[l, j * 128:(j + 1) * 128, :], reads=[wd], writes=[wdn], slot=wdn)
                g_post = load_gvec(s2, "c2_gpost", l, 3)
                g_next = load_gvec(s2, "c2_gnext", l + 1, 0) if not last else g_post
                h2 = [P.sbuf(s2, "c2_h2%d" % i, [128, 8, T], BF16) for i in range(2)]
                fT = P.sbuf(s2, "c2_fT", [128, NJ, T], BF16)
                NR = 2
                wgr = [P.sbuf(s2, "c2_wg%d" % i, [128, 8, 256], BF16) for i in range(NR)]
                wur = [P.sbuf(s2, "c2_wu%d" % i, [128, 8, 256], BF16) for i in range(NR)]
                sg = [P.sbuf(s2, "c2_sg%d" % i, [128, 512], F32) for i in range(2)]
                ho = [P.sbuf(s2, "c2_ho%d" % i, [128, 8, 512], BF16) for i in range(2)]
                gps = [P.psum(s2, "c2_g%d" % i) for i in range(2)]
                ups = [P.psum(s2, "c2_u%d" % i) for i in range(2)]
                yps = [P.psum(s2, "c2_y%d" % i) for i in range(2)]
                tps = [P.psum(s2, "c2_t%d" % i) for i in range(2)]
                ep = EpiPipe(s2, "c2e", g_post, g_next, not last, [tps])

                def after(t, x_t):
                    dst = out_d if last else xs
                    P.dma("sp", dst[t * 128:(t + 1) * 128, :], x_t[:, :], reads=[x_t], writes=[dst], slot=x_t)
                    if (not last) and t % 4 == 3:
                        n = t // 4
                        o = ho[n % 2]
                        P.dma("sp", hTown[:, n * 512:(n + 1) * 512].rearrange("(k p) t -> p k t", p=128), o[:, :, :], reads=[o], writes=[hTown], slot=o)

                def load_w(jp, slot_i):
                    for (ring, src) in ((wgr, wg), (wur, wu)):
                        r_ = ring[slot_i % NR]
                        P.dma("pool", r_[:, :, :], src[l, :, jp * 256:(jp + 1) * 256].rearrange("(k p) c -> p k c", p=128), reads=[src], writes=[r_], slot=r_)

                def load_h(tt):
                    h_ = h2[tt % 2]
                    P.dma("sp", h_[:, :, :], h2T[:, :, tt * T:(tt + 1) * T], reads=[h2T], writes=[h_], slot=h_)

                load_h(0)
                load_w(0, 0)
                for tt in range(NTT):
                    if tt + 1 < NTT:
                        load_h(tt + 1)
                    h_ = h2[tt % 2]
                    for jp in range(NJ // 2):
                        nxt = (tt * (NJ // 2) + jp + 1)
                        if nxt < NTT * (NJ // 2):
                            load_w(nxt % (NJ // 2), nxt)
                        cur = tt * (NJ // 2) + jp
                        wg_, wu_ = wgr[cur % NR], wur[cur % NR]
                        for jj in range(2):
                            j = jp * 2 + jj
                            for hf in range(T // 512):
                                gp = gps[(j * 2 + hf) % 2]
                                up = ups[(j * 2 + hf) % 2]
                                for (pp, w_) in ((gp, wg_), (up, wu_)):
                                    for k in range(8):
                                        P.op("pe", lambda e: e.matmul(pp[:, :], lhsT=w_[:, k, jj * 128:(jj + 1) * 128], rhs=h_[:, k, hf * 512:(hf + 1) * 512], start=(k == 0), stop=(k == 7)),
                                             reads=[w_, h_], writes=[pp], inc=(k == 7))
                                s_ = sg[(j * 2 + hf) % 2]
                                P.op("act", lambda e: e.activation(out=s_[:, :], in_=gp[:, :], func=AF.Silu), reads=[gp], writes=[s_])
                                P.op("dve", lambda e: e.tensor_tensor(out=fT[:, j, hf * 512:(hf + 1) * 512], in0=s_[:, :], in1=up[:, :], op=ALU.mult), reads=[s_, up], writes=[fT])
                    for sub in range(T // 128):
                        t = tt * (T // 128) + sub
                        x_t = ep.xbuf(t)
                        P.dma("sp", x_t[:, :], xs[t * 128:(t + 1) * 128, :], reads=[xs], writes=[x_t], slot=x_t)
                        for hf in range(2):
                            for j in range(NJ):
                                P.op("pe", lambda e: e.matmul(yps[hf][:, :], lhsT=fT[:, j, sub * 128:(sub + 1) * 128], rhs=wdn[:, j, hf * 512:(hf + 1) * 512], start=(j == 0), stop=(j == NJ - 1)),
                                     reads=[fT, wdn], writes=[yps[hf]], inc=(j == NJ - 1))
                        ep.push(t, yps, {"hdst": ho[(t // 4) % 2], "hsl": slice((t % 4) * 128, (t % 4 + 1) * 128), "after": after})
                ep.flush()
            P.end_stage()
            if HS > 1 and not last:
                for c in range(4):
                    collective(hTown, hTf, hTown[c * 256:(c + 1) * 256, :], hTf[c * HS * 256:(c + 1) * HS * 256, :])

        def dump_bf16(src, rows):
            with ExitStack() as s2:
                a = P.sbuf(s2, "dbg_a", [128, SO], BF16)
                b = P.sbuf(s2, "dbg_b", [128, SO], F32)
                for r0 in range(0, rows, 128):
                    P.dma("sp", a[:, :], src[r0:r0 + 128, :], reads=[src], writes=[a], slot=a)
                    P.op("dve", lambda e: e.tensor_copy(out=b[:, :], in_=a[:, :]), reads=[a], writes=[b])
                    P.dma("sp", dbg_out[r0:r0 + 128, :], b[:, :], reads=[b], writes=[dbg_out], slot=b)
            P.end_stage()

        P.end_stage()
        stage_tables()
        stage_h0()
        for l in range(depth):
            stage_a(l)
            def gather_mix(m_):
                if HS > 1:
                    for j in range(HS):
                        collective(mixX[j], mixG[j], mixX[j][m_ * NM:(m_ + 1) * NM, :], mixG[j][m_ * HS * NM:(m_ + 1) * HS * NM, :])
            stage_causal(l, "moba")
            gather_mix(0)
            stage_causal(l, "fox")
            gather_mix(1)
            stage_causal(l, "mla")
            gather_mix(2)
            stage_dil(l)
            gather_mix(3)
            if dbg == "mix" and l == 0:
                dump_bf16(mixG[0], HS * 4 * NM)
            stage_c1(l)
            stage_c2(l, l == depth - 1)
        P.barrier()
        print("program built: instrs=%d sems=%d" % (P.ninstr, P.nsem))
    return nc, groups


_CACHE = {}


def prepare_weights(inp, depth, heads, groups):
    w_in = np.asarray(inp["w_in"], np.float32)
    NH = len(heads)
    cols = np.concatenate([c for _, c in groups]).astype(np.int64)
    wA = np.ascontiguousarray(w_in[:depth][:, :, cols])
    perm32 = np.concatenate([np.arange(16, 32), np.arange(0, 16)])
    wq = np.asarray(inp["w_mla_q_up"], np.float32)[:depth]
    qcols = []
    for h in heads:
        qcols.append(h * 96 + np.arange(96))
        qcols.append(np.concatenate([h * 96 + np.arange(64), h * 96 + 64 + perm32]))
    wqu = np.ascontiguousarray(wq[:, :, np.concatenate(qcols)])
    wkv = np.asarray(inp["w_mla_kv_up"], np.float32)[:depth]
    kcols = np.concatenate([h * 128 + np.arange(128) for h in heads])
    wkvu = np.ascontiguousarray(wkv[:, :, kcols])
    gvv = np.stack([np.asarray(inp[k], np.float32)[:depth] for k in ("g_pre_mix", "g_post_mix", "g_pre_ffn", "g_post_ffn")], axis=1)
    d = {
        "wA": wA, "wqu": wqu, "wkvu": wkvu,
        "wout": np.ascontiguousarray(np.asarray(inp["w_out"], np.float32)[:depth]),
        "wg": np.ascontiguousarray(np.asarray(inp["w_gate"], np.float32)[:depth]),
        "wu": np.ascontiguousarray(np.asarray(inp["w_up"], np.float32)[:depth]),
        "wd": np.ascontiguousarray(np.asarray(inp["w_down"], np.float32)[:depth]),
        "gv": np.ascontiguousarray(gvv),
        "gq": np.ascontiguousarray(np.asarray(inp["g_mla_q"], np.float32)[:depth, :, None]),
        "gkv": np.ascontiguousarray(np.asarray(inp["g_mla_kv"], np.float32)[:depth, :, None]),
        "bfg": np.ascontiguousarray(np.asarray(inp["b_forget"], np.float32)[:depth][:, heads, None]),
    }
    return d


def run(inp, S, depth, B, dbg=None, HS=2):
    NH = 4 // HS
    ncores = B * HS
    key = (S, depth, NH, HS, ncores, dbg)
    if key not in _CACHE:
        _CACHE[key] = build_program(S, depth, NH, HS, dbg, ncores)
    nc, _ = _CACHE[key]
    consts = make_consts(S)
    x = np.asarray(inp["x"], np.float32)
    pos = np.asarray(inp["positions"], np.int32)
    SO = S // HS
    perm = []
    for m in range(4):
        for r in range(HS):
            for i in range(NH):
                perm.append(m * 256 + (r * NH + i) * 64 + np.arange(64))
    perm = np.concatenate(perm)
    per_half = []
    for hf in range(HS):
        heads = [hf * NH + i for i in range(NH)]
        wd_ = prepare_weights(inp, depth, heads, make_groups(heads))
        wd_["wout"] = np.ascontiguousarray(wd_["wout"][:, perm, :])
        sel = np.zeros((128, 2), np.float32)
        sel[:, hf] = 1.0
        wd_["sel"] = sel
        per_half.append(wd_)
    in_maps = []
    for b in range(B):
        for hf in range(HS):
            m = dict(per_half[hf])
            m.update(consts)
            m["x"] = np.ascontiguousarray(x[b])
            if HS > 1:
                m["x_own"] = np.ascontiguousarray(x[b, hf * SO:(hf + 1) * SO])
            m["pos"] = np.ascontiguousarray(pos[b][None, :])
            in_maps.append(m)
    res = run_bass_kernel_spmd(nc, in_maps, core_ids=list(range(ncores)))
    out = np.empty((B, S, D), np.float32)
    for b in range(B):
        for hf in range(HS):
            out[b, hf * SO:(hf + 1) * SO] = np.asarray(res.results[b * HS + hf]["out"], np.float32)
    if dbg:
        return out, [np.asarray(r["dbg"]) for r in res.results]
    return out


def kernel(**inputs):
    return run(inputs, 8192, 4, 4, HS=2)
```
